# Optimizing a Trainium2 kernel written in Bass

```python
import math
import jax, jax.numpy as jnp
from jax import lax
import numpy as np

D_MODEL = 1024
BATCH = 16
SEQ = 2048
DEPTH = 2

PLE_DIM = 256
Q_BLOCK = 128
RNN_WIDTH = 512
RNN_BLOCKS = 8
CONV_WIDTH = 4
LRU_C = 8.0
DSA_HEADS = 8
DSA_HEAD_DIM = 64
IDX_HEADS = 4
IDX_DIM = 64
DSA_TOPK_MAX = 256
MLA_HEADS = 16
MLA_Q_LORA = 512
MLA_KV_LORA = 256
MLA_NOPE = 64
MLA_ROPE = 32
MLA_V = 64
ROPE_BASE = 10000.0
D_FF = 4 * D_MODEL
DN_ALPHA = (2 * DEPTH) ** 0.25
DN_BETA = (8 * DEPTH) ** -0.25
LN_EPS = 1e-5
RMS_EPS = 1e-6
N_EVEN = (DEPTH + 1) // 2
N_ODD = DEPTH // 2
EVEN_SPLITS = (RNN_WIDTH, RNN_WIDTH, DSA_HEADS * DSA_HEAD_DIM, DSA_HEAD_DIM, DSA_HEAD_DIM,
               IDX_HEADS * IDX_DIM, IDX_DIM, IDX_HEADS)
EVEN_IN = sum(EVEN_SPLITS)
EVEN_MIX = RNN_WIDTH + DSA_HEADS * DSA_HEAD_DIM
MLA_DOWN = MLA_Q_LORA + MLA_KV_LORA + MLA_ROPE
MLA_MIX = MLA_HEADS * MLA_V

kernel_name = "hybrid_rglru_dsa_mla_deepnorm"


def layer_norm(x, g, b):
    xf = x.astype(jnp.float32)
    mu = jnp.mean(xf, axis=-1, keepdims=True)
    var = jnp.mean(jnp.square(xf - mu), axis=-1, keepdims=True)
    y = (xf - mu) * lax.rsqrt(var + LN_EPS) * g.astype(jnp.float32) + b.astype(jnp.float32)
    return y.astype(x.dtype)


def rms_norm(x, g):
    xf = x.astype(jnp.float32)
    y = xf * lax.rsqrt(jnp.mean(jnp.square(xf), axis=-1, keepdims=True) + RMS_EPS)
    return (y * g.astype(jnp.float32)).astype(x.dtype)


def causal_depthwise_conv(x, w, b):
    c = x.shape[-1]
    y = lax.conv_general_dilated(x, w[:, None, :], window_strides=(1,), padding=[(CONV_WIDTH - 1, 0)],
                                 dimension_numbers=("NWC", "WIO", "NWC"), feature_group_count=c)
    return y + b


def rg_lru(x, w_a, b_a, w_x, b_x, lam):
    bsz, s, c = x.shape
    xb = x.reshape(bsz, s, RNN_BLOCKS, c // RNN_BLOCKS)
    r = jax.nn.sigmoid(jnp.einsum("bsnc,ncd->bsnd", xb, w_a).reshape(bsz, s, c) + b_a)
    i = jax.nn.sigmoid(jnp.einsum("bsnc,ncd->bsnd", xb, w_x).reshape(bsz, s, c) + b_x)
    log_a = -LRU_C * r.astype(jnp.float32) * jax.nn.softplus(-lam.astype(jnp.float32))
    a = jnp.exp(log_a)
    u = jnp.sqrt(-jnp.expm1(2.0 * log_a)) * (i * x).astype(jnp.float32)

    def combine(left, right):
        a1, b1 = left
        a2, b2 = right
        return a1 * a2, a2 * b1 + b2

    _, h = lax.associative_scan(combine, (a, u), axis=1)
    return h.astype(x.dtype)


def alibi_slopes(n):
    return jnp.exp2(-8.0 * (jnp.arange(n, dtype=jnp.float32) + 1.0) / n)


def rope(x, cos, sin):
    half = x.shape[-1] // 2
    x1, x2 = x[..., :half], x[..., half:]
    return jnp.concatenate([x1 * cos - x2 * sin, x2 * cos + x1 * sin], axis=-1)


def to_blocks(a, nb):
    a = a.reshape((a.shape[0], nb, Q_BLOCK) + a.shape[2:])
    return jnp.moveaxis(a, 1, 0)


def from_blocks(o):
    o = jnp.moveaxis(o, 0, 1)
    return o.reshape((o.shape[0], o.shape[1] * o.shape[2]) + o.shape[3:])


def gather_rows(src, idx):
    return jax.vmap(lambda sb, ib: sb[ib])(src, idx)


def dsa_attention(q, k, v, iq, ik, iw):
    bsz, s = q.shape[:2]
    nb = s // Q_BLOCK
    topk = min(DSA_TOPK_MAX, s // 4)
    slopes = alibi_slopes(DSA_HEADS)
    key_pos = jnp.arange(s, dtype=jnp.int32)
    iw = iw.astype(jnp.float32) * (IDX_HEADS ** -0.5 * IDX_DIM ** -0.5)

    def block(args):
        qb, iqb, iwb, t0 = args
        qpos = t0 + jnp.arange(Q_BLOCK, dtype=jnp.int32)
        dots = jnp.einsum("bthd,bsd->bths", iqb, ik).astype(jnp.float32)
        isc = jnp.einsum("bths,bth->bts", jax.nn.relu(dots), iwb)
        causal = key_pos[None, :] <= qpos[:, None]
        isc = jnp.where(causal[None], isc, -jnp.inf)
        _, idx = lax.top_k(isc, topk)
        ksel = gather_rows(k, idx)
        vsel = gather_rows(v, idx)
        logits = jnp.einsum("bthd,btkd->bthk", qb, ksel).astype(jnp.float32) * (DSA_HEAD_DIM ** -0.5)
        dist = qpos[None, :, None] - idx
        logits = logits - slopes[None, None, :, None] * dist.astype(jnp.float32)[:, :, None, :]
        logits = jnp.where((dist >= 0)[:, :, None, :], logits, -jnp.inf)
        probs = jax.nn.softmax(logits, axis=-1).astype(v.dtype)
        return jnp.einsum("bthk,btkd->bthd", probs, vsel)

    starts = jnp.arange(nb, dtype=jnp.int32) * Q_BLOCK
    out = lax.map(block, (to_blocks(q, nb), to_blocks(iq, nb), to_blocks(iw, nb), starts))
    return from_blocks(out)


def mla_attention(q, k, v):
    bsz, s = q.shape[:2]
    nb = s // Q_BLOCK
    scale = q.shape[-1] ** -0.5
    key_pos = jnp.arange(s, dtype=jnp.int32)

    def block(args):
        qb, t0 = args
        qpos = t0 + jnp.arange(Q_BLOCK, dtype=jnp.int32)
        logits = jnp.einsum("bthd,bshd->bhts", qb, k).astype(jnp.float32) * scale
        causal = key_pos[None, :] <= qpos[:, None]
        logits = jnp.where(causal[None, None], logits, -jnp.inf)
        probs = jax.nn.softmax(logits, axis=-1).astype(v.dtype)
        return jnp.einsum("bhts,bshd->bthd", probs, v)

    starts = jnp.arange(nb, dtype=jnp.int32) * Q_BLOCK
    out = lax.map(block, (to_blocks(q, nb), starts))
    return from_blocks(out)


def hybrid_mixer(x, w_in, conv_w, conv_b, ga_w, ga_b, gx_w, gx_b, lam, w_out):
    bsz, s, _ = x.shape
    proj = x @ w_in
    split_points = np.cumsum(EVEN_SPLITS)[:-1].tolist()
    xr, yr, q, k, v, iq, ik, iw = jnp.split(proj, split_points, axis=-1)
    xr = causal_depthwise_conv(xr, conv_w, conv_b)
    rec = rg_lru(xr, ga_w, ga_b, gx_w, gx_b, lam) * jax.nn.gelu(yr)
    att = dsa_attention(q.reshape(bsz, s, DSA_HEADS, DSA_HEAD_DIM), k, v,
                        iq.reshape(bsz, s, IDX_HEADS, IDX_DIM), ik, iw)
    mixed = jnp.concatenate([rec, att.reshape(bsz, s, DSA_HEADS * DSA_HEAD_DIM)], axis=-1)
    return mixed @ w_out


def mla_mixer(x, w_down, q_norm, kv_norm, w_uq, w_ukv, w_out):
    bsz, s, _ = x.shape
    down = x @ w_down
    cq, ckv, kr = jnp.split(down, [MLA_Q_LORA, MLA_Q_LORA + MLA_KV_LORA], axis=-1)
    q = (rms_norm(cq, q_norm) @ w_uq).reshape(bsz, s, MLA_HEADS, MLA_NOPE + MLA_ROPE)
    kv = (rms_norm(ckv, kv_norm) @ w_ukv).reshape(bsz, s, MLA_HEADS, MLA_NOPE + MLA_V)
    q_nope, q_rope = q[..., :MLA_NOPE], q[..., MLA_NOPE:]
    k_nope, v = kv[..., :MLA_NOPE], kv[..., MLA_NOPE:]
    pos = jnp.arange(s, dtype=jnp.float32)
    freq = ROPE_BASE ** (-jnp.arange(0, MLA_ROPE, 2, dtype=jnp.float32) / MLA_ROPE)
    ang = pos[:, None] * freq[None, :]
    cos = jnp.cos(ang).astype(x.dtype)
    sin = jnp.sin(ang).astype(x.dtype)
    q_rope = rope(q_rope, cos[:, None, :], sin[:, None, :])
    k_rope = rope(kr, cos, sin)
    qf = jnp.concatenate([q_nope, q_rope], axis=-1)
    kf = jnp.concatenate([k_nope, jnp.broadcast_to(k_rope[:, :, None, :], (bsz, s, MLA_HEADS, MLA_ROPE))], axis=-1)
    o = mla_attention(qf, kf, v)
    return o.reshape(bsz, s, MLA_MIX) @ w_out


def setup_inputs(seed: int = 0) -> dict:
    key = jax.random.key(seed)
    ks = jax.random.split(key, 32)

    def nrm(k, shape, scale):
        return jax.random.normal(k, shape, jnp.float32) * scale

    u = jax.random.uniform(ks[12], (N_EVEN, RNN_WIDTH), jnp.float32, 0.9, 0.999)
    sig = u ** (1.0 / LRU_C)
    lam = jnp.log(sig) - jnp.log1p(-sig)
    bw = RNN_WIDTH // RNN_BLOCKS
    return {
        "x": nrm(ks[0], (BATCH, SEQ, D_MODEL), 1.0),
        "p": nrm(ks[1], (DEPTH, BATCH, SEQ, PLE_DIM), 1.0),
        "ln1_g": 1.0 + nrm(ks[2], (DEPTH, D_MODEL), 0.02),
        "ln1_b": nrm(ks[3], (DEPTH, D_MODEL), 0.02),
        "ln2_g": 1.0 + nrm(ks[4], (DEPTH, D_MODEL), 0.02),
        "ln2_b": nrm(ks[5], (DEPTH, D_MODEL), 0.02),
        "mlp_w1": nrm(ks[6], (DEPTH, D_MODEL, D_FF), D_MODEL ** -0.5),
        "mlp_w2": nrm(ks[7], (DEPTH, D_FF, D_MODEL), DN_BETA * D_FF ** -0.5),
        "ple_w_proj": nrm(ks[8], (DEPTH, PLE_DIM, D_MODEL), DN_BETA * PLE_DIM ** -0.5),
        "ple_w_gate": nrm(ks[9], (DEPTH, D_MODEL, D_MODEL), D_MODEL ** -0.5),
        "hy_w_in": nrm(ks[10], (N_EVEN, D_MODEL, EVEN_IN), D_MODEL ** -0.5),
        "hy_conv_w": nrm(ks[11], (N_EVEN, CONV_WIDTH, RNN_WIDTH), CONV_WIDTH ** -0.5),
        "hy_conv_b": nrm(ks[13], (N_EVEN, RNN_WIDTH), 0.01),
        "hy_ga_w": nrm(ks[14], (N_EVEN, RNN_BLOCKS, bw, bw), bw ** -0.5),
        "hy_ga_b": nrm(ks[15], (N_EVEN, RNN_WIDTH), 0.01),
        "hy_gx_w": nrm(ks[16], (N_EVEN, RNN_BLOCKS, bw, bw), bw ** -0.5),
        "hy_gx_b": nrm(ks[17], (N_EVEN, RNN_WIDTH), 0.01),
        "hy_lambda": lam,
        "hy_w_out": nrm(ks[18], (N_EVEN, EVEN_MIX, D_MODEL), DN_BETA * EVEN_MIX ** -0.5),
        "mla_w_down": nrm(ks[19], (N_ODD, D_MODEL, MLA_DOWN), D_MODEL ** -0.5),
        "mla_q_norm": 1.0 + nrm(ks[20], (N_ODD, MLA_Q_LORA), 0.02),
        "mla_kv_norm": 1.0 + nrm(ks[21], (N_ODD, MLA_KV_LORA), 0.02),
        "mla_w_uq": nrm(ks[22], (N_ODD, MLA_Q_LORA, MLA_HEADS * (MLA_NOPE + MLA_ROPE)), MLA_Q_LORA ** -0.5),
        "mla_w_ukv": nrm(ks[23], (N_ODD, MLA_KV_LORA, MLA_HEADS * (MLA_NOPE + MLA_V)), MLA_KV_LORA ** -0.5),
        "mla_w_out": nrm(ks[24], (N_ODD, MLA_MIX, D_MODEL), DN_BETA * MLA_MIX ** -0.5),
    }


def reference(x, p, ln1_g, ln1_b, ln2_g, ln2_b, mlp_w1, mlp_w2, ple_w_proj, ple_w_gate,
              hy_w_in, hy_conv_w, hy_conv_b, hy_ga_w, hy_ga_b, hy_gx_w, hy_gx_b, hy_lambda, hy_w_out,
              mla_w_down, mla_q_norm, mla_kv_norm, mla_w_uq, mla_w_ukv, mla_w_out):
    for i in range(DEPTH):
        j = i // 2
        if i % 2 == 0:
            m = hybrid_mixer(x, hy_w_in[j], hy_conv_w[j], hy_conv_b[j], hy_ga_w[j], hy_ga_b[j],
                             hy_gx_w[j], hy_gx_b[j], hy_lambda[j], hy_w_out[j])
        else:
            m = mla_mixer(x, mla_w_down[j], mla_q_norm[j], mla_kv_norm[j], mla_w_uq[j],
                          mla_w_ukv[j], mla_w_out[j])
        h = layer_norm(DN_ALPHA * x + m, ln1_g[i], ln1_b[i])
        f = jnp.square(jax.nn.relu(h @ mlp_w1[i])) @ mlp_w2[i]
        e = jax.nn.sigmoid(h @ ple_w_gate[i]) * (p[i] @ ple_w_proj[i])
        x = layer_norm(DN_ALPHA * h + f + e, ln2_g[i], ln2_b[i])
    return x
```

```python
import contextlib
import numpy as np
import concourse.bass as bass
import concourse.mybir as mybir
from concourse.bass_utils import run_bass_kernel_spmd

F32 = mybir.dt.float32
BF16 = mybir.dt.bfloat16
F32R = mybir.dt.float32r
AF = mybir.ActivationFunctionType
ALU = mybir.AluOpType

S = 2048
D = 1024
TT = 512
NCH = S // TT
NT = S // 128
DN_ALPHA = 4 ** 0.25
LN_EPS = 1e-5
RMS_EPS = 1e-6
import os
DBG_BARRIER = bool(int(os.environ.get('DBG_BARRIER', '0')))
NEG_FILL = -1.0e30
NEG_REPL = -2.0e30
MASK_NEG = -30000.0

SP_LN = 0
SP_CONVW = 64
SP_CONVB = 80
SP_GAB = 84
SP_GXB = 88
SP_LAM = 92
SP_QN = 96
SP_KVN = 100
SP_N = 104

WIN_COLS = 1988
WIN_TM = 1920


class Sched:
    def __init__(self, nc, es):
        self.nc = nc
        self.engs = {"pe": nc.tensor, "act": nc.scalar, "dve": nc.vector, "pool": nc.gpsimd, "sp": nc.sync}
        self.comp = ["pe", "act", "dve", "pool"]
        self.csem, self.ccnt, self.last = {}, {}, {}
        for e in self.comp:
            self.csem[e] = es.enter_context(nc.semaphore("c_" + e))
            self.ccnt[e] = 0
            self.last[e] = None
        self.rings = {}
        for rname, n in (("pool", 5), ("sp", 24), ("bg", 3)):
            self.rings[rname] = {"sem": [es.enter_context(nc.semaphore("d%s%d" % (rname, i))) for i in range(n)],
                                 "cnt": [0] * n, "next": 0}
        self.seen = {}
        self.track = {}
        self.ninst = 0

    def _flush(self, e):
        if e in self.last and self.last[e] is not None:
            self.ccnt[e] += 1
            self.last[e].then_inc(self.csem[e], 1)
            self.last[e] = None

    def _wait(self, e, key, val):
        kind, who = key
        if kind == "c":
            if who == e and e == "pe":
                return
            if val > self.ccnt[who]:
                self._flush(who)
            sem = self.csem[who]
        else:
            sem = self.rings[who[0]]["sem"][who[1]]
        if self.seen.get((e, key), 0) >= val:
            return
        self._flush(e)
        self.engs[e].wait_ge(sem, val)
        self.seen[(e, key)] = val
        self.ninst += 1

    def _deps(self, e, reads, writes):
        deps = {}
        for k in reads:
            t = self.track.get(k)
            if t and t[0] is not None:
                kk, v = t[0]
                deps[kk] = max(deps.get(kk, 0), v)
            if t and isinstance(k, tuple) and k[0] == "ps":
                for kk, v in t[1].items():
                    if kk != ("c", e):
                        deps[kk] = max(deps.get(kk, 0), v)
        for k in writes:
            t = self.track.get(k)
            if t:
                if t[0] is not None:
                    kk, v = t[0]
                    deps[kk] = max(deps.get(kk, 0), v)
                for kk, v in t[1].items():
                    deps[kk] = max(deps.get(kk, 0), v)
        for kk, v in deps.items():
            self._wait(e, kk, v)

    def _record(self, ev, reads, writes):
        kk, v = ev
        for k in reads:
            t = self.track.setdefault(k, [None, {}])
            t[1][kk] = max(t[1].get(kk, 0), v)
        for k in writes:
            self.track[k] = [ev, {}]

    def op(self, e, fn, reads=(), writes=()):
        self._deps(e, reads, writes)
        h = fn(self.engs[e])
        self.last[e] = h
        self.ninst += 1
        self._record((("c", e), self.ccnt[e] + 1), reads, writes)
        return h

    def dma(self, q, out, in_, reads=(), writes=(), bg=False):
        self._deps(q, reads, writes)
        rname = "bg" if bg else q
        ring = self.rings[rname]
        idx = ring["next"]
        ring["next"] = (idx + 1) % len(ring["sem"])
        if ring["cnt"][idx] > 0:
            self._wait(q, ("d", (rname, idx)), ring["cnt"][idx])
        h = self.engs[q].dma_start(out=out, in_=in_)
        ring["cnt"][idx] += 16
        h.then_inc(ring["sem"][idx], 16)
        self.ninst += 1
        self._record((("d", (rname, idx)), ring["cnt"][idx]), reads, writes)

    def barrier(self, final=False):
        for e in self.comp:
            self._flush(e)
        for e in ["pe", "act", "dve", "pool", "sp"]:
            for who in self.comp:
                if self.ccnt[who] > 0 and not (who == e and e == "pe"):
                    self._wait(e, ("c", who), self.ccnt[who])
            for rname, ring in self.rings.items():
                if rname == "bg" and not final:
                    continue
                for idx in range(len(ring["sem"])):
                    if ring["cnt"][idx] > 0:
                        self._wait(e, ("d", (rname, idx)), ring["cnt"][idx])
        keep = {k: v for k, v in self.track.items() if isinstance(k, tuple) and k[0] == "wb"}
        self.track.clear()
        if not final:
            self.track.update(keep)


def _w_specs():
    return {
        "w_in": (1024, WIN_COLS),
        "gates": (128, 1024),
        "w_out0": (1024, 1024),
        "w_out1": (1024, 1024),
        "w1": (2 * 32 * 128, 1024),
        "w2": (2 * 8 * 128 * 2, 2048),
        "wg": (2 * 128 * 8, 1024),
        "wp": (2 * 128 * 2, 1024),
        "w_down": (1024, 896),
        "w_uq": (512, 2048),
        "w_ukv": (256, 2048),
    }


def build(dbg_stage=None):
    nc = bass.Bass("TRN2", target_bir_lowering=False)
    dbg_out = {}
    with contextlib.ExitStack() as es:
        sc = Sched(nc, es)
        ent = es.enter_context

        def dram_in(name, shape, dt=F32):
            return nc.dram_tensor(name, list(shape), dt, kind="ExternalInput").ap()

        xT = dram_in("xT", [2, D, S])
        pT = dram_in("pT", [2, 2, 256, S])
        small_d = dram_in("small", [128, SP_N])
        c_ident = dram_in("c_ident", [128, 128])
        c_causal = dram_in("c_causal", [128, 128])
        c_tri = dram_in("c_tri", [128, 128])
        c_kaug = dram_in("c_kaug", [4, S])
        c_qaug = dram_in("c_qaug", [4, 8 * S])
        c_cos = dram_in("c_cos", [32, S])
        c_sin = dram_in("c_sin", [32, S])
        wf, wb = {}, {}
        for name, (r, c) in _w_specs().items():
            wf[name] = dram_in(name, [r, c])
            wb[name] = nc.dram_tensor(name + "_b", [r, c], BF16, kind="Internal").ap()
        x1T = nc.dram_tensor("x1T", [2, D, S], F32, kind="Internal").ap()
        x1b = nc.dram_tensor("x1b", [2, D, S], BF16, kind="Internal").ap()
        yT = nc.dram_tensor("yT", [2, D, S], F32, kind="ExternalOutput").ap()

        def dbg(name, src_ap, shape, key, dt=F32):
            t = nc.dram_tensor("dbg_" + name, list(shape), dt, kind="ExternalOutput").ap()
            dbg_out[name] = (list(shape), dt)
            sc.dma("pool", t, src_ap, reads=[key])

        def sb(name, shape, dt):
            return ent(nc.sbuf_tensor("s_" + name, list(shape), dt))

        psf = [ent(nc.psum_tensor("psf%d" % i, [128, 512], F32)) for i in range(7)]
        psb = ent(nc.psum_tensor("psb", [128, 1024], BF16))
        ident_b = sb("ident_b", [128, 128], BF16)
        identneg_b = sb("identneg_b", [128, 128], BF16)
        causal_f = sb("causal_f", [128, 128], F32)
        tri_b = sb("tri_b", [128, 128], BF16)
        ones_f = sb("ones_f", [128, 128], F32)
        small = sb("small", [128, SP_N], F32)
        negc = sb("negc", [128, 8], F32)
        mixedT = sb("mixedT", [128, 8, S], BF16)

        conv_tasks = []
        wchunks = {}
        for name in ["w_in", "gates", "w_out0", "wg", "wp", "w1", "w2", "w_down", "w_uq", "w_ukv", "w_out1"]:
            r, c = _w_specs()[name]
            step = max(16, (1 << 19) // c // 16 * 16)
            wchunks[name] = []
            for r0 in range(0, r, step):
                r1 = min(r, r0 + step)
                wchunks[name].append((r0, r1))
                conv_tasks.append((name, r0, r1))
        def _prio(t):
            name, r0, r1 = t
            half = _w_specs()[name][0] // 2
            if name in ("w1", "w2", "wg", "wp") and r0 >= half:
                return 2
            if name in ("w_down", "w_uq", "w_ukv", "w_out1"):
                return 1
            return 0
        conv_tasks.sort(key=_prio)
        conv_pos = [0]

        def conv_pump(n):
            while n > 0 and conv_pos[0] < len(conv_tasks):
                name, r0, r1 = conv_tasks[conv_pos[0]]
                conv_pos[0] += 1
                n -= 1
                sc.dma("pool", wb[name][r0:r1, :], wf[name][r0:r1, :], writes=[("wb", name, r0)], bg=True)

        def wbk(name, a=None, b=None):
            r = _w_specs()[name][0]
            a = 0 if a is None else a
            b = r if b is None else b
            need = [(name, r0, r1) for (r0, r1) in wchunks[name] if r0 < b and r1 > a]
            while any(t in conv_tasks[conv_pos[0]:] for t in need):
                conv_pump(1)
            return [("wb", n_, r0) for (n_, r0, r1) in need]

        conv_pump(6)
        sc.dma("pool", ident_b[:], c_ident[:, :], writes=["ident_b"])
        sc.dma("pool", tri_b[:], c_tri[:, :], writes=["tri_b"])
        sc.dma("sp", causal_f[:], c_causal[:, :], writes=["causal_f"])
        sc.dma("sp", small[:], small_d[:, :], writes=["small"])
        sc.op("dve", lambda e: e.memset(ones_f[:], 1.0), writes=["ones_f"])
        sc.op("dve", lambda e: e.tensor_scalar(out=identneg_b[:], in0=ident_b[:], scalar1=MASK_NEG, scalar2=None,
                                               op0=ALU.mult), reads=["ident_b"], writes=["identneg_b"])
        sc.op("act", lambda e: e.activation(out=negc[:, 4:8], in_=small[:, SP_LAM:SP_LAM + 4], func=AF.Exp, scale=-1.0),
              reads=["small"], writes=["negc_t"])
        sc.op("act", lambda e: e.activation(out=negc[:, 4:8], in_=negc[:, 4:8], func=AF.Ln, bias=1.0),
              reads=["negc_t"], writes=["negc_t"])
        sc.op("dve", lambda e: e.tensor_scalar(out=negc[:, 0:4], in0=negc[:, 4:8], scalar1=-8.0, scalar2=None,
                                               op0=ALU.mult), reads=["negc_t"], writes=["negc"])
        sc.barrier()

        def ln_feature_major(z, zkey, layer, which, out_f, out_f_key, out_b, out_b_key, hid, st, stkey):
            gof = SP_LN + layer * 32 + (0 if which == 1 else 16)
            ps_m, ps_q = psf[5], psf[6]
            tmp = hid.bitcast(F32)[:, 16:32, :].rearrange("p (m a) b -> p m (a b)", a=2)
            tk = lambda m: [("hid", 16 + 2 * m), ("hid", 17 + 2 * m)]
            for m in range(8):
                sc.op("act", lambda e: e.activation(out=hid[:, m, :], in_=z[:, m, :], func=AF.Square),
                      reads=[(zkey, m)], writes=[("hid", m)])
                sc.op("pool", lambda e: e.tensor_copy(out=hid[:, 8 + m, :], in_=z[:, m, :]),
                      reads=[(zkey, m)], writes=[("hid", 8 + m)])
            for m in range(8):
                sc.op("pe", lambda e: e.matmul(ps_m[:], lhsT=ones_b[:], rhs=hid[:, 8 + m, :], start=(m == 0), stop=(m == 7)),
                      reads=[("hid", 8 + m), "ones_b"], writes=[("ps", 5)])
            for m in range(8):
                sc.op("pe", lambda e: e.matmul(ps_q[:], lhsT=ones_b[:], rhs=hid[:, m, :], start=(m == 0), stop=(m == 7)),
                      reads=[("hid", m), "ones_b"], writes=[("ps", 6)])
            mean, msq, var, rstd = st[:, 0, :], st[:, 1, :], st[:, 2, :], st[:, 3, :]
            sc.op("act", lambda e: e.activation(out=mean, in_=ps_m[:], func=AF.Copy, scale=1.0 / D),
                  reads=[("ps", 5)], writes=[(stkey, 0)])
            sc.op("act", lambda e: e.activation(out=msq, in_=ps_m[:], func=AF.Square, scale=1.0 / D),
                  reads=[("ps", 5)], writes=[(stkey, 1)])
            sc.op("dve", lambda e: e.scalar_tensor_tensor(out=var, in0=ps_q[:], scalar=1.0 / D, in1=msq,
                                                          op0=ALU.mult, op1=ALU.subtract),
                  reads=[("ps", 6), (stkey, 1)], writes=[(stkey, 2)])
            sc.op("act", lambda e: e.activation(out=var, in_=var, func=AF.Ln, bias=eps_t[:, 0:1]),
                  reads=[(stkey, 2), "eps_t"], writes=[(stkey, 2)])
            sc.op("act", lambda e: e.activation(out=rstd, in_=var, func=AF.Exp, scale=-0.5),
                  reads=[(stkey, 2)], writes=[(stkey, 3)])
            for m in range(8):
                eng = "pool" if m in (1, 3, 5) else "dve"
                sc.op(eng, lambda e: e.tensor_tensor(out=tmp[:, m, :], in0=z[:, m, :], in1=mean, op=ALU.subtract),
                      reads=[(zkey, m), (stkey, 0)], writes=tk(m))
                sc.op(eng, lambda e: e.tensor_tensor(out=tmp[:, m, :], in0=tmp[:, m, :], in1=rstd, op=ALU.mult),
                      reads=tk(m) + [(stkey, 3)], writes=tk(m))
                sc.op("act", lambda e: e.activation(out=out_b[:, m, :], in_=tmp[:, m, :], func=AF.Identity,
                                                    scale=small[:, gof + m:gof + m + 1], bias=small[:, gof + 8 + m:gof + 9 + m]),
                      reads=tk(m) + ["small"], writes=[(out_b_key, m)])
                sc.op("dve", lambda e: e.tensor_scalar(out=out_f[:, m, :], in0=tmp[:, m, :],
                                                       scalar1=small[:, gof + m:gof + m + 1],
                                                       scalar2=small[:, gof + 8 + m:gof + 9 + m],
                                                       op0=ALU.mult, op1=ALU.add),
                      reads=tk(m) + ["small"], writes=[(out_f_key, m)])

        ones_b = sb("ones_b", [128, 128], BF16)
        sc.op("dve", lambda e: e.memset(ones_b[:], 1.0), writes=["ones_b"])
        eps_t = sb("eps_t", [128, 2], F32)
        sc.op("dve", lambda e: e.memset(eps_t[:, 0:1], LN_EPS), writes=["eps_t0"])
        sc.op("dve", lambda e: e.memset(eps_t[:, 1:2], RMS_EPS), writes=["eps_t1"])
        sc.barrier()

        def phase_c(layer, s, x_src, final):
            with contextlib.ExitStack() as es2:
                e2 = es2.enter_context

                def sb2(name, shape, dt):
                    return e2(nc.sbuf_tensor("c%d%d_" % (layer, s) + name, list(shape), dt))

                wout_b = sb2("wout", [128, 8, D], BF16)
                wg_b = sb2("wg", [128, 8, D], BF16)
                wp_b = sb2("wp", [128, 2, D], BF16)
                xch = sb2("xch", [128, 8, TT], F32)
                z = sb2("z", [128, 8, TT], F32)
                hf = sb2("hf", [128, 8, TT], F32)
                hb = sb2("hb", [128, 8, TT], BF16)
                st = sb2("st", [128, 4, TT], F32)
                pb = sb2("pb", [128, 2, TT], BF16)
                hid = sb2("hid", [128, 32, TT], BF16)
                rl = [sb2("rl%d" % i, [128, TT], F32) for i in range(2)]
                NW1, NW2 = 6, 2
                w1buf = [sb2("w1b%d" % i, [128, 8, 128], BF16) for i in range(NW1)]
                w2buf = [sb2("w2b%d" % i, [128, 32, 128], BF16) for i in range(NW2)]
                wname = "w_out0" if layer == 0 else "w_out1"
                sc.dma("sp", wout_b[:], wb[wname].rearrange("(p k) c -> p k c", k=8), reads=wbk(wname), writes=["wout"])
                sc.dma("sp", wg_b[:], wb["wg"][layer * 1024:(layer + 1) * 1024, :].rearrange("(p k) c -> p k c", k=8),
                       reads=wbk("wg", layer * 1024, (layer + 1) * 1024), writes=["wg"])
                sc.dma("sp", wp_b[:], wb["wp"][layer * 256:(layer + 1) * 256, :].rearrange("(p k) c -> p k c", k=2),
                       reads=wbk("wp", layer * 256, (layer + 1) * 256), writes=["wp"])
                bank = [0]

                def nb():
                    b = bank[0]
                    bank[0] = (b + 1) % 5
                    return b

                def load_x(cc):
                    sc.dma("pool", xch[:], x_src[s, :, cc * TT:(cc + 1) * TT].rearrange("(k p) t -> p k t", p=128),
                           reads=[("xsrc", layer, s)], writes=["xch"])

                def load_p(cc):
                    sc.dma("pool", pb[:], pT[layer, s, :, cc * TT:(cc + 1) * TT].rearrange("(k p) t -> p k t", p=128), writes=["pb"])

                load_x(0)
                load_p(0)
                for c in range(NCH):
                    t0 = c * TT
                    tasks = [("w1", j) for j in range(32)] + [("w2", m) for m in range(8)]
                    issued = [0]
                    slot_of = {}

                    def issue_upto(n):
                        while issued[0] < min(n, len(tasks)):
                            kind, idx = tasks[issued[0]]
                            if kind == "w1":
                                slot = idx % NW1
                                r0 = (layer * 32 + idx) * 128
                                sc.dma("sp", w1buf[slot][:], wb["w1"][r0:r0 + 128, :].rearrange("p (k c) -> p k c", k=8),
                                       reads=wbk("w1", r0, r0 + 128), writes=[("w1buf", slot)])
                            else:
                                slot = idx % NW2
                                r0 = (layer * 8 + idx) * 256
                                sc.dma("sp", w2buf[slot][:],
                                       wb["w2"][r0:r0 + 256, :].rearrange("(p h) (j c) -> p (h j) c", h=2, c=128),
                                       reads=wbk("w2", r0, r0 + 256), writes=[("w2buf", slot)])
                            issued[0] += 1

                    issue_upto(NW1)
                    for m in range(8):
                        b = nb()
                        for k in range(8):
                            sc.op("pe", lambda e, m=m, k=k, b=b: e.matmul(psf[b][:], lhsT=wout_b[:, k, m * 128:(m + 1) * 128],
                                                                           rhs=mixedT[:, k, t0:t0 + TT], start=(k == 0), stop=(k == 7)),
                                  reads=["wout", ("mixed", k, c)], writes=[("ps", b)])
                        sc.op("dve", lambda e, m=m, b=b: e.scalar_tensor_tensor(out=z[:, m, :], in0=xch[:, m, :], scalar=DN_ALPHA,
                                                                               in1=psf[b][:], op0=ALU.mult, op1=ALU.add),
                              reads=["xch", ("ps", b)], writes=[("acc", m)])
                    if c + 1 < NCH:
                        load_x(c + 1)
                    ln_feature_major(z, "acc", layer, 1, hf, "hf", hb, "hb", hid, st, "st")
                    for m in range(8):
                        b = nb()
                        for k in range(8):
                            sc.op("pe", lambda e, m=m, k=k, b=b: e.matmul(psf[b][:], lhsT=wg_b[:, k, m * 128:(m + 1) * 128],
                                                                           rhs=hb[:, k, :], start=(k == 0), stop=(k == 7)),
                                  reads=["wg", ("hb", k)], writes=[("ps", b)])
                        r = rl[m % 2]
                        sc.op("act", lambda e, b=b, r=r: e.activation(out=r[:], in_=psf[b][:], func=AF.Sigmoid),
                              reads=[("ps", b)], writes=[("rl", m % 2)])
                        b2 = nb()
                        for k in range(2):
                            sc.op("pe", lambda e, m=m, k=k, b2=b2: e.matmul(psf[b2][:], lhsT=wp_b[:, k, m * 128:(m + 1) * 128],
                                                                             rhs=pb[:, k, :], start=(k == 0), stop=(k == 1)),
                                  reads=["wp", "pb"], writes=[("ps", b2)])
                        sc.op("dve", lambda e, m=m, b2=b2, r=r: e.tensor_tensor(out=z[:, m, :], in0=r[:], in1=psf[b2][:], op=ALU.mult),
                              reads=[("rl", m % 2), ("ps", b2)], writes=[("acc", m)])
                        sc.op("dve", lambda e, m=m: e.scalar_tensor_tensor(out=z[:, m, :], in0=hf[:, m, :], scalar=DN_ALPHA,
                                                                          in1=z[:, m, :], op0=ALU.mult, op1=ALU.add),
                              reads=[("hf", m), ("acc", m)], writes=[("acc", m)])
                    if c + 1 < NCH:
                        load_p(c + 1)
                    for j in range(32):
                        issue_upto(min(j + NW1, 32 + NW2))
                        slot = j % NW1
                        b = nb()
                        for k in range(8):
                            sc.op("pe", lambda e, k=k, b=b, slot=slot: e.matmul(psf[b][:], lhsT=w1buf[slot][:, k, :], rhs=hb[:, k, :],
                                                                                 start=(k == 0), stop=(k == 7)),
                                  reads=[("w1buf", slot), ("hb", k)], writes=[("ps", b)])
                        r = rl[j % 2]
                        sc.op("act", lambda e, b=b, r=r: e.activation(out=r[:], in_=psf[b][:], func=AF.Relu),
                              reads=[("ps", b)], writes=[("rl", j % 2)])
                        eng = "dve" if j % 2 == 0 else "pool"
                        sc.op(eng, lambda e, j=j, r=r: e.tensor_tensor(out=hid[:, j, :], in0=r[:], in1=r[:], op=ALU.mult),
                              reads=[("rl", j % 2)], writes=[("hid", j)])
                    for m in range(8):
                        issue_upto(32 + m + NW2)
                        slot = m % NW2
                        b = nb()
                        for j in range(32):
                            sc.op("pe", lambda e, j=j, b=b, slot=slot: e.matmul(psf[b][:], lhsT=w2buf[slot][:, j, :], rhs=hid[:, j, :],
                                                                                 start=(j == 0), stop=(j == 31)),
                                  reads=[("w2buf", slot), ("hid", j)], writes=[("ps", b)])
                        sc.op("dve", lambda e, m=m, b=b: e.tensor_tensor(out=z[:, m, :], in0=z[:, m, :], in1=psf[b][:], op=ALU.add),
                              reads=[("acc", m), ("ps", b)], writes=[("acc", m)])
                    ln_feature_major(z, "acc", layer, 2, hf, "hf", hb, "hb", hid, st, "st")
                    hfr = [("hf", m) for m in range(8)]
                    hbr = [("hb", m) for m in range(8)]
                    if final:
                        sc.dma("pool", yT[s, :, t0:t0 + TT].rearrange("(k p) t -> p k t", p=128), hf[:], reads=hfr,
                               writes=[("y", s, c)])
                    else:
                        sc.dma("pool", x1T[s, :, t0:t0 + TT].rearrange("(k p) t -> p k t", p=128), hf[:], reads=hfr,
                               writes=[("xsrc", 1, s)])
                        sc.dma("pool", x1b[s, :, t0:t0 + TT].rearrange("(k p) t -> p k t", p=128), hb[:], reads=hbr,
                               writes=[("x1b", s)])
                sc.barrier()

        def layer0_mixer(s):
            with contextlib.ExitStack() as esA:
                eA = esA.enter_context

                def sbA(name, shape, dt):
                    return eA(nc.sbuf_tensor("a%d_" % s + name, list(shape), dt))

                qT = sbA("qT", [128, 8, S], BF16)
                kT = sbA("kT", [128, S], BF16)
                vA = sbA("vA", [128, NT, 65], BF16)
                iqT = sbA("iqT", [64, 4, S], BF16)
                ikT = sbA("ikT", [64, S], BF16)
                iw = sbA("iw", [128, NT, 4], F32)
                sc.dma("pool", kT[64:68, :], c_kaug[:, :], writes=["kT_aug"])
                for h in range(8):
                    sc.dma("pool", qT[64:68, h, :], c_qaug[:, h * S:(h + 1) * S], writes=[("qT_aug", h)])
                sc.op("dve", lambda e: e.memset(vA[:, :, 64:65], 1.0), writes=["vA_ones"])
                with contextlib.ExitStack() as esB:
                    eB = esB.enter_context

                    def sbB(name, shape, dt):
                        return eB(nc.sbuf_tensor("b%d_" % s + name, list(shape), dt))

                    win_b = sbB("win", [128, 8, WIN_COLS], BF16)
                    gates_b = sbB("gates", [128, 8, 128], BF16)
                    xbuf = [sbB("xb%d" % i, [128, 8, TT], BF16) for i in range(2)]
                    xrh = sbB("xrh", [128, 4, TT + 3], F32)
                    hlast = sbB("hlast", [128, 4], F32)
                    Tsets = [{n: sbB("t%d_" % i + n, [128, TT], F32) for n in
                              ["xc", "r", "ig", "a", "a2", "u", "h", "y", "g"]} for i in range(2)]
                    xcbs = [sbB("xcb%d" % i, [128, TT], BF16) for i in range(2)]
                    sc.dma("sp", win_b[:], wb["w_in"].rearrange("(p k) c -> p k c", k=8), reads=wbk("w_in"), writes=["win"])
                    sc.dma("sp", gates_b[:], wb["gates"].rearrange("p (k c) -> p k c", k=8), reads=wbk("gates"), writes=["gates"])
                    sc.op("dve", lambda e: e.memset(xrh[:, :, 0:3], 0.0), writes=[("xrh_halo", b) for b in range(4)])
                    sc.op("dve", lambda e: e.memset(hlast[:], 0.0), writes=[("hlast", b) for b in range(4)])
                    bank = [0]

                    def nb():
                        b = bank[0]
                        bank[0] = (b + 1) % 6
                        return b

                    def proj(xb, c0, M, b):
                        for k in range(8):
                            sc.op("pe", lambda e, k=k: e.matmul(psf[b][0:M, :], lhsT=win_b[:, k, c0:c0 + M], rhs=xb[:, k, :],
                                                                 start=(k == 0), stop=(k == 7)),
                                  reads=["win", ("xb", c % 2)], writes=[("ps", b)])

                    def load_xb(cc):
                        sc.dma("pool", xbuf[cc % 2][:], xT[s, :, cc * TT:(cc + 1) * TT].rearrange("(k p) t -> p k t", p=128),
                               writes=[("xb", cc % 2)])

                    load_xb(0)
                    for c in range(NCH):
                        t0 = c * TT
                        xb = xbuf[c % 2]
                        if c + 1 < NCH:
                            load_xb(c + 1)
                        conv_pump(5)
                        for h in range(8):
                            b = nb()
                            proj(xb, 1024 + 64 * h, 64, b)
                            sc.op("act", lambda e, h=h, b=b: e.activation(out=qT[0:64, h, t0:t0 + TT], in_=psf[b][0:64, :], func=AF.Copy),
                                  reads=[("ps", b)], writes=[("qT", h, c)])
                        b = nb()
                        proj(xb, 1536, 64, b)
                        sc.op("dve", lambda e, b=b: e.tensor_copy(out=kT[0:64, t0:t0 + TT], in_=psf[b][0:64, :]),
                              reads=[("ps", b)], writes=[("kT", c)])
                        for h in range(4):
                            b = nb()
                            proj(xb, 1600 + 64 * h, 64, b)
                            sc.op("act", lambda e, h=h, b=b: e.activation(out=iqT[:, h, t0:t0 + TT], in_=psf[b][0:64, :], func=AF.Copy),
                                  reads=[("ps", b)], writes=[("iqT", h, c)])
                        b = nb()
                        proj(xb, 1856, 64, b)
                        sc.op("dve", lambda e, b=b: e.tensor_copy(out=ikT[:, t0:t0 + TT], in_=psf[b][0:64, :]),
                              reads=[("ps", b)], writes=[("ikT", c)])
                        for tt in range(4 if dbg_stage != "A1" else 0):
                            tile_i = c * 4 + tt
                            b = nb()
                            for k in range(8):
                                sc.op("pe", lambda e, k=k, tt=tt, b=b: e.matmul(psf[b][:, 0:68], lhsT=xb[:, k, tt * 128:(tt + 1) * 128],
                                                                                 rhs=win_b[:, k, WIN_TM:WIN_TM + 68],
                                                                                 start=(k == 0), stop=(k == 7)),
                                      reads=["win", ("xb", c % 2)], writes=[("ps", b)])
                            sc.op("act", lambda e, b=b, tile_i=tile_i: e.activation(out=vA[:, tile_i, 0:64], in_=psf[b][:, 0:64], func=AF.Copy),
                                  reads=[("ps", b), "vA_ones"], writes=[("vA", tile_i)])
                            sc.op("dve", lambda e, b=b, tile_i=tile_i: e.tensor_scalar(out=iw[:, tile_i, :], in0=psf[b][:, 64:68],
                                                                                     scalar1=1.0 / 16.0, scalar2=None, op0=ALU.mult),
                                  reads=[("ps", b)], writes=[("iw", tile_i)])
                            if DBG_BARRIER:
                                sc.barrier()
                        def rg_block(blk):
                            T = Tsets[blk % 2]
                            xcb = xcbs[blk % 2]
                            tb = blk % 2
                            b = nb()
                            yield
                            proj(xb, 128 * blk, 128, b)
                            if c > 0:
                                yield
                                sc.op("dve", lambda e, blk=blk: e.tensor_copy(out=xrh[:, blk, 0:3], in_=xrh[:, blk, TT:TT + 3]),
                                      reads=[("xrh", blk)], writes=[("xrh_halo", blk)])
                            yield
                            sc.op("act", lambda e, blk=blk, b=b: e.activation(out=xrh[:, blk, 3:TT + 3], in_=psf[b][:], func=AF.Copy),
                                  reads=[("ps", b), ("xrh_halo", blk)], writes=[("xrh", blk)])
                            by = nb()
                            yield
                            proj(xb, 512 + 128 * blk, 128, by)
                            yield
                            sc.op("act", lambda e, by=by: e.activation(out=T["y"][:], in_=psf[by][:], func=AF.Copy),
                                  reads=[("ps", by)], writes=[("t_y", tb)])
                            cw = SP_CONVW + blk * 4
                            yield
                            sc.op("dve", lambda e, blk=blk, cw=cw: e.tensor_scalar(out=T["xc"][:], in0=xrh[:, blk, 3:TT + 3],
                                                                                 scalar1=small[:, cw + 3:cw + 4],
                                                                                 scalar2=small[:, SP_CONVB + blk:SP_CONVB + blk + 1],
                                                                                 op0=ALU.mult, op1=ALU.add),
                                  reads=[("xrh", blk), ("xrh_halo", blk), "small"], writes=[("t_xc", tb)])
                            for tap in (2, 1, 0):
                                yield
                                sc.op("dve", lambda e, blk=blk, cw=cw, tap=tap: e.scalar_tensor_tensor(
                                    out=T["xc"][:], in0=xrh[:, blk, tap:tap + TT], scalar=small[:, cw + tap:cw + tap + 1],
                                    in1=T["xc"][:], op0=ALU.mult, op1=ALU.add),
                                    reads=[("xrh", blk), ("xrh_halo", blk), ("t_xc", tb), "small"], writes=[("t_xc", tb)])
                            yield
                            sc.op("act", lambda e: e.activation(out=xcb[:], in_=T["xc"][:], func=AF.Copy), reads=[("t_xc", tb)], writes=[("xcb", tb)])
                            ba, bx = nb(), nb()
                            yield
                            sc.op("pe", lambda e, blk=blk, ba=ba: e.matmul(psf[ba][:], lhsT=gates_b[:, blk, :], rhs=xcb[:], start=True, stop=True),
                                  reads=["gates", ("xcb", tb)], writes=[("ps", ba)])
                            yield
                            sc.op("pe", lambda e, blk=blk, bx=bx: e.matmul(psf[bx][:], lhsT=gates_b[:, 4 + blk, :], rhs=xcb[:], start=True, stop=True),
                                  reads=["gates", ("xcb", tb)], writes=[("ps", bx)])
                            yield
                            sc.op("act", lambda e, blk=blk, ba=ba: e.activation(out=T["r"][:], in_=psf[ba][:], func=AF.Sigmoid,
                                                                                bias=small[:, SP_GAB + blk:SP_GAB + blk + 1]),
                                  reads=[("ps", ba), "small"], writes=[("t_r", tb)])
                            yield
                            sc.op("act", lambda e, blk=blk, bx=bx: e.activation(out=T["ig"][:], in_=psf[bx][:], func=AF.Sigmoid,
                                                                                bias=small[:, SP_GXB + blk:SP_GXB + blk + 1]),
                                  reads=[("ps", bx), "small"], writes=[("t_ig", tb)])
                            yield
                            sc.op("pool", lambda e: e.tensor_tensor(out=T["g"][:], in0=T["y"][:], in1=T["y"][:], op=ALU.mult),
                                  reads=[("t_y", tb)], writes=[("t_g", tb)])
                            yield
                            sc.op("dve", lambda e: e.tensor_scalar(out=T["g"][:], in0=T["g"][:], scalar1=0.044715, scalar2=1.0,
                                                                   op0=ALU.mult, op1=ALU.add), reads=[("t_g", tb)], writes=[("t_g", tb)])
                            yield
                            sc.op("pool", lambda e: e.tensor_tensor(out=T["g"][:], in0=T["g"][:], in1=T["y"][:], op=ALU.mult),
                                  reads=[("t_g", tb), ("t_y", tb)], writes=[("t_g", tb)])
                            yield
                            sc.op("act", lambda e: e.activation(out=T["g"][:], in_=T["g"][:], func=AF.Sigmoid, scale=1.5957691216057308),
                                  reads=[("t_g", tb)], writes=[("t_g", tb)])
                            yield
                            sc.op("pool", lambda e: e.tensor_tensor(out=T["g"][:], in0=T["g"][:], in1=T["y"][:], op=ALU.mult),
                                  reads=[("t_g", tb), ("t_y", tb)], writes=[("t_g", tb)])
                            yield
                            sc.op("act", lambda e, blk=blk: e.activation(out=T["a"][:], in_=T["r"][:], func=AF.Exp, scale=negc[:, blk:blk + 1]),
                                  reads=[("t_r", tb), "negc"], writes=[("t_a", tb)])
                            yield
                            sc.op("pool", lambda e: e.tensor_tensor(out=T["a2"][:], in0=T["a"][:], in1=T["a"][:], op=ALU.mult),
                                  reads=[("t_a", tb)], writes=[("t_a2", tb)])
                            yield
                            sc.op("act", lambda e: e.activation(out=T["a2"][:], in_=T["a2"][:], func=AF.Sqrt, scale=-1.0, bias=1.0),
                                  reads=[("t_a2", tb)], writes=[("t_a2", tb)])
                            yield
                            sc.op("dve", lambda e: e.tensor_tensor(out=T["u"][:], in0=T["ig"][:], in1=T["xc"][:], op=ALU.mult),
                                  reads=[("t_ig", tb), ("t_xc", tb)], writes=[("t_u", tb)])
                            yield
                            sc.op("dve", lambda e: e.tensor_tensor(out=T["u"][:], in0=T["u"][:], in1=T["a2"][:], op=ALU.mult),
                                  reads=[("t_u", tb), ("t_a2", tb)], writes=[("t_u", tb)])
                            yield
                            sc.op("dve", lambda e, blk=blk: e.tensor_tensor_scan(out=T["h"][:], data0=T["a"][:], data1=T["u"][:],
                                                                               initial=hlast[:, blk:blk + 1], op0=ALU.mult, op1=ALU.add),
                                  reads=[("t_a", tb), ("t_u", tb), ("hlast", blk)], writes=[("t_h", tb)])
                            yield
                            sc.op("dve", lambda e, blk=blk: e.tensor_copy(out=hlast[:, blk:blk + 1], in_=T["h"][:, TT - 1:TT]),
                                  reads=[("t_h", tb)], writes=[("hlast", blk)])
                            yield
                            sc.op("dve", lambda e, blk=blk: e.tensor_tensor(out=mixedT[:, blk, t0:t0 + TT], in0=T["h"][:], in1=T["g"][:], op=ALU.mult),
                                  reads=[("t_h", tb), ("t_g", tb)], writes=[("mixed", blk, c)])
                        if dbg_stage not in ("A1", "A2"):
                            for pair in ((0, 1), (2, 3)):
                                gens = [rg_block(bk) for bk in pair]
                                while gens:
                                    for g_ in list(gens):
                                        try:
                                            next(g_)
                                        except StopIteration:
                                            gens.remove(g_)
                    if dbg_stage in ("A", "A1", "A2") and s == 0:
                        sc.barrier()
                        dbg("mixed_rec", mixedT[:, 0:4, :], [128, 4, S], "x", BF16)
                        dbg("qT", qT[0:68, :, :], [68, 8, S], "x", BF16)
                        dbg("kT", kT[0:68, :], [68, S], "x", BF16)
                        dbg("vA", vA[:], [128, NT, 65], "x", BF16)
                        dbg("iqT", iqT[:], [64, 4, S], "x", BF16)
                        dbg("ikT", ikT[:], [64, S], "x", BF16)
                        dbg("iw", iw[:], [128, NT, 4], "x", F32)
                    sc.barrier()
                with contextlib.ExitStack() as esB:
                    eB = esB.enter_context

                    def sbB(name, shape, dt):
                        return eB(nc.sbuf_tensor("d%d_" % s + name, list(shape), dt))

                    isc = [sbB("isc%d" % i, [128, S], F32) for i in range(2)]
                    work = sbB("work", [128, S], F32)
                    eqm = [sbB("eqm%d" % i, [128, S], BF16) for i in range(2)]
                    rl = [sbB("rl%d" % i, [128, TT], F32) for i in range(4)]
                    m8 = sbB("m8", [128, 8], F32)
                    P = [sbB("P%d" % i, [128, 512], BF16) for i in range(5)]
                    rc = sbB("rc", [128, 8], F32)
                    o_b = sbB("o_b", [128, 512], BF16)
                    def idx_topk(i):
                        L = 128 * (i + 1)
                        q0 = i * 128
                        I = isc[i % 2]
                        E = eqm[i % 2]
                        ik_ = ("isc", i % 2)
                        ek_ = ("eqm", i % 2)
                        nkb = (L + 511) // 512
                        for kb in range(nkb):
                            w = min(512, L - kb * 512)
                            k0 = kb * 512
                            for hI in range(4):
                                sc.op("pe", lambda e: e.matmul(psf[hI][:, 0:w], lhsT=iqT[:, hI, q0:q0 + 128],
                                                               rhs=ikT[:, k0:k0 + w], start=True, stop=True),
                                      reads=["iqT", "ikT"], writes=[("ps", hI)])
                                sc.op("act", lambda e: e.activation(out=rl[hI][:, 0:w], in_=psf[hI][:, 0:w], func=AF.Relu),
                                      reads=[("ps", hI)], writes=[("rl", hI)])
                            sc.op("dve", lambda e: e.tensor_scalar(out=I[:, k0:k0 + w], in0=rl[0][:, 0:w], scalar1=iw[:, i, 0:1],
                                                                   scalar2=None, op0=ALU.mult),
                                  reads=[("rl", 0), "iw"], writes=[ik_])
                            for hI in range(1, 4):
                                sc.op("dve", lambda e: e.scalar_tensor_tensor(
                                    out=I[:, k0:k0 + w], in0=rl[hI][:, 0:w], scalar=iw[:, i, hI:hI + 1], in1=I[:, k0:k0 + w],
                                    op0=ALU.mult, op1=ALU.add), reads=[("rl", hI), "iw", ik_], writes=[ik_])
                        sc.op("dve", lambda e: e.tensor_tensor(out=I[:, q0:q0 + 128], in0=I[:, q0:q0 + 128], in1=causal_f[:], op=ALU.add),
                              reads=[ik_, "causal_f"], writes=[ik_])
                        if dbg_stage == "B1":
                            if i in (1, 5):
                                dbg("isc%d" % i, I[:, 0:L], [128, L], ik_, F32)
                            return
                        if i < 2:
                            sc.op("dve", lambda e: e.tensor_scalar(out=E[:, 0:L], in0=I[:, 0:L], scalar1=-1.0e29, scalar2=None, op0=ALU.is_lt),
                                  reads=[ik_], writes=[ek_])
                        else:
                            for r in range(32):
                                src_ = I if r == 0 else work
                                sk = ik_ if r == 0 else "work"
                                sc.op("dve", lambda e: e.max(out=m8[:], in_=src_[:, 0:L]), reads=[sk], writes=["m8"])
                                sc.op("dve", lambda e: e.match_replace(out=work[:, 0:L], in_to_replace=m8[:], in_values=src_[:, 0:L],
                                                                       imm_value=NEG_REPL), reads=[sk, "m8"], writes=["work"])
                            sc.op("dve", lambda e: e.tensor_tensor(out=E[:, 0:L], in0=work[:, 0:L], in1=I[:, 0:L], op=ALU.is_equal),
                                  reads=["work", ik_], writes=[ek_])
                        if dbg_stage in ("B2",) and s == 0 and i in (1, 5):
                            dbg("eqm%d" % i, E[:, 0:L], [128, L], ek_, BF16)
                            dbg("isc%d" % i, I[:, 0:L], [128, L], ik_, F32)

                    def attention(i):
                        q0 = i * 128
                        E = eqm[i % 2]
                        ek_ = ("eqm", i % 2)
                        ng = (i + 4) // 4
                        units = [(h, g) for h in range(8) for g in range(ng)]

                        def qk(u):
                            h, g = units[u]
                            nj = min(4, i + 1 - 4 * g)
                            sbank = u % 4
                            for jj in range(nj):
                                j = 4 * g + jj
                                sc.op("pe", lambda e: e.matmul(psf[sbank][:, jj * 128:(jj + 1) * 128], lhsT=kT[0:68, j * 128:(j + 1) * 128],
                                                               rhs=qT[0:68, h, q0:q0 + 128], start=True, stop=False),
                                      reads=["qT", "kT"], writes=[("ps", sbank)])
                                sc.op("pe", lambda e: e.matmul(psf[sbank][:, jj * 128:(jj + 1) * 128], lhsT=E[:, j * 128:(j + 1) * 128],
                                                               rhs=identneg_b[:], start=False, stop=True),
                                      reads=[ek_, "identneg_b"], writes=[("ps", sbank)])

                        def tail_dve(hh):
                            ob = psf[4 + hh]
                            sc.op("dve", lambda e: e.reciprocal(out=rc[:, hh * 4:(hh + 1) * 4],
                                                                in_=ob[:, 0:260].rearrange("p (h c) -> p h c", c=65)[:, :, 64]),
                                  reads=[("ps", 4 + hh)], writes=[("rc", hh)])
                            for h4 in range(4):
                                h = hh * 4 + h4
                                sc.op("dve", lambda e: e.tensor_scalar(out=o_b[:, h * 64:(h + 1) * 64], in0=ob[:, h4 * 65:h4 * 65 + 64],
                                                                       scalar1=rc[:, h:h + 1], scalar2=None, op0=ALU.mult),
                                      reads=[("ps", 4 + hh), ("rc", hh)], writes=[("o_b", hh)])

                        def tail_pe(hh):
                            for kk in (2 * hh, 2 * hh + 1):
                                sc.op("pe", lambda e: e.transpose(out=psb[:, kk * 128:(kk + 1) * 128], in_=o_b[:, kk * 128:(kk + 1) * 128],
                                                                  identity=ident_b[:]),
                                      reads=[("o_b", hh), "ident_b"], writes=[("ps", "b")])
                            sc.op("act", lambda e: e.activation(out=mixedT[:, 4 + 2 * hh:6 + 2 * hh, q0:q0 + 128],
                                                                in_=psb[:, hh * 256:(hh + 1) * 256].rearrange("p (k t) -> p k t", k=2), func=AF.Copy),
                                  reads=[("ps", "b")], writes=[("mixed_att", i, hh)])

                        deferred = []
                        LOOK = 3
                        for u0 in range(min(LOOK, len(units))):
                            qk(u0)
                        for u in range(len(units)):
                            h, g = units[u]
                            nj = min(4, i + 1 - 4 * g)
                            sbank = u % 4
                            Pt = P[u % 5]
                            pk = ("P", u % 5)
                            if u + LOOK < len(units):
                                qk(u + LOOK)
                            sc.op("act", lambda e: e.activation(out=Pt[:, 0:nj * 128], in_=psf[sbank][:, 0:nj * 128], func=AF.Exp, scale=0.125),
                                  reads=[("ps", sbank)], writes=[pk])
                            ob = psf[4 + h // 4]
                            oc = (h % 4) * 65
                            for jj in range(nj):
                                j = 4 * g + jj
                                sc.op("pe", lambda e: e.matmul(ob[:, oc:oc + 65], lhsT=Pt[:, jj * 128:(jj + 1) * 128], rhs=vA[:, j, :],
                                                               start=(g == 0 and jj == 0), stop=(g == ng - 1 and jj == nj - 1)),
                                      reads=[pk, "vA"], writes=[("ps", 4 + h // 4)])
                            for d in deferred:
                                d[0] -= 1
                            while deferred and deferred[0][0] <= 0:
                                tail_pe(deferred.pop(0)[1])
                            if g == ng - 1 and h % 4 == 3:
                                tail_dve(h // 4)
                                deferred.append([4, h // 4])
                        for d in deferred:
                            tail_pe(d[1])

                    idx_topk(0)
                    for i in range(NT):
                        if i + 1 < NT:
                            idx_topk(i + 1)
                        conv_pump(2)
                        if dbg_stage in ("B1", "B2"):
                            continue
                        attention(i)
                    if dbg_stage in ("A", "B") and s == 0:
                        sc.barrier()
                        dbg("mixed_att", mixedT[:, 4:8, :], [128, 4, S], "x", BF16)
                    sc.barrier()

        def layer1_mixer(s):
            with contextlib.ExitStack() as esA:
                eA = esA.enter_context

                def sbA(name, shape, dt):
                    return eA(nc.sbuf_tensor("m%d_" % s + name, list(shape), dt))

                cqn = sbA("cqn", [128, 4, S], BF16)
                ckvn = sbA("ckvn", [128, 2, S], BF16)
                krope = sbA("krope", [128, S], BF16)
                vM = sbA("vM", [128, NT, 16, 65], BF16)
                cosF = sbA("cossin", [128, S], F32)
                sinS = cosF
                wuq_b = sbA("wuq", [128, 4, 2048], BF16)
                wukv_b = sbA("wukv", [128, 2, 2048], BF16)
                sc.dma("sp", cosF[64:96, :], c_cos[:, :], writes=["cosF"])
                sc.dma("sp", sinS[96:128, :], c_sin[:, :], writes=["sinS"])
                sc.dma("sp", wuq_b[:], wb["w_uq"].rearrange("(p k) c -> p k c", k=4), reads=wbk("w_uq"), writes=["wuq"])
                sc.dma("sp", wukv_b[:], wb["w_ukv"].rearrange("(p k) c -> p k c", k=2), reads=wbk("w_ukv"), writes=["wukv"])
                sc.op("dve", lambda e: e.memset(vM[:, :, :, 64:65], 1.0), writes=["vM_ones"])
                bank = [0]

                def nb():
                    b = bank[0]
                    bank[0] = (b + 1) % 4
                    return b

                with contextlib.ExitStack() as esB:
                    eB = esB.enter_context

                    def sbB(name, shape, dt):
                        return eB(nc.sbuf_tensor("n%d_" % s + name, list(shape), dt))

                    wdn_b = sbB("wdn", [128, 8, 896], BF16)
                    xbuf = [sbB("xb%d" % i, [128, 8, TT], BF16) for i in range(2)]
                    cf = sbB("cf", [128, 6, TT], F32)
                    sq = sbB("sq", [128, 6, TT], F32)
                    rs = sbB("rs", [128, 2, TT], F32)
                    rt = sbB("rt", [128, 2, TT], F32)
                    sc.dma("sp", wdn_b[:], wb["w_down"].rearrange("(p k) c -> p k c", k=8), reads=wbk("w_down"), writes=["wdn"])
                    def load_xb(cc):
                        sc.dma("pool", xbuf[cc % 2][:], x1b[s, :, cc * TT:(cc + 1) * TT].rearrange("(k p) t -> p k t", p=128),
                               reads=[("x1b", s)], writes=[("xb", cc % 2)])

                    load_xb(0)
                    for c in range(NCH):
                        t0 = c * TT
                        xb = xbuf[c % 2]
                        if c + 1 < NCH:
                            load_xb(c + 1)
                        for blk in range(7):
                            b = nb()
                            for k in range(8):
                                sc.op("pe", lambda e, k=k, blk=blk, b=b: e.matmul(psf[b][:], lhsT=wdn_b[:, k, blk * 128:(blk + 1) * 128],
                                                                                   rhs=xb[:, k, :], start=(k == 0), stop=(k == 7)),
                                      reads=["wdn", ("xb", c % 2)], writes=[("ps", b)])
                            if blk < 6:
                                sc.op("act", lambda e, blk=blk, b=b: e.activation(out=cf[:, blk, :], in_=psf[b][:], func=AF.Copy),
                                      reads=[("ps", b)], writes=[("cf", blk)])
                                sc.op("act", lambda e, blk=blk, b=b: e.activation(out=sq[:, blk, :], in_=psf[b][:], func=AF.Square),
                                      reads=[("ps", b)], writes=[("sq", blk)])
                            else:
                                sc.op("dve", lambda e, b=b: e.tensor_tensor(out=rt[64:96, 0, :], in0=psf[b][96:128, :], in1=sinS[96:128, t0:t0 + TT], op=ALU.mult),
                                      reads=[("ps", b), "sinS"], writes=["rt0"])
                                sc.op("dve", lambda e, b=b: e.tensor_tensor(out=rt[64:96, 1, :], in0=psf[b][64:96, :], in1=cosF[64:96, t0:t0 + TT], op=ALU.mult),
                                      reads=[("ps", b), "cosF"], writes=["rt1"])
                                sc.op("dve", lambda e: e.tensor_tensor(out=krope[64:96, t0:t0 + TT], in0=rt[64:96, 0, :], in1=rt[64:96, 1, :], op=ALU.add),
                                      reads=["rt0", "rt1"], writes=[("krope", c)])
                        for grp, (k0, nk, dim, nof) in enumerate([(0, 4, 512, SP_QN), (4, 2, 256, SP_KVN)]):
                            pq = psf[4 + grp]
                            for kk in range(nk):
                                sc.op("pe", lambda e, kk=kk, k0=k0, nk=nk, pq=pq: e.matmul(pq[:], lhsT=ones_f[:], rhs=sq[:, k0 + kk, :],
                                                                                          start=(kk == 0), stop=(kk == nk - 1)),
                                      reads=[("sq", k0 + kk), "ones_f"], writes=[("ps", 4 + grp)])
                            sc.op("act", lambda e, grp=grp, dim=dim, pq=pq: e.activation(out=rs[:, grp, :], in_=pq[:], func=AF.Ln, scale=1.0 / dim,
                                                                                         bias=eps_t[:, 1:2]),
                                  reads=[("ps", 4 + grp), "eps_t"], writes=[("rs", grp)])
                            sc.op("act", lambda e, grp=grp: e.activation(out=rs[:, grp, :], in_=rs[:, grp, :], func=AF.Exp, scale=-0.5),
                                  reads=[("rs", grp)], writes=[("rs", grp)])
                            for kk in range(nk):
                                dst = cqn[:, kk, t0:t0 + TT] if grp == 0 else ckvn[:, kk, t0:t0 + TT]
                                sc.op("dve", lambda e, kk=kk, k0=k0, grp=grp, nof=nof, dst=dst: e.scalar_tensor_tensor(
                                    out=dst, in0=cf[:, k0 + kk, :], scalar=small[:, nof + kk:nof + kk + 1], in1=rs[:, grp, :],
                                    op0=ALU.mult, op1=ALU.mult),
                                    reads=[("cf", k0 + kk), ("rs", grp), "small"], writes=[("cn", grp, kk, c)])
                        for tt in range(4):
                            ti = c * 4 + tt
                            for half in range(2):
                                b = nb()
                                for kk in range(2):
                                    sc.op("pe", lambda e, kk=kk, tt=tt, half=half, b=b: e.matmul(
                                        psf[b][:], lhsT=ckvn[:, kk, t0 + tt * 128:t0 + (tt + 1) * 128],
                                        rhs=wukv_b[:, kk, 1024 + half * 512:1024 + (half + 1) * 512], start=(kk == 0), stop=(kk == 1)),
                                        reads=[("cn", 1, kk, c), "wukv"], writes=[("ps", b)])
                                sc.op("act", lambda e, ti=ti, half=half, b=b: e.activation(
                                    out=vM[:, ti, half * 8:(half + 1) * 8, 0:64], in_=psf[b][:].rearrange("p (h d) -> p h d", d=64), func=AF.Copy),
                                    reads=[("ps", b), "vM_ones"], writes=[("vM", ti, half)])
                    if dbg_stage == "D" and s == 0:
                        sc.barrier()
                        dbg("cqn", cqn[:], [128, 4, S], "x", BF16)
                        dbg("ckvn", ckvn[:], [128, 2, S], "x", BF16)
                        dbg("krope", krope[64:96, :], [32, S], "x", BF16)
                        dbg("vM", vM[:], [128, NT, 16, 65], "x", BF16)
                    sc.barrier()
                with contextlib.ExitStack() as esB:
                    eB = esB.enter_context

                    def sbB(name, shape, dt):
                        return eB(nc.sbuf_tensor("e%d_" % s + name, list(shape), dt))

                    qTb = [sbB("qT%d" % i, [128, 4, S], BF16) for i in range(2)]
                    kTb = [sbB("kT%d" % i, [128, 4, S], BF16) for i in range(2)]
                    rt = sbB("rt", [128, 2, TT], F32)
                    P = [sbB("P%d" % i, [128, 512], BF16) for i in range(5)]
                    rc = sbB("rc", [128, 8], F32)
                    o_b = sbB("o_b", [128, 512], BF16)
                    pcnt = [0]
                    def proj_gen(hg):
                        qT = qTb[hg % 2]
                        kT = kTb[hg % 2]
                        pb_ = hg % 2
                        b = 6
                        for hh in range(4):
                            head = hg * 4 + hh
                            for c in range(NCH):
                                t0 = c * TT
                                for kk in range(4):
                                    sc.op("pe", lambda e: e.matmul(psf[b][:], lhsT=wuq_b[:, kk, head * 128:(head + 1) * 128], rhs=cqn[:, kk, t0:t0 + TT],
                                                                   start=(kk == 0), stop=(kk == 3)), reads=["wuq", "cqn"], writes=[("ps", b)])
                                sc.op("dve", lambda e: e.tensor_copy(out=qT[0:64, hh, t0:t0 + TT], in_=psf[b][0:64, :]),
                                      reads=[("ps", b)], writes=[("qT", pb_, hh)])
                                sc.op("dve", lambda e: e.tensor_tensor(out=rt[64:96, 0, :], in0=psf[b][96:128, :], in1=sinS[96:128, t0:t0 + TT], op=ALU.mult),
                                      reads=[("ps", b), "sinS"], writes=["rt0"])
                                sc.op("dve", lambda e: e.tensor_tensor(out=rt[64:96, 1, :], in0=psf[b][64:96, :], in1=cosF[64:96, t0:t0 + TT], op=ALU.mult),
                                      reads=[("ps", b), "cosF"], writes=["rt1"])
                                sc.op("dve", lambda e: e.tensor_tensor(out=qT[64:96, hh, t0:t0 + TT], in0=rt[64:96, 0, :], in1=rt[64:96, 1, :], op=ALU.add),
                                      reads=["rt0", "rt1"], writes=[("qT", pb_, hh)])
                                yield
                            sc.op("pool", lambda e: e.tensor_copy(out=kT[64:96, hh, :], in_=krope[64:96, :]),
                                  reads=["krope"], writes=[("kT", pb_, hh)])
                        for pr in range(2):
                            h0 = hg * 4 + pr * 2
                            for c in range(NCH):
                                t0 = c * TT
                                for kk in range(2):
                                    sc.op("pe", lambda e: e.matmul(psf[b][:], lhsT=wukv_b[:, kk, h0 * 64:h0 * 64 + 128], rhs=ckvn[:, kk, t0:t0 + TT],
                                                                   start=(kk == 0), stop=(kk == 1)), reads=["wukv", "ckvn"], writes=[("ps", b)])
                                sc.op("dve", lambda e: e.tensor_copy(out=kT[0:64, pr * 2, t0:t0 + TT], in_=psf[b][0:64, :]),
                                      reads=[("ps", b)], writes=[("kT", pb_, pr * 2)])
                                sc.op("dve", lambda e: e.tensor_copy(out=kT[0:64, pr * 2 + 1, t0:t0 + TT], in_=psf[b][64:128, :]),
                                      reads=[("ps", b)], writes=[("kT", pb_, pr * 2 + 1)])
                                yield

                    gen_next = proj_gen(0)
                    for hg in range(4):
                        for _ in gen_next:
                            pass
                        gen_next = proj_gen(hg + 1) if hg + 1 < 4 else iter(())
                        qT = qTb[hg % 2]
                        kT = kTb[hg % 2]
                        pb_ = hg % 2
                        if dbg_stage == "E" and s == 0 and hg == 0:
                            sc.barrier()
                            dbg("qT", qT[0:96, :, :], [96, 4, S], "x", BF16)
                            dbg("kT", kT[0:96, :, :], [96, 4, S], "x", BF16)
                        units = [(i, hh, g) for i in range(NT) for hh in range(4) for g in range((i + 4) // 4)]

                        def qk(u):
                            i, hh, g = units[u]
                            q0 = i * 128
                            nj = min(4, i + 1 - 4 * g)
                            sbank = u % 4
                            for jj in range(nj):
                                j = 4 * g + jj
                                diag = (j == i)
                                sc.op("pe", lambda e: e.matmul(psf[sbank][:, jj * 128:(jj + 1) * 128], lhsT=kT[0:96, hh, j * 128:(j + 1) * 128],
                                                               rhs=qT[0:96, hh, q0:q0 + 128], start=True, stop=not diag),
                                      reads=[("qT", pb_, hh), ("kT", pb_, hh)], writes=[("ps", sbank)])
                                if diag:
                                    sc.op("pe", lambda e: e.matmul(psf[sbank][:, jj * 128:(jj + 1) * 128], lhsT=tri_b[:], rhs=identneg_b[:],
                                                                   start=False, stop=True),
                                          reads=["tri_b", "identneg_b"], writes=[("ps", sbank)])

                        def tail_dve(i):
                            ob = psf[4 + (i % 2)]
                            obk = ("ps", 4 + (i % 2))
                            sc.op("dve", lambda e: e.reciprocal(out=rc[:, (i % 2) * 4:(i % 2) * 4 + 4],
                                                                in_=ob[:, 0:260].rearrange("p (h c) -> p h c", c=65)[:, :, 64]),
                                  reads=[obk], writes=[("rc", i % 2)])
                            for hh in range(4):
                                sc.op("dve", lambda e: e.tensor_scalar(out=o_b[:, (i % 2) * 256 + hh * 64:(i % 2) * 256 + (hh + 1) * 64],
                                                                       in0=ob[:, hh * 65:hh * 65 + 64],
                                                                       scalar1=rc[:, (i % 2) * 4 + hh:(i % 2) * 4 + hh + 1], scalar2=None, op0=ALU.mult),
                                      reads=[obk, ("rc", i % 2)], writes=[("o_b", i % 2)])

                        def tail_pe(i):
                            q0 = i * 128
                            o0 = (i % 2) * 256
                            for kk in range(2):
                                sc.op("pe", lambda e: e.transpose(out=psb[:, o0 + kk * 128:o0 + (kk + 1) * 128],
                                                                  in_=o_b[:, o0 + kk * 128:o0 + (kk + 1) * 128], identity=ident_b[:]),
                                      reads=[("o_b", i % 2), "ident_b"], writes=[("ps", "b")])
                            sc.op("act", lambda e: e.activation(out=mixedT[:, 2 * hg:2 * hg + 2, q0:q0 + 128],
                                                                in_=psb[:, o0:o0 + 256].rearrange("p (k t) -> p k t", k=2), func=AF.Copy),
                                  reads=[("ps", "b")], writes=[("mixed_att", i)])

                        deferred = []
                        LOOK = 3
                        for u0 in range(min(LOOK, len(units))):
                            qk(u0)
                        for u in range(len(units)):
                            i, hh, g = units[u]
                            head = hg * 4 + hh
                            ng = (i + 4) // 4
                            nj = min(4, i + 1 - 4 * g)
                            sbank = u % 4
                            Pt = P[u % 5]
                            pk = ("P", u % 5)
                            if u + LOOK < len(units):
                                qk(u + LOOK)
                            sc.op("act", lambda e: e.activation(out=Pt[:, 0:nj * 128], in_=psf[sbank][:, 0:nj * 128], func=AF.Exp,
                                                                scale=96.0 ** -0.5),
                                  reads=[("ps", sbank)], writes=[pk])
                            ob = psf[4 + (i % 2)]
                            oc = hh * 65
                            for jj in range(nj):
                                j = 4 * g + jj
                                sc.op("pe", lambda e: e.matmul(ob[:, oc:oc + 65], lhsT=Pt[:, jj * 128:(jj + 1) * 128], rhs=vM[:, j, head, :],
                                                               start=(g == 0 and jj == 0), stop=(g == ng - 1 and jj == nj - 1)),
                                      reads=[pk, "vM"], writes=[("ps", 4 + (i % 2))])
                            for d in deferred:
                                d[0] -= 1
                            while deferred and deferred[0][0] <= 0:
                                tail_pe(deferred.pop(0)[1])
                            if g == ng - 1 and hh == 3:
                                tail_dve(i)
                                deferred.append([4, i])
                            if u % 5 == 4:
                                next(gen_next, None)
                        for d in deferred:
                            tail_pe(d[1])
                    if dbg_stage == "E" and s == 0:
                        sc.barrier()
                        dbg("mixed1", mixedT[:], [128, 8, S], "x", BF16)
                    sc.barrier()

        stop = False
        if dbg_stage == "0":
            dbg("negc", negc[:], [128, 8], "negc", F32)
        for s in range(2):
            if dbg_stage == "0":
                break
            layer0_mixer(s)
            if dbg_stage in ("A", "A1", "A2", "B", "B1", "B2"):
                break
            phase_c(0, s, xT, final=False)
            if dbg_stage == "C":
                break
            layer1_mixer(s)
            if dbg_stage in ("D", "E"):
                break
            phase_c(1, s, x1T, final=True)
        if dbg_stage == "C":
            dbg("x1T", x1T[0], [D, S], ("xsrc", 1, 0), F32)
        conv_pump(1000)
        sc.barrier(final=True)
        dbg_out["_ninst"] = (sc.ninst, dict(sc.ccnt))
    return nc, dbg_out


def _host_prep(inputs):
    f = np.float32
    g = {k: np.asarray(v, dtype=f) for k, v in inputs.items()}
    W = {}
    w_in = g["hy_w_in"][0]
    perm = np.concatenate([np.arange(0, 1600), np.arange(1664, 1984), np.arange(1600, 1664), np.arange(1984, 1988)])
    w = w_in[:, perm]
    W["w_in"] = np.ascontiguousarray(w.reshape(8, 128, WIN_COLS).transpose(1, 0, 2).reshape(1024, WIN_COLS))
    gates = np.zeros((128, 8, 128), f)
    for gi, name in enumerate(["hy_ga_w", "hy_gx_w"]):
        gw = g[name][0]
        for blk in range(4):
            for half in range(2):
                n = 2 * blk + half
                gates[half * 64:(half + 1) * 64, gi * 4 + blk, half * 64:(half + 1) * 64] = gw[n]
    W["gates"] = gates.reshape(128, 1024)

    def pk(wm, kch):
        r, c = wm.shape
        return np.ascontiguousarray(wm.reshape(kch, 128, c).transpose(1, 0, 2).reshape(128 * kch, c))

    W["w_out0"] = pk(g["hy_w_out"][0], 8)
    W["w_out1"] = pk(g["mla_w_out"][0], 8)
    w1 = g["mlp_w1"]
    W["w1"] = np.ascontiguousarray(w1.reshape(2, 8, 128, 32, 128).transpose(0, 3, 2, 1, 4).reshape(2 * 32 * 128, 1024))
    w2 = g["mlp_w2"]
    W["w2"] = np.ascontiguousarray(w2.reshape(2, 2, 16, 128, 8, 128).transpose(0, 4, 3, 1, 2, 5).reshape(2 * 8 * 128 * 2, 2048))
    W["wg"] = np.concatenate([pk(g["ple_w_gate"][l], 8) for l in range(2)], axis=0)
    W["wp"] = np.concatenate([pk(g["ple_w_proj"][l], 2) for l in range(2)], axis=0)
    wd = g["mla_w_down"][0]
    sw = (np.arange(32) + 16) % 32
    kr = wd[:, 768:800]
    wdn = np.concatenate([wd[:, 0:768], kr, kr[:, sw], kr, kr[:, sw]], axis=1)
    W["w_down"] = pk(wdn, 8)
    wuq = g["mla_w_uq"][0].reshape(512, 16, 96)
    wuq2 = np.concatenate([wuq[:, :, 0:64], wuq[:, :, 64:96], wuq[:, :, 64:96][:, :, sw]], axis=2).reshape(512, 2048)
    W["w_uq"] = pk(wuq2, 4)
    wukv = g["mla_w_ukv"][0].reshape(256, 16, 128)
    wukv2 = np.concatenate([wukv[:, :, 0:64].reshape(256, 1024), wukv[:, :, 64:128].reshape(256, 1024)], axis=1)
    W["w_ukv"] = pk(wukv2, 2)

    small = np.zeros((128, SP_N), f)

    def col(v, n):
        return v.reshape(n, 128).T

    for l in range(2):
        small[:, SP_LN + l * 32 + 0:SP_LN + l * 32 + 8] = col(g["ln1_g"][l], 8)
        small[:, SP_LN + l * 32 + 8:SP_LN + l * 32 + 16] = col(g["ln1_b"][l], 8)
        small[:, SP_LN + l * 32 + 16:SP_LN + l * 32 + 24] = col(g["ln2_g"][l], 8)
        small[:, SP_LN + l * 32 + 24:SP_LN + l * 32 + 32] = col(g["ln2_b"][l], 8)
    cw = g["hy_conv_w"][0]
    for blk in range(4):
        for tap in range(4):
            small[:, SP_CONVW + blk * 4 + tap] = cw[tap, blk * 128:(blk + 1) * 128]
    small[:, SP_CONVB:SP_CONVB + 4] = col(g["hy_conv_b"][0], 4)
    small[:, SP_GAB:SP_GAB + 4] = col(g["hy_ga_b"][0], 4)
    small[:, SP_GXB:SP_GXB + 4] = col(g["hy_gx_b"][0], 4)
    small[:, SP_LAM:SP_LAM + 4] = col(g["hy_lambda"][0], 4)
    small[:, SP_QN:SP_QN + 4] = col(g["mla_q_norm"][0], 4)
    small[:, SP_KVN:SP_KVN + 2] = col(g["mla_kv_norm"][0], 2)

    C = {}
    C["c_ident"] = np.eye(128, dtype=f)
    tl = np.arange(128)[:, None]
    sl = np.arange(128)[None, :]
    C["c_causal"] = np.where(sl <= tl, 0.0, NEG_FILL).astype(f)
    C["c_tri"] = (sl > tl).astype(f)
    pos = np.arange(S)
    C["c_kaug"] = np.stack([np.ones(S), np.ones(S), pos % 128, (pos // 128) * 128]).astype(f)
    qa = np.zeros((4, 8, S), f)
    for h in range(8):
        sl8 = 8.0 * 2.0 ** (-(h + 1))
        qa[0, h] = -sl8 * 128.0 * (pos // 128)
        qa[1, h] = -sl8 * (pos % 128)
        qa[2, h] = sl8
        qa[3, h] = sl8
    C["c_qaug"] = qa.reshape(4, 8 * S)
    freq = (np.float32(10000.0) ** (-np.arange(0, 32, 2, dtype=f) / np.float32(32))).astype(f)
    ang = pos.astype(f)[:, None] * freq[None, :]
    cos = np.cos(ang).astype(f).T
    sin = np.sin(ang).astype(f).T
    C["c_cos"] = np.concatenate([cos, cos], axis=0)
    C["c_sin"] = np.concatenate([-sin, sin], axis=0)
    return g, W, small, C


_NC_CACHE = {}


def _run(inputs, dbg_stage=None, n_cores=8):
    g, W, small, C = _host_prep(inputs)
    key = dbg_stage
    if key not in _NC_CACHE:
        _NC_CACHE[key] = build(dbg_stage)
    nc, dbg_out = _NC_CACHE[key]
    x = g["x"]
    p = g["p"]
    in_maps = []
    for c in range(n_cores):
        m = {}
        m["xT"] = np.ascontiguousarray(x[2 * c:2 * c + 2].transpose(0, 2, 1))
        m["pT"] = np.ascontiguousarray(p[:, 2 * c:2 * c + 2].transpose(0, 1, 3, 2))
        m["small"] = small
        m.update(C)
        m.update(W)
        in_maps.append(m)
    res = run_bass_kernel_spmd(nc, in_maps, core_ids=list(range(n_cores)))
    return res, dbg_out


def kernel(**inputs):
    res, _ = _run(inputs)
    out = np.empty((16, S, D), np.float32)
    for c in range(8):
        yT = np.asarray(res.results[c]["yT"])
        out[2 * c:2 * c + 2] = yT.transpose(0, 2, 1)
    return out
```

```python
import contextlib
import numpy as np
import concourse.bass as bass
import concourse.mybir as mybir
from concourse.bass_utils import run_bass_kernel_spmd

F32 = mybir.dt.float32
BF16 = mybir.dt.bfloat16
F32R = mybir.dt.float32r
AF = mybir.ActivationFunctionType
ALU = mybir.AluOpType

S = 2048
D = 1024
TT = 512
NCH = S // TT
NT = S // 128
DN_ALPHA = 4 ** 0.25
LN_EPS = 1e-5
RMS_EPS = 1e-6
import os
DBG_BARRIER = bool(int(os.environ.get('DBG_BARRIER', '0')))
NEG_FILL = -1.0e30
NEG_REPL = -2.0e30
MASK_NEG = -30000.0

SP_LN = 0
SP_CONVW = 64
SP_CONVB = 80
SP_GAB = 84
SP_GXB = 88
SP_LAM = 92
SP_QN = 96
SP_KVN = 100
SP_N = 104

WIN_COLS = 1988
WIN_TM = 1920


class Sched:
    def __init__(self, nc, es):
        self.nc = nc
        self.engs = {"pe": nc.tensor, "act": nc.scalar, "dve": nc.vector, "pool": nc.gpsimd, "sp": nc.sync}
        self.comp = ["pe", "act", "dve", "pool"]
        self.csem, self.ccnt, self.last = {}, {}, {}
        for e in self.comp:
            self.csem[e] = es.enter_context(nc.semaphore("c_" + e))
            self.ccnt[e] = 0
            self.last[e] = None
        self.rings = {}
        for rname, n in (("pool", 5), ("sp", 24), ("bg", 3)):
            self.rings[rname] = {"sem": [es.enter_context(nc.semaphore("d%s%d" % (rname, i))) for i in range(n)],
                                 "cnt": [0] * n, "next": 0}
        self.seen = {}
        self.track = {}
        self.ninst = 0

    def _flush(self, e):
        if e in self.last and self.last[e] is not None:
            self.ccnt[e] += 1
            self.last[e].then_inc(self.csem[e], 1)
            self.last[e] = None

    def _wait(self, e, key, val):
        kind, who = key
        if kind == "c":
            if who == e and e == "pe":
                return
            if val > self.ccnt[who]:
                self._flush(who)
            sem = self.csem[who]
        else:
            sem = self.rings[who[0]]["sem"][who[1]]
        if self.seen.get((e, key), 0) >= val:
            return
        self._flush(e)
        self.engs[e].wait_ge(sem, val)
        self.seen[(e, key)] = val
        self.ninst += 1

    def _deps(self, e, reads, writes):
        deps = {}
        for k in reads:
            t = self.track.get(k)
            if t and t[0] is not None:
                kk, v = t[0]
                deps[kk] = max(deps.get(kk, 0), v)
            if t and isinstance(k, tuple) and k[0] == "ps":
                for kk, v in t[1].items():
                    if kk != ("c", e):
                        deps[kk] = max(deps.get(kk, 0), v)
        for k in writes:
            t = self.track.get(k)
            if t:
                if t[0] is not None:
                    kk, v = t[0]
                    deps[kk] = max(deps.get(kk, 0), v)
                for kk, v in t[1].items():
                    deps[kk] = max(deps.get(kk, 0), v)
        for kk, v in deps.items():
            self._wait(e, kk, v)

    def _record(self, ev, reads, writes):
        kk, v = ev
        for k in reads:
            t = self.track.setdefault(k, [None, {}])
            t[1][kk] = max(t[1].get(kk, 0), v)
        for k in writes:
            self.track[k] = [ev, {}]

    def op(self, e, fn, reads=(), writes=()):
        self._deps(e, reads, writes)
        h = fn(self.engs[e])
        self.last[e] = h
        self.ninst += 1
        self._record((("c", e), self.ccnt[e] + 1), reads, writes)
        return h

    def dma(self, q, out, in_, reads=(), writes=(), bg=False):
        self._deps(q, reads, writes)
        rname = "bg" if bg else q
        ring = self.rings[rname]
        idx = ring["next"]
        ring["next"] = (idx + 1) % len(ring["sem"])
        if ring["cnt"][idx] > 0:
            self._wait(q, ("d", (rname, idx)), ring["cnt"][idx])
        h = self.engs[q].dma_start(out=out, in_=in_)
        ring["cnt"][idx] += 16
        h.then_inc(ring["sem"][idx], 16)
        self.ninst += 1
        self._record((("d", (rname, idx)), ring["cnt"][idx]), reads, writes)

    def barrier(self, final=False):
        for e in self.comp:
            self._flush(e)
        for e in ["pe", "act", "dve", "pool", "sp"]:
            for who in self.comp:
                if self.ccnt[who] > 0 and not (who == e and e == "pe"):
                    self._wait(e, ("c", who), self.ccnt[who])
            for rname, ring in self.rings.items():
                if rname == "bg" and not final:
                    continue
                for idx in range(len(ring["sem"])):
                    if ring["cnt"][idx] > 0:
                        self._wait(e, ("d", (rname, idx)), ring["cnt"][idx])
        keep = {k: v for k, v in self.track.items() if isinstance(k, tuple) and k[0] == "wb"}
        self.track.clear()
        if not final:
            self.track.update(keep)


def _w_specs():
    return {
        "w_in": (1024, WIN_COLS),
        "gates": (128, 1024),
        "w_out0": (1024, 1024),
        "w_out1": (1024, 1024),
        "w1": (2 * 32 * 128, 1024),
        "w2": (2 * 8 * 128 * 2, 2048),
        "wg": (2 * 128 * 8, 1024),
        "wp": (2 * 128 * 2, 1024),
        "w_down": (1024, 896),
        "w_uq": (512, 2048),
        "w_ukv": (256, 2048),
    }


def build(dbg_stage=None):
    nc = bass.Bass("TRN2", target_bir_lowering=False)
    dbg_out = {}
    with contextlib.ExitStack() as es:
        sc = Sched(nc, es)
        ent = es.enter_context

        def dram_in(name, shape, dt=F32):
            return nc.dram_tensor(name, list(shape), dt, kind="ExternalInput").ap()

        xT = dram_in("xT", [2, D, S])
        pT = dram_in("pT", [2, 2, 256, S])
        small_d = dram_in("small", [128, SP_N])
        c_ident = dram_in("c_ident", [128, 128])
        c_causal = dram_in("c_causal", [128, 128])
        c_tri = dram_in("c_tri", [128, 128])
        c_kaug = dram_in("c_kaug", [4, S])
        c_qaug = dram_in("c_qaug", [4, 8 * S])
        c_cos = dram_in("c_cos", [32, S])
        c_sin = dram_in("c_sin", [32, S])
        wf, wb = {}, {}
        for name, (r, c) in _w_specs().items():
            wf[name] = dram_in(name, [r, c])
            wb[name] = nc.dram_tensor(name + "_b", [r, c], BF16, kind="Internal").ap()
        x1T = nc.dram_tensor("x1T", [2, D, S], F32, kind="Internal").ap()
        x1b = nc.dram_tensor("x1b", [2, D, S], BF16, kind="Internal").ap()
        yT = nc.dram_tensor("yT", [2, D, S], F32, kind="ExternalOutput").ap()

        def dbg(name, src_ap, shape, key, dt=F32):
            t = nc.dram_tensor("dbg_" + name, list(shape), dt, kind="ExternalOutput").ap()
            dbg_out[name] = (list(shape), dt)
            sc.dma("pool", t, src_ap, reads=[key])

        def sb(name, shape, dt):
            return ent(nc.sbuf_tensor("s_" + name, list(shape), dt))

        psf = [ent(nc.psum_tensor("psf%d" % i, [128, 512], F32)) for i in range(7)]
        psb = ent(nc.psum_tensor("psb", [128, 1024], BF16))
        ident_b = sb("ident_b", [128, 128], BF16)
        identneg_b = sb("identneg_b", [128, 128], BF16)
        causal_f = sb("causal_f", [128, 128], F32)
        tri_b = sb("tri_b", [128, 128], BF16)
        ones_f = sb("ones_f", [128, 128], F32)
        small = sb("small", [128, SP_N], F32)
        negc = sb("negc", [128, 8], F32)
        mixedT = sb("mixedT", [128, 8, S], BF16)

        conv_tasks = []
        wchunks = {}
        for name in ["w_in", "gates", "w_out0", "wg", "wp", "w1", "w2", "w_down", "w_uq", "w_ukv", "w_out1"]:
            r, c = _w_specs()[name]
            step = max(16, (1 << 19) // c // 16 * 16)
            wchunks[name] = []
            for r0 in range(0, r, step):
                r1 = min(r, r0 + step)
                wchunks[name].append((r0, r1))
                conv_tasks.append((name, r0, r1))
        def _prio(t):
            name, r0, r1 = t
            half = _w_specs()[name][0] // 2
            if name in ("w1", "w2", "wg", "wp") and r0 >= half:
                return 2
            if name in ("w_down", "w_uq", "w_ukv", "w_out1"):
                return 1
            return 0
        conv_tasks.sort(key=_prio)
        conv_pos = [0]

        def conv_pump(n):
            while n > 0 and conv_pos[0] < len(conv_tasks):
                name, r0, r1 = conv_tasks[conv_pos[0]]
                conv_pos[0] += 1
                n -= 1
                sc.dma("pool", wb[name][r0:r1, :], wf[name][r0:r1, :], writes=[("wb", name, r0)], bg=True)

        def wbk(name, a=None, b=None):
            r = _w_specs()[name][0]
            a = 0 if a is None else a
            b = r if b is None else b
            need = [(name, r0, r1) for (r0, r1) in wchunks[name] if r0 < b and r1 > a]
            while any(t in conv_tasks[conv_pos[0]:] for t in need):
                conv_pump(1)
            return [("wb", n_, r0) for (n_, r0, r1) in need]

        conv_pump(6)
        sc.dma("pool", ident_b[:], c_ident[:, :], writes=["ident_b"])
        sc.dma("pool", tri_b[:], c_tri[:, :], writes=["tri_b"])
        sc.dma("sp", causal_f[:], c_causal[:, :], writes=["causal_f"])
        sc.dma("sp", small[:], small_d[:, :], writes=["small"])
        sc.op("dve", lambda e: e.memset(ones_f[:], 1.0), writes=["ones_f"])
        sc.op("dve", lambda e: e.tensor_scalar(out=identneg_b[:], in0=ident_b[:], scalar1=MASK_NEG, scalar2=None,
                                               op0=ALU.mult), reads=["ident_b"], writes=["identneg_b"])
        sc.op("act", lambda e: e.activation(out=negc[:, 4:8], in_=small[:, SP_LAM:SP_LAM + 4], func=AF.Exp, scale=-1.0),
              reads=["small"], writes=["negc_t"])
        sc.op("act", lambda e: e.activation(out=negc[:, 4:8], in_=negc[:, 4:8], func=AF.Ln, bias=1.0),
              reads=["negc_t"], writes=["negc_t"])
        sc.op("dve", lambda e: e.tensor_scalar(out=negc[:, 0:4], in0=negc[:, 4:8], scalar1=-8.0, scalar2=None,
                                               op0=ALU.mult), reads=["negc_t"], writes=["negc"])
        sc.barrier()

        def ln_feature_major(z, zkey, layer, which, out_f, out_f_key, out_b, out_b_key, hid, st, stkey):
            gof = SP_LN + layer * 32 + (0 if which == 1 else 16)
            ps_m, ps_q = psf[5], psf[6]
            tmp = hid.bitcast(F32)[:, 16:32, :].rearrange("p (m a) b -> p m (a b)", a=2)
            tk = lambda m: [("hid", 16 + 2 * m), ("hid", 17 + 2 * m)]
            for m in range(8):
                sc.op("act", lambda e: e.activation(out=hid[:, m, :], in_=z[:, m, :], func=AF.Square),
                      reads=[(zkey, m)], writes=[("hid", m)])
                sc.op("dve", lambda e: e.tensor_copy(out=hid[:, 8 + m, :], in_=z[:, m, :]),
                      reads=[(zkey, m)], writes=[("hid", 8 + m)])
            for m in range(8):
                sc.op("pe", lambda e: e.matmul(ps_m[:], lhsT=ones_b[:], rhs=hid[:, 8 + m, :], start=(m == 0), stop=(m == 7)),
                      reads=[("hid", 8 + m), "ones_b"], writes=[("ps", 5)])
            for m in range(8):
                sc.op("pe", lambda e: e.matmul(ps_q[:], lhsT=ones_b[:], rhs=hid[:, m, :], start=(m == 0), stop=(m == 7)),
                      reads=[("hid", m), "ones_b"], writes=[("ps", 6)])
            mean, msq, var, rstd = st[:, 0, :], st[:, 1, :], st[:, 2, :], st[:, 3, :]
            sc.op("act", lambda e: e.activation(out=mean, in_=ps_m[:], func=AF.Copy, scale=1.0 / D),
                  reads=[("ps", 5)], writes=[(stkey, 0)])
            sc.op("act", lambda e: e.activation(out=msq, in_=ps_m[:], func=AF.Square, scale=1.0 / D),
                  reads=[("ps", 5)], writes=[(stkey, 1)])
            sc.op("dve", lambda e: e.scalar_tensor_tensor(out=var, in0=ps_q[:], scalar=1.0 / D, in1=msq,
                                                          op0=ALU.mult, op1=ALU.subtract),
                  reads=[("ps", 6), (stkey, 1)], writes=[(stkey, 2)])
            sc.op("act", lambda e: e.activation(out=var, in_=var, func=AF.Ln, bias=eps_t[:, 0:1]),
                  reads=[(stkey, 2), "eps_t"], writes=[(stkey, 2)])
            sc.op("act", lambda e: e.activation(out=rstd, in_=var, func=AF.Exp, scale=-0.5),
                  reads=[(stkey, 2)], writes=[(stkey, 3)])
            for m in range(8):
                eng = "dve"
                sc.op(eng, lambda e: e.tensor_tensor(out=tmp[:, m, :], in0=z[:, m, :], in1=mean, op=ALU.subtract),
                      reads=[(zkey, m), (stkey, 0)], writes=tk(m))
                sc.op(eng, lambda e: e.tensor_tensor(out=tmp[:, m, :], in0=tmp[:, m, :], in1=rstd, op=ALU.mult),
                      reads=tk(m) + [(stkey, 3)], writes=tk(m))
                sc.op("act", lambda e: e.activation(out=out_b[:, m, :], in_=tmp[:, m, :], func=AF.Identity,
                                                    scale=small[:, gof + m:gof + m + 1], bias=small[:, gof + 8 + m:gof + 9 + m]),
                      reads=tk(m) + ["small"], writes=[(out_b_key, m)])
                sc.op("dve", lambda e: e.tensor_scalar(out=out_f[:, m, :], in0=tmp[:, m, :],
                                                       scalar1=small[:, gof + m:gof + m + 1],
                                                       scalar2=small[:, gof + 8 + m:gof + 9 + m],
                                                       op0=ALU.mult, op1=ALU.add),
                      reads=tk(m) + ["small"], writes=[(out_f_key, m)])

        ones_b = sb("ones_b", [128, 128], BF16)
        sc.op("dve", lambda e: e.memset(ones_b[:], 1.0), writes=["ones_b"])
        eps_t = sb("eps_t", [128, 2], F32)
        sc.op("dve", lambda e: e.memset(eps_t[:, 0:1], LN_EPS), writes=["eps_t0"])
        sc.op("dve", lambda e: e.memset(eps_t[:, 1:2], RMS_EPS), writes=["eps_t1"])
        sc.barrier()

        def phase_c(layer, s, x_src, final):
            with contextlib.ExitStack() as es2:
                e2 = es2.enter_context

                def sb2(name, shape, dt):
                    return e2(nc.sbuf_tensor("c%d%d_" % (layer, s) + name, list(shape), dt))

                wout_b = sb2("wout", [128, 8, D], BF16)
                wg_b = sb2("wg", [128, 8, D], BF16)
                wp_b = sb2("wp", [128, 2, D], BF16)
                xch = sb2("xch", [128, 8, TT], F32)
                z = sb2("z", [128, 8, TT], F32)
                hf = sb2("hf", [128, 8, TT], F32)
                hb = sb2("hb", [128, 8, TT], BF16)
                st = sb2("st", [128, 4, TT], F32)
                pb = sb2("pb", [128, 2, TT], BF16)
                hid = sb2("hid", [128, 32, TT], BF16)
                rl = [sb2("rl%d" % i, [128, TT], F32) for i in range(2)]
                NW1, NW2 = 6, 2
                w1buf = [sb2("w1b%d" % i, [128, 8, 128], BF16) for i in range(NW1)]
                w2buf = [sb2("w2b%d" % i, [128, 32, 128], BF16) for i in range(NW2)]
                wname = "w_out0" if layer == 0 else "w_out1"
                sc.dma("sp", wout_b[:], wb[wname].rearrange("(p k) c -> p k c", k=8), reads=wbk(wname), writes=["wout"])
                sc.dma("sp", wg_b[:], wb["wg"][layer * 1024:(layer + 1) * 1024, :].rearrange("(p k) c -> p k c", k=8),
                       reads=wbk("wg", layer * 1024, (layer + 1) * 1024), writes=["wg"])
                sc.dma("sp", wp_b[:], wb["wp"][layer * 256:(layer + 1) * 256, :].rearrange("(p k) c -> p k c", k=2),
                       reads=wbk("wp", layer * 256, (layer + 1) * 256), writes=["wp"])
                bank = [0]

                def nb():
                    b = bank[0]
                    bank[0] = (b + 1) % 5
                    return b

                def load_x(cc):
                    sc.dma("pool", xch[:], x_src[s, :, cc * TT:(cc + 1) * TT].rearrange("(k p) t -> p k t", p=128),
                           reads=[("xsrc", layer, s)], writes=["xch"])

                def load_p(cc):
                    sc.dma("pool", pb[:], pT[layer, s, :, cc * TT:(cc + 1) * TT].rearrange("(k p) t -> p k t", p=128), writes=["pb"])

                load_x(0)
                load_p(0)
                for c in range(NCH):
                    t0 = c * TT
                    tasks = [("w1", j) for j in range(32)] + [("w2", m) for m in range(8)]
                    issued = [0]
                    slot_of = {}

                    def issue_upto(n):
                        while issued[0] < min(n, len(tasks)):
                            kind, idx = tasks[issued[0]]
                            if kind == "w1":
                                slot = idx % NW1
                                r0 = (layer * 32 + idx) * 128
                                sc.dma("sp", w1buf[slot][:], wb["w1"][r0:r0 + 128, :].rearrange("p (k c) -> p k c", k=8),
                                       reads=wbk("w1", r0, r0 + 128), writes=[("w1buf", slot)])
                            else:
                                slot = idx % NW2
                                r0 = (layer * 8 + idx) * 256
                                sc.dma("sp", w2buf[slot][:],
                                       wb["w2"][r0:r0 + 256, :].rearrange("(p h) (j c) -> p (h j) c", h=2, c=128),
                                       reads=wbk("w2", r0, r0 + 256), writes=[("w2buf", slot)])
                            issued[0] += 1

                    issue_upto(NW1)
                    for m in range(8):
                        b = nb()
                        for k in range(8):
                            sc.op("pe", lambda e, m=m, k=k, b=b: e.matmul(psf[b][:], lhsT=wout_b[:, k, m * 128:(m + 1) * 128],
                                                                           rhs=mixedT[:, k, t0:t0 + TT], start=(k == 0), stop=(k == 7)),
                                  reads=["wout", ("mixed", k, c)], writes=[("ps", b)])
                        sc.op("dve", lambda e, m=m, b=b: e.scalar_tensor_tensor(out=z[:, m, :], in0=xch[:, m, :], scalar=DN_ALPHA,
                                                                               in1=psf[b][:], op0=ALU.mult, op1=ALU.add),
                              reads=["xch", ("ps", b)], writes=[("acc", m)])
                    if c + 1 < NCH:
                        load_x(c + 1)
                    ln_feature_major(z, "acc", layer, 1, hf, "hf", hb, "hb", hid, st, "st")
                    for m in range(8):
                        b = nb()
                        for k in range(8):
                            sc.op("pe", lambda e, m=m, k=k, b=b: e.matmul(psf[b][:], lhsT=wg_b[:, k, m * 128:(m + 1) * 128],
                                                                           rhs=hb[:, k, :], start=(k == 0), stop=(k == 7)),
                                  reads=["wg", ("hb", k)], writes=[("ps", b)])
                        r = rl[m % 2]
                        sc.op("act", lambda e, b=b, r=r: e.activation(out=r[:], in_=psf[b][:], func=AF.Sigmoid),
                              reads=[("ps", b)], writes=[("rl", m % 2)])
                        b2 = nb()
                        for k in range(2):
                            sc.op("pe", lambda e, m=m, k=k, b2=b2: e.matmul(psf[b2][:], lhsT=wp_b[:, k, m * 128:(m + 1) * 128],
                                                                             rhs=pb[:, k, :], start=(k == 0), stop=(k == 1)),
                                  reads=["wp", "pb"], writes=[("ps", b2)])
                        sc.op("dve", lambda e, m=m, b2=b2, r=r: e.tensor_tensor(out=z[:, m, :], in0=r[:], in1=psf[b2][:], op=ALU.mult),
                              reads=[("rl", m % 2), ("ps", b2)], writes=[("acc", m)])
                        sc.op("dve", lambda e, m=m: e.scalar_tensor_tensor(out=z[:, m, :], in0=hf[:, m, :], scalar=DN_ALPHA,
                                                                          in1=z[:, m, :], op0=ALU.mult, op1=ALU.add),
                              reads=[("hf", m), ("acc", m)], writes=[("acc", m)])
                    if c + 1 < NCH:
                        load_p(c + 1)
                    for j in range(32):
                        issue_upto(min(j + NW1, 32 + NW2))
                        slot = j % NW1
                        b = nb()
                        for k in range(8):
                            sc.op("pe", lambda e, k=k, b=b, slot=slot: e.matmul(psf[b][:], lhsT=w1buf[slot][:, k, :], rhs=hb[:, k, :],
                                                                                 start=(k == 0), stop=(k == 7)),
                                  reads=[("w1buf", slot), ("hb", k)], writes=[("ps", b)])
                        r = rl[j % 2]
                        sc.op("act", lambda e, b=b, r=r: e.activation(out=r[:], in_=psf[b][:], func=AF.Relu),
                              reads=[("ps", b)], writes=[("rl", j % 2)])
                        eng = "dve"
                        sc.op(eng, lambda e, j=j, r=r: e.tensor_tensor(out=hid[:, j, :], in0=r[:], in1=r[:], op=ALU.mult),
                              reads=[("rl", j % 2)], writes=[("hid", j)])
                    for m in range(8):
                        issue_upto(32 + m + NW2)
                        slot = m % NW2
                        b = nb()
                        for j in range(32):
                            sc.op("pe", lambda e, j=j, b=b, slot=slot: e.matmul(psf[b][:], lhsT=w2buf[slot][:, j, :], rhs=hid[:, j, :],
                                                                                 start=(j == 0), stop=(j == 31)),
                                  reads=[("w2buf", slot), ("hid", j)], writes=[("ps", b)])
                        sc.op("dve", lambda e, m=m, b=b: e.tensor_tensor(out=z[:, m, :], in0=z[:, m, :], in1=psf[b][:], op=ALU.add),
                              reads=[("acc", m), ("ps", b)], writes=[("acc", m)])
                    ln_feature_major(z, "acc", layer, 2, hf, "hf", hb, "hb", hid, st, "st")
                    hfr = [("hf", m) for m in range(8)]
                    hbr = [("hb", m) for m in range(8)]
                    if final:
                        sc.dma("pool", yT[s, :, t0:t0 + TT].rearrange("(k p) t -> p k t", p=128), hf[:], reads=hfr,
                               writes=[("y", s, c)])
                    else:
                        sc.dma("pool", x1T[s, :, t0:t0 + TT].rearrange("(k p) t -> p k t", p=128), hf[:], reads=hfr,
                               writes=[("xsrc", 1, s)])
                        sc.dma("pool", x1b[s, :, t0:t0 + TT].rearrange("(k p) t -> p k t", p=128), hb[:], reads=hbr,
                               writes=[("x1b", s)])
                sc.barrier()

        def layer0_mixer(s):
            with contextlib.ExitStack() as esA:
                eA = esA.enter_context

                def sbA(name, shape, dt):
                    return eA(nc.sbuf_tensor("a%d_" % s + name, list(shape), dt))

                qT = sbA("qT", [128, 8, S], BF16)
                kT = sbA("kT", [128, S], BF16)
                vA = sbA("vA", [128, NT, 65], BF16)
                iqT = sbA("iqT", [64, 4, S], BF16)
                ikT = sbA("ikT", [64, S], BF16)
                iw = sbA("iw", [128, NT, 4], F32)
                sc.dma("pool", kT[64:68, :], c_kaug[:, :], writes=["kT_aug"])
                for h in range(8):
                    sc.dma("pool", qT[64:68, h, :], c_qaug[:, h * S:(h + 1) * S], writes=[("qT_aug", h)])
                sc.op("dve", lambda e: e.memset(vA[:, :, 64:65], 1.0), writes=["vA_ones"])
                with contextlib.ExitStack() as esB:
                    eB = esB.enter_context

                    def sbB(name, shape, dt):
                        return eB(nc.sbuf_tensor("b%d_" % s + name, list(shape), dt))

                    win_b = sbB("win", [128, 8, WIN_COLS], BF16)
                    gates_b = sbB("gates", [128, 8, 128], BF16)
                    xbuf = [sbB("xb%d" % i, [128, 8, TT], BF16) for i in range(2)]
                    xrh = sbB("xrh", [128, 4, TT + 3], F32)
                    hlast = sbB("hlast", [128, 4], F32)
                    Tsets = [{n: sbB("t%d_" % i + n, [128, TT], F32) for n in
                              ["xc", "r", "ig", "a", "a2", "u", "h", "y", "g"]} for i in range(2)]
                    xcbs = [sbB("xcb%d" % i, [128, TT], BF16) for i in range(2)]
                    sc.dma("sp", win_b[:], wb["w_in"].rearrange("(p k) c -> p k c", k=8), reads=wbk("w_in"), writes=["win"])
                    sc.dma("sp", gates_b[:], wb["gates"].rearrange("p (k c) -> p k c", k=8), reads=wbk("gates"), writes=["gates"])
                    sc.op("dve", lambda e: e.memset(xrh[:, :, 0:3], 0.0), writes=[("xrh_halo", b) for b in range(4)])
                    sc.op("dve", lambda e: e.memset(hlast[:], 0.0), writes=[("hlast", b) for b in range(4)])
                    bank = [0]

                    def nb():
                        b = bank[0]
                        bank[0] = (b + 1) % 6
                        return b

                    def proj(xb, c0, M, b):
                        for k in range(8):
                            sc.op("pe", lambda e, k=k: e.matmul(psf[b][0:M, :], lhsT=win_b[:, k, c0:c0 + M], rhs=xb[:, k, :],
                                                                 start=(k == 0), stop=(k == 7)),
                                  reads=["win", ("xb", c % 2)], writes=[("ps", b)])

                    def load_xb(cc):
                        sc.dma("pool", xbuf[cc % 2][:], xT[s, :, cc * TT:(cc + 1) * TT].rearrange("(k p) t -> p k t", p=128),
                               writes=[("xb", cc % 2)])

                    load_xb(0)
                    for c in range(NCH):
                        t0 = c * TT
                        xb = xbuf[c % 2]
                        if c + 1 < NCH:
                            load_xb(c + 1)
                        conv_pump(5)
                        for h in range(8):
                            b = nb()
                            proj(xb, 1024 + 64 * h, 64, b)
                            sc.op("act", lambda e, h=h, b=b: e.activation(out=qT[0:64, h, t0:t0 + TT], in_=psf[b][0:64, :], func=AF.Copy),
                                  reads=[("ps", b)], writes=[("qT", h, c)])
                        b = nb()
                        proj(xb, 1536, 64, b)
                        sc.op("dve", lambda e, b=b: e.tensor_copy(out=kT[0:64, t0:t0 + TT], in_=psf[b][0:64, :]),
                              reads=[("ps", b)], writes=[("kT", c)])
                        for h in range(4):
                            b = nb()
                            proj(xb, 1600 + 64 * h, 64, b)
                            sc.op("act", lambda e, h=h, b=b: e.activation(out=iqT[:, h, t0:t0 + TT], in_=psf[b][0:64, :], func=AF.Copy),
                                  reads=[("ps", b)], writes=[("iqT", h, c)])
                        b = nb()
                        proj(xb, 1856, 64, b)
                        sc.op("dve", lambda e, b=b: e.tensor_copy(out=ikT[:, t0:t0 + TT], in_=psf[b][0:64, :]),
                              reads=[("ps", b)], writes=[("ikT", c)])
                        for tt in range(4 if dbg_stage != "A1" else 0):
                            tile_i = c * 4 + tt
                            b = nb()
                            for k in range(8):
                                sc.op("pe", lambda e, k=k, tt=tt, b=b: e.matmul(psf[b][:, 0:68], lhsT=xb[:, k, tt * 128:(tt + 1) * 128],
                                                                                 rhs=win_b[:, k, WIN_TM:WIN_TM + 68],
                                                                                 start=(k == 0), stop=(k == 7)),
                                      reads=["win", ("xb", c % 2)], writes=[("ps", b)])
                            sc.op("act", lambda e, b=b, tile_i=tile_i: e.activation(out=vA[:, tile_i, 0:64], in_=psf[b][:, 0:64], func=AF.Copy),
                                  reads=[("ps", b), "vA_ones"], writes=[("vA", tile_i)])
                            sc.op("dve", lambda e, b=b, tile_i=tile_i: e.tensor_scalar(out=iw[:, tile_i, :], in0=psf[b][:, 64:68],
                                                                                     scalar1=1.0 / 16.0, scalar2=None, op0=ALU.mult),
                                  reads=[("ps", b)], writes=[("iw", tile_i)])
                            if DBG_BARRIER:
                                sc.barrier()
                        def rg_block(blk):
                            T = Tsets[blk % 2]
                            xcb = xcbs[blk % 2]
                            tb = blk % 2
                            b = nb()
                            yield
                            proj(xb, 128 * blk, 128, b)
                            if c > 0:
                                yield
                                sc.op("dve", lambda e, blk=blk: e.tensor_copy(out=xrh[:, blk, 0:3], in_=xrh[:, blk, TT:TT + 3]),
                                      reads=[("xrh", blk)], writes=[("xrh_halo", blk)])
                            yield
                            sc.op("act", lambda e, blk=blk, b=b: e.activation(out=xrh[:, blk, 3:TT + 3], in_=psf[b][:], func=AF.Copy),
                                  reads=[("ps", b), ("xrh_halo", blk)], writes=[("xrh", blk)])
                            by = nb()
                            yield
                            proj(xb, 512 + 128 * blk, 128, by)
                            yield
                            sc.op("act", lambda e, by=by: e.activation(out=T["y"][:], in_=psf[by][:], func=AF.Copy),
                                  reads=[("ps", by)], writes=[("t_y", tb)])
                            cw = SP_CONVW + blk * 4
                            yield
                            sc.op("dve", lambda e, blk=blk, cw=cw: e.tensor_scalar(out=T["xc"][:], in0=xrh[:, blk, 3:TT + 3],
                                                                                 scalar1=small[:, cw + 3:cw + 4],
                                                                                 scalar2=small[:, SP_CONVB + blk:SP_CONVB + blk + 1],
                                                                                 op0=ALU.mult, op1=ALU.add),
                                  reads=[("xrh", blk), ("xrh_halo", blk), "small"], writes=[("t_xc", tb)])
                            for tap in (2, 1, 0):
                                yield
                                sc.op("dve", lambda e, blk=blk, cw=cw, tap=tap: e.scalar_tensor_tensor(
                                    out=T["xc"][:], in0=xrh[:, blk, tap:tap + TT], scalar=small[:, cw + tap:cw + tap + 1],
                                    in1=T["xc"][:], op0=ALU.mult, op1=ALU.add),
                                    reads=[("xrh", blk), ("xrh_halo", blk), ("t_xc", tb), "small"], writes=[("t_xc", tb)])
                            yield
                            sc.op("act", lambda e: e.activation(out=xcb[:], in_=T["xc"][:], func=AF.Copy), reads=[("t_xc", tb)], writes=[("xcb", tb)])
                            ba, bx = nb(), nb()
                            yield
                            sc.op("pe", lambda e, blk=blk, ba=ba: e.matmul(psf[ba][:], lhsT=gates_b[:, blk, :], rhs=xcb[:], start=True, stop=True),
                                  reads=["gates", ("xcb", tb)], writes=[("ps", ba)])
                            yield
                            sc.op("pe", lambda e, blk=blk, bx=bx: e.matmul(psf[bx][:], lhsT=gates_b[:, 4 + blk, :], rhs=xcb[:], start=True, stop=True),
                                  reads=["gates", ("xcb", tb)], writes=[("ps", bx)])
                            yield
                            sc.op("act", lambda e, blk=blk, ba=ba: e.activation(out=T["r"][:], in_=psf[ba][:], func=AF.Sigmoid,
                                                                                bias=small[:, SP_GAB + blk:SP_GAB + blk + 1]),
                                  reads=[("ps", ba), "small"], writes=[("t_r", tb)])
                            yield
                            sc.op("act", lambda e, blk=blk, bx=bx: e.activation(out=T["ig"][:], in_=psf[bx][:], func=AF.Sigmoid,
                                                                                bias=small[:, SP_GXB + blk:SP_GXB + blk + 1]),
                                  reads=[("ps", bx), "small"], writes=[("t_ig", tb)])
                            yield
                            sc.op("pool", lambda e: e.tensor_tensor(out=T["g"][:], in0=T["y"][:], in1=T["y"][:], op=ALU.mult),
                                  reads=[("t_y", tb)], writes=[("t_g", tb)])
                            yield
                            sc.op("dve", lambda e: e.tensor_scalar(out=T["g"][:], in0=T["g"][:], scalar1=0.044715, scalar2=1.0,
                                                                   op0=ALU.mult, op1=ALU.add), reads=[("t_g", tb)], writes=[("t_g", tb)])
                            yield
                            sc.op("pool", lambda e: e.tensor_tensor(out=T["g"][:], in0=T["g"][:], in1=T["y"][:], op=ALU.mult),
                                  reads=[("t_g", tb), ("t_y", tb)], writes=[("t_g", tb)])
                            yield
                            sc.op("act", lambda e: e.activation(out=T["g"][:], in_=T["g"][:], func=AF.Sigmoid, scale=1.5957691216057308),
                                  reads=[("t_g", tb)], writes=[("t_g", tb)])
                            yield
                            sc.op("pool", lambda e: e.tensor_tensor(out=T["g"][:], in0=T["g"][:], in1=T["y"][:], op=ALU.mult),
                                  reads=[("t_g", tb), ("t_y", tb)], writes=[("t_g", tb)])
                            yield
                            sc.op("act", lambda e, blk=blk: e.activation(out=T["a"][:], in_=T["r"][:], func=AF.Exp, scale=negc[:, blk:blk + 1]),
                                  reads=[("t_r", tb), "negc"], writes=[("t_a", tb)])
                            yield
                            sc.op("pool", lambda e: e.tensor_tensor(out=T["a2"][:], in0=T["a"][:], in1=T["a"][:], op=ALU.mult),
                                  reads=[("t_a", tb)], writes=[("t_a2", tb)])
                            yield
                            sc.op("act", lambda e: e.activation(out=T["a2"][:], in_=T["a2"][:], func=AF.Sqrt, scale=-1.0, bias=1.0),
                                  reads=[("t_a2", tb)], writes=[("t_a2", tb)])
                            yield
                            sc.op("dve", lambda e: e.tensor_tensor(out=T["u"][:], in0=T["ig"][:], in1=T["xc"][:], op=ALU.mult),
                                  reads=[("t_ig", tb), ("t_xc", tb)], writes=[("t_u", tb)])
                            yield
                            sc.op("dve", lambda e: e.tensor_tensor(out=T["u"][:], in0=T["u"][:], in1=T["a2"][:], op=ALU.mult),
                                  reads=[("t_u", tb), ("t_a2", tb)], writes=[("t_u", tb)])
                            yield
                            sc.op("dve", lambda e, blk=blk: e.tensor_tensor_scan(out=T["h"][:], data0=T["a"][:], data1=T["u"][:],
                                                                               initial=hlast[:, blk:blk + 1], op0=ALU.mult, op1=ALU.add),
                                  reads=[("t_a", tb), ("t_u", tb), ("hlast", blk)], writes=[("t_h", tb)])
                            yield
                            sc.op("dve", lambda e, blk=blk: e.tensor_copy(out=hlast[:, blk:blk + 1], in_=T["h"][:, TT - 1:TT]),
                                  reads=[("t_h", tb)], writes=[("hlast", blk)])
                            yield
                            sc.op("dve", lambda e, blk=blk: e.tensor_tensor(out=mixedT[:, blk, t0:t0 + TT], in0=T["h"][:], in1=T["g"][:], op=ALU.mult),
                                  reads=[("t_h", tb), ("t_g", tb)], writes=[("mixed", blk, c)])
                        if dbg_stage not in ("A1", "A2"):
                            for pair in ((0, 1), (2, 3)):
                                gens = [rg_block(bk) for bk in pair]
                                while gens:
                                    for g_ in list(gens):
                                        try:
                                            next(g_)
                                        except StopIteration:
                                            gens.remove(g_)
                    if dbg_stage in ("A", "A1", "A2") and s == 0:
                        sc.barrier()
                        dbg("mixed_rec", mixedT[:, 0:4, :], [128, 4, S], "x", BF16)
                        dbg("qT", qT[0:68, :, :], [68, 8, S], "x", BF16)
                        dbg("kT", kT[0:68, :], [68, S], "x", BF16)
                        dbg("vA", vA[:], [128, NT, 65], "x", BF16)
                        dbg("iqT", iqT[:], [64, 4, S], "x", BF16)
                        dbg("ikT", ikT[:], [64, S], "x", BF16)
                        dbg("iw", iw[:], [128, NT, 4], "x", F32)
                    sc.barrier()
                with contextlib.ExitStack() as esB:
                    eB = esB.enter_context

                    def sbB(name, shape, dt):
                        return eB(nc.sbuf_tensor("d%d_" % s + name, list(shape), dt))

                    isc = [sbB("isc%d" % i, [128, S], F32) for i in range(2)]
                    work = sbB("work", [128, S], F32)
                    eqm = [sbB("eqm%d" % i, [128, S], BF16) for i in range(2)]
                    rl = [sbB("rl%d" % i, [128, TT], F32) for i in range(4)]
                    m8 = sbB("m8", [128, 8], F32)
                    P = [sbB("P%d" % i, [128, 512], BF16) for i in range(5)]
                    rc = sbB("rc", [128, 8], F32)
                    o_b = sbB("o_b", [128, 512], BF16)
                    def idx_topk(i):
                        L = 128 * (i + 1)
                        q0 = i * 128
                        I = isc[i % 2]
                        E = eqm[i % 2]
                        ik_ = ("isc", i % 2)
                        ek_ = ("eqm", i % 2)
                        nkb = (L + 511) // 512
                        for kb in range(nkb):
                            w = min(512, L - kb * 512)
                            k0 = kb * 512
                            for hI in range(4):
                                sc.op("pe", lambda e: e.matmul(psf[hI][:, 0:w], lhsT=iqT[:, hI, q0:q0 + 128],
                                                               rhs=ikT[:, k0:k0 + w], start=True, stop=True),
                                      reads=["iqT", "ikT"], writes=[("ps", hI)])
                                sc.op("act", lambda e: e.activation(out=rl[hI][:, 0:w], in_=psf[hI][:, 0:w], func=AF.Relu),
                                      reads=[("ps", hI)], writes=[("rl", hI)])
                            sc.op("dve", lambda e: e.tensor_scalar(out=I[:, k0:k0 + w], in0=rl[0][:, 0:w], scalar1=iw[:, i, 0:1],
                                                                   scalar2=None, op0=ALU.mult),
                                  reads=[("rl", 0), "iw"], writes=[ik_])
                            for hI in range(1, 4):
                                sc.op("dve", lambda e: e.scalar_tensor_tensor(
                                    out=I[:, k0:k0 + w], in0=rl[hI][:, 0:w], scalar=iw[:, i, hI:hI + 1], in1=I[:, k0:k0 + w],
                                    op0=ALU.mult, op1=ALU.add), reads=[("rl", hI), "iw", ik_], writes=[ik_])
                        sc.op("dve", lambda e: e.tensor_tensor(out=I[:, q0:q0 + 128], in0=I[:, q0:q0 + 128], in1=causal_f[:], op=ALU.add),
                              reads=[ik_, "causal_f"], writes=[ik_])
                        if dbg_stage == "B1":
                            if i in (1, 5):
                                dbg("isc%d" % i, I[:, 0:L], [128, L], ik_, F32)
                            return
                        if i < 2:
                            sc.op("dve", lambda e: e.tensor_scalar(out=E[:, 0:L], in0=I[:, 0:L], scalar1=-1.0e29, scalar2=None, op0=ALU.is_lt),
                                  reads=[ik_], writes=[ek_])
                        else:
                            for r in range(32):
                                src_ = I if r == 0 else work
                                sk = ik_ if r == 0 else "work"
                                sc.op("dve", lambda e: e.max(out=m8[:], in_=src_[:, 0:L]), reads=[sk], writes=["m8"])
                                sc.op("dve", lambda e: e.match_replace(out=work[:, 0:L], in_to_replace=m8[:], in_values=src_[:, 0:L],
                                                                       imm_value=NEG_REPL), reads=[sk, "m8"], writes=["work"])
                            sc.op("dve", lambda e: e.tensor_tensor(out=E[:, 0:L], in0=work[:, 0:L], in1=I[:, 0:L], op=ALU.is_equal),
                                  reads=["work", ik_], writes=[ek_])
                        if dbg_stage in ("B2",) and s == 0 and i in (1, 5):
                            dbg("eqm%d" % i, E[:, 0:L], [128, L], ek_, BF16)
                            dbg("isc%d" % i, I[:, 0:L], [128, L], ik_, F32)

                    def attention(i):
                        q0 = i * 128
                        E = eqm[i % 2]
                        ek_ = ("eqm", i % 2)
                        ng = (i + 4) // 4
                        units = [(h, g) for h in range(8) for g in range(ng)]

                        def qk(u):
                            h, g = units[u]
                            nj = min(4, i + 1 - 4 * g)
                            sbank = u % 4
                            for jj in range(nj):
                                j = 4 * g + jj
                                sc.op("pe", lambda e: e.matmul(psf[sbank][:, jj * 128:(jj + 1) * 128], lhsT=kT[0:68, j * 128:(j + 1) * 128],
                                                               rhs=qT[0:68, h, q0:q0 + 128], start=True, stop=False),
                                      reads=["qT", "kT"], writes=[("ps", sbank)])
                                sc.op("pe", lambda e: e.matmul(psf[sbank][:, jj * 128:(jj + 1) * 128], lhsT=E[:, j * 128:(j + 1) * 128],
                                                               rhs=identneg_b[:], start=False, stop=True),
                                      reads=[ek_, "identneg_b"], writes=[("ps", sbank)])

                        def tail_dve(hh):
                            ob = psf[4 + hh]
                            sc.op("dve", lambda e: e.reciprocal(out=rc[:, hh * 4:(hh + 1) * 4],
                                                                in_=ob[:, 0:260].rearrange("p (h c) -> p h c", c=65)[:, :, 64]),
                                  reads=[("ps", 4 + hh)], writes=[("rc", hh)])
                            for h4 in range(4):
                                h = hh * 4 + h4
                                sc.op("dve", lambda e: e.tensor_scalar(out=o_b[:, h * 64:(h + 1) * 64], in0=ob[:, h4 * 65:h4 * 65 + 64],
                                                                       scalar1=rc[:, h:h + 1], scalar2=None, op0=ALU.mult),
                                      reads=[("ps", 4 + hh), ("rc", hh)], writes=[("o_b", hh)])

                        def tail_pe(hh):
                            for kk in (2 * hh, 2 * hh + 1):
                                sc.op("pe", lambda e: e.transpose(out=psb[:, kk * 128:(kk + 1) * 128], in_=o_b[:, kk * 128:(kk + 1) * 128],
                                                                  identity=ident_b[:]),
                                      reads=[("o_b", hh), "ident_b"], writes=[("ps", "b")])
                            sc.op("act", lambda e: e.activation(out=mixedT[:, 4 + 2 * hh:6 + 2 * hh, q0:q0 + 128],
                                                                in_=psb[:, hh * 256:(hh + 1) * 256].rearrange("p (k t) -> p k t", k=2), func=AF.Copy),
                                  reads=[("ps", "b")], writes=[("mixed_att", i, hh)])

                        deferred = []
                        LOOK = 3
                        for u0 in range(min(LOOK, len(units))):
                            qk(u0)
                        for u in range(len(units)):
                            h, g = units[u]
                            nj = min(4, i + 1 - 4 * g)
                            sbank = u % 4
                            Pt = P[u % 5]
                            pk = ("P", u % 5)
                            if u + LOOK < len(units):
                                qk(u + LOOK)
                            sc.op("act", lambda e: e.activation(out=Pt[:, 0:nj * 128], in_=psf[sbank][:, 0:nj * 128], func=AF.Exp, scale=0.125),
                                  reads=[("ps", sbank)], writes=[pk])
                            ob = psf[4 + h // 4]
                            oc = (h % 4) * 65
                            for jj in range(nj):
                                j = 4 * g + jj
                                sc.op("pe", lambda e: e.matmul(ob[:, oc:oc + 65], lhsT=Pt[:, jj * 128:(jj + 1) * 128], rhs=vA[:, j, :],
                                                               start=(g == 0 and jj == 0), stop=(g == ng - 1 and jj == nj - 1)),
                                      reads=[pk, "vA"], writes=[("ps", 4 + h // 4)])
                            for d in deferred:
                                d[0] -= 1
                            while deferred and deferred[0][0] <= 0:
                                tail_pe(deferred.pop(0)[1])
                            if g == ng - 1 and h % 4 == 3:
                                tail_dve(h // 4)
                                deferred.append([4, h // 4])
                        for d in deferred:
                            tail_pe(d[1])

                    idx_topk(0)
                    for i in range(NT):
                        if i + 1 < NT:
                            idx_topk(i + 1)
                        conv_pump(2)
                        if dbg_stage in ("B1", "B2"):
                            continue
                        attention(i)
                    if dbg_stage in ("A", "B") and s == 0:
                        sc.barrier()
                        dbg("mixed_att", mixedT[:, 4:8, :], [128, 4, S], "x", BF16)
                    sc.barrier()

        def layer1_mixer(s):
            with contextlib.ExitStack() as esA:
                eA = esA.enter_context

                def sbA(name, shape, dt):
                    return eA(nc.sbuf_tensor("m%d_" % s + name, list(shape), dt))

                cqn = sbA("cqn", [128, 4, S], BF16)
                ckvn = sbA("ckvn", [128, 2, S], BF16)
                krope = sbA("krope", [128, S], BF16)
                vM = sbA("vM", [128, NT, 16, 65], BF16)
                cosF = sbA("cossin", [128, S], F32)
                sinS = cosF
                wuq_b = sbA("wuq", [128, 4, 2048], BF16)
                wukv_b = sbA("wukv", [128, 2, 2048], BF16)
                sc.dma("sp", cosF[64:96, :], c_cos[:, :], writes=["cosF"])
                sc.dma("sp", sinS[96:128, :], c_sin[:, :], writes=["sinS"])
                sc.dma("sp", wuq_b[:], wb["w_uq"].rearrange("(p k) c -> p k c", k=4), reads=wbk("w_uq"), writes=["wuq"])
                sc.dma("sp", wukv_b[:], wb["w_ukv"].rearrange("(p k) c -> p k c", k=2), reads=wbk("w_ukv"), writes=["wukv"])
                sc.op("dve", lambda e: e.memset(vM[:, :, :, 64:65], 1.0), writes=["vM_ones"])
                bank = [0]

                def nb():
                    b = bank[0]
                    bank[0] = (b + 1) % 4
                    return b

                with contextlib.ExitStack() as esB:
                    eB = esB.enter_context

                    def sbB(name, shape, dt):
                        return eB(nc.sbuf_tensor("n%d_" % s + name, list(shape), dt))

                    wdn_b = sbB("wdn", [128, 8, 896], BF16)
                    xbuf = [sbB("xb%d" % i, [128, 8, TT], BF16) for i in range(2)]
                    cf = sbB("cf", [128, 6, TT], F32)
                    sq = sbB("sq", [128, 6, TT], F32)
                    rs = sbB("rs", [128, 2, TT], F32)
                    rt = sbB("rt", [128, 2, TT], F32)
                    sc.dma("sp", wdn_b[:], wb["w_down"].rearrange("(p k) c -> p k c", k=8), reads=wbk("w_down"), writes=["wdn"])
                    def load_xb(cc):
                        sc.dma("pool", xbuf[cc % 2][:], x1b[s, :, cc * TT:(cc + 1) * TT].rearrange("(k p) t -> p k t", p=128),
                               reads=[("x1b", s)], writes=[("xb", cc % 2)])

                    load_xb(0)
                    for c in range(NCH):
                        t0 = c * TT
                        xb = xbuf[c % 2]
                        if c + 1 < NCH:
                            load_xb(c + 1)
                        for blk in range(7):
                            b = nb()
                            for k in range(8):
                                sc.op("pe", lambda e, k=k, blk=blk, b=b: e.matmul(psf[b][:], lhsT=wdn_b[:, k, blk * 128:(blk + 1) * 128],
                                                                                   rhs=xb[:, k, :], start=(k == 0), stop=(k == 7)),
                                      reads=["wdn", ("xb", c % 2)], writes=[("ps", b)])
                            if blk < 6:
                                sc.op("act", lambda e, blk=blk, b=b: e.activation(out=cf[:, blk, :], in_=psf[b][:], func=AF.Copy),
                                      reads=[("ps", b)], writes=[("cf", blk)])
                                sc.op("act", lambda e, blk=blk, b=b: e.activation(out=sq[:, blk, :], in_=psf[b][:], func=AF.Square),
                                      reads=[("ps", b)], writes=[("sq", blk)])
                            else:
                                sc.op("dve", lambda e, b=b: e.tensor_tensor(out=rt[64:96, 0, :], in0=psf[b][96:128, :], in1=sinS[96:128, t0:t0 + TT], op=ALU.mult),
                                      reads=[("ps", b), "sinS"], writes=["rt0"])
                                sc.op("dve", lambda e, b=b: e.tensor_tensor(out=rt[64:96, 1, :], in0=psf[b][64:96, :], in1=cosF[64:96, t0:t0 + TT], op=ALU.mult),
                                      reads=[("ps", b), "cosF"], writes=["rt1"])
                                sc.op("dve", lambda e: e.tensor_tensor(out=krope[64:96, t0:t0 + TT], in0=rt[64:96, 0, :], in1=rt[64:96, 1, :], op=ALU.add),
                                      reads=["rt0", "rt1"], writes=[("krope", c)])
                        for grp, (k0, nk, dim, nof) in enumerate([(0, 4, 512, SP_QN), (4, 2, 256, SP_KVN)]):
                            pq = psf[4 + grp]
                            for kk in range(nk):
                                sc.op("pe", lambda e, kk=kk, k0=k0, nk=nk, pq=pq: e.matmul(pq[:], lhsT=ones_f[:], rhs=sq[:, k0 + kk, :],
                                                                                          start=(kk == 0), stop=(kk == nk - 1)),
                                      reads=[("sq", k0 + kk), "ones_f"], writes=[("ps", 4 + grp)])
                            sc.op("act", lambda e, grp=grp, dim=dim, pq=pq: e.activation(out=rs[:, grp, :], in_=pq[:], func=AF.Ln, scale=1.0 / dim,
                                                                                         bias=eps_t[:, 1:2]),
                                  reads=[("ps", 4 + grp), "eps_t"], writes=[("rs", grp)])
                            sc.op("act", lambda e, grp=grp: e.activation(out=rs[:, grp, :], in_=rs[:, grp, :], func=AF.Exp, scale=-0.5),
                                  reads=[("rs", grp)], writes=[("rs", grp)])
                            for kk in range(nk):
                                dst = cqn[:, kk, t0:t0 + TT] if grp == 0 else ckvn[:, kk, t0:t0 + TT]
                                sc.op("dve", lambda e, kk=kk, k0=k0, grp=grp, nof=nof, dst=dst: e.scalar_tensor_tensor(
                                    out=dst, in0=cf[:, k0 + kk, :], scalar=small[:, nof + kk:nof + kk + 1], in1=rs[:, grp, :],
                                    op0=ALU.mult, op1=ALU.mult),
                                    reads=[("cf", k0 + kk), ("rs", grp), "small"], writes=[("cn", grp, kk, c)])
                        for tt in range(4):
                            ti = c * 4 + tt
                            for half in range(2):
                                b = nb()
                                for kk in range(2):
                                    sc.op("pe", lambda e, kk=kk, tt=tt, half=half, b=b: e.matmul(
                                        psf[b][:], lhsT=ckvn[:, kk, t0 + tt * 128:t0 + (tt + 1) * 128],
                                        rhs=wukv_b[:, kk, 1024 + half * 512:1024 + (half + 1) * 512], start=(kk == 0), stop=(kk == 1)),
                                        reads=[("cn", 1, kk, c), "wukv"], writes=[("ps", b)])
                                sc.op("act", lambda e, ti=ti, half=half, b=b: e.activation(
                                    out=vM[:, ti, half * 8:(half + 1) * 8, 0:64], in_=psf[b][:].rearrange("p (h d) -> p h d", d=64), func=AF.Copy),
                                    reads=[("ps", b), "vM_ones"], writes=[("vM", ti, half)])
                    if dbg_stage == "D" and s == 0:
                        sc.barrier()
                        dbg("cqn", cqn[:], [128, 4, S], "x", BF16)
                        dbg("ckvn", ckvn[:], [128, 2, S], "x", BF16)
                        dbg("krope", krope[64:96, :], [32, S], "x", BF16)
                        dbg("vM", vM[:], [128, NT, 16, 65], "x", BF16)
                    sc.barrier()
                with contextlib.ExitStack() as esB:
                    eB = esB.enter_context

                    def sbB(name, shape, dt):
                        return eB(nc.sbuf_tensor("e%d_" % s + name, list(shape), dt))

                    qTb = [sbB("qT%d" % i, [128, 4, S], BF16) for i in range(2)]
                    kTb = [sbB("kT%d" % i, [128, 4, S], BF16) for i in range(2)]
                    rt = sbB("rt", [128, 2, TT], F32)
                    P = [sbB("P%d" % i, [128, 512], BF16) for i in range(5)]
                    rc = sbB("rc", [128, 8], F32)
                    o_b = sbB("o_b", [128, 512], BF16)
                    pcnt = [0]
                    def proj_gen(hg):
                        qT = qTb[hg % 2]
                        kT = kTb[hg % 2]
                        pb_ = hg % 2
                        b = 6
                        for hh in range(4):
                            head = hg * 4 + hh
                            for c in range(NCH):
                                t0 = c * TT
                                for kk in range(4):
                                    sc.op("pe", lambda e: e.matmul(psf[b][:], lhsT=wuq_b[:, kk, head * 128:(head + 1) * 128], rhs=cqn[:, kk, t0:t0 + TT],
                                                                   start=(kk == 0), stop=(kk == 3)), reads=["wuq", "cqn"], writes=[("ps", b)])
                                sc.op("dve", lambda e: e.tensor_copy(out=qT[0:64, hh, t0:t0 + TT], in_=psf[b][0:64, :]),
                                      reads=[("ps", b)], writes=[("qT", pb_, hh)])
                                sc.op("dve", lambda e: e.tensor_tensor(out=rt[64:96, 0, :], in0=psf[b][96:128, :], in1=sinS[96:128, t0:t0 + TT], op=ALU.mult),
                                      reads=[("ps", b), "sinS"], writes=["rt0"])
                                sc.op("dve", lambda e: e.tensor_tensor(out=rt[64:96, 1, :], in0=psf[b][64:96, :], in1=cosF[64:96, t0:t0 + TT], op=ALU.mult),
                                      reads=[("ps", b), "cosF"], writes=["rt1"])
                                sc.op("dve", lambda e: e.tensor_tensor(out=qT[64:96, hh, t0:t0 + TT], in0=rt[64:96, 0, :], in1=rt[64:96, 1, :], op=ALU.add),
                                      reads=["rt0", "rt1"], writes=[("qT", pb_, hh)])
                                yield
                            sc.op("pool", lambda e: e.tensor_copy(out=kT[64:96, hh, :], in_=krope[64:96, :]),
                                  reads=["krope"], writes=[("kT", pb_, hh)])
                        for pr in range(2):
                            h0 = hg * 4 + pr * 2
                            for c in range(NCH):
                                t0 = c * TT
                                for kk in range(2):
                                    sc.op("pe", lambda e: e.matmul(psf[b][:], lhsT=wukv_b[:, kk, h0 * 64:h0 * 64 + 128], rhs=ckvn[:, kk, t0:t0 + TT],
                                                                   start=(kk == 0), stop=(kk == 1)), reads=["wukv", "ckvn"], writes=[("ps", b)])
                                sc.op("dve", lambda e: e.tensor_copy(out=kT[0:64, pr * 2, t0:t0 + TT], in_=psf[b][0:64, :]),
                                      reads=[("ps", b)], writes=[("kT", pb_, pr * 2)])
                                sc.op("dve", lambda e: e.tensor_copy(out=kT[0:64, pr * 2 + 1, t0:t0 + TT], in_=psf[b][64:128, :]),
                                      reads=[("ps", b)], writes=[("kT", pb_, pr * 2 + 1)])
                                yield

                    gen_next = proj_gen(0)
                    for hg in range(4):
                        for _ in gen_next:
                            pass
                        gen_next = proj_gen(hg + 1) if hg + 1 < 4 else iter(())
                        qT = qTb[hg % 2]
                        kT = kTb[hg % 2]
                        pb_ = hg % 2
                        if dbg_stage == "E" and s == 0 and hg == 0:
                            sc.barrier()
                            dbg("qT", qT[0:96, :, :], [96, 4, S], "x", BF16)
                            dbg("kT", kT[0:96, :, :], [96, 4, S], "x", BF16)
                        units = [(i, hh, g) for i in range(NT) for hh in range(4) for g in range((i + 4) // 4)]

                        def qk(u):
                            i, hh, g = units[u]
                            q0 = i * 128
                            nj = min(4, i + 1 - 4 * g)
                            sbank = u % 4
                            for jj in range(nj):
                                j = 4 * g + jj
                                diag = (j == i)
                                sc.op("pe", lambda e: e.matmul(psf[sbank][:, jj * 128:(jj + 1) * 128], lhsT=kT[0:96, hh, j * 128:(j + 1) * 128],
                                                               rhs=qT[0:96, hh, q0:q0 + 128], start=True, stop=not diag),
                                      reads=[("qT", pb_, hh), ("kT", pb_, hh)], writes=[("ps", sbank)])
                                if diag:
                                    sc.op("pe", lambda e: e.matmul(psf[sbank][:, jj * 128:(jj + 1) * 128], lhsT=tri_b[:], rhs=identneg_b[:],
                                                                   start=False, stop=True),
                                          reads=["tri_b", "identneg_b"], writes=[("ps", sbank)])

                        def tail_dve(i):
                            ob = psf[4 + (i % 2)]
                            obk = ("ps", 4 + (i % 2))
                            sc.op("dve", lambda e: e.reciprocal(out=rc[:, (i % 2) * 4:(i % 2) * 4 + 4],
                                                                in_=ob[:, 0:260].rearrange("p (h c) -> p h c", c=65)[:, :, 64]),
                                  reads=[obk], writes=[("rc", i % 2)])
                            for hh in range(4):
                                sc.op("dve", lambda e: e.tensor_scalar(out=o_b[:, (i % 2) * 256 + hh * 64:(i % 2) * 256 + (hh + 1) * 64],
                                                                       in0=ob[:, hh * 65:hh * 65 + 64],
                                                                       scalar1=rc[:, (i % 2) * 4 + hh:(i % 2) * 4 + hh + 1], scalar2=None, op0=ALU.mult),
                                      reads=[obk, ("rc", i % 2)], writes=[("o_b", i % 2)])

                        def tail_pe(i):
                            q0 = i * 128
                            o0 = (i % 2) * 256
                            for kk in range(2):
                                sc.op("pe", lambda e: e.transpose(out=psb[:, o0 + kk * 128:o0 + (kk + 1) * 128],
                                                                  in_=o_b[:, o0 + kk * 128:o0 + (kk + 1) * 128], identity=ident_b[:]),
                                      reads=[("o_b", i % 2), "ident_b"], writes=[("ps", "b")])
                            sc.op("act", lambda e: e.activation(out=mixedT[:, 2 * hg:2 * hg + 2, q0:q0 + 128],
                                                                in_=psb[:, o0:o0 + 256].rearrange("p (k t) -> p k t", k=2), func=AF.Copy),
                                  reads=[("ps", "b")], writes=[("mixed_att", i)])

                        deferred = []
                        LOOK = 3
                        for u0 in range(min(LOOK, len(units))):
                            qk(u0)
                        for u in range(len(units)):
                            i, hh, g = units[u]
                            head = hg * 4 + hh
                            ng = (i + 4) // 4
                            nj = min(4, i + 1 - 4 * g)
                            sbank = u % 4
                            Pt = P[u % 5]
                            pk = ("P", u % 5)
                            if u + LOOK < len(units):
                                qk(u + LOOK)
                            sc.op("act", lambda e: e.activation(out=Pt[:, 0:nj * 128], in_=psf[sbank][:, 0:nj * 128], func=AF.Exp,
                                                                scale=96.0 ** -0.5),
                                  reads=[("ps", sbank)], writes=[pk])
                            ob = psf[4 + (i % 2)]
                            oc = hh * 65
                            for jj in range(nj):
                                j = 4 * g + jj
                                sc.op("pe", lambda e: e.matmul(ob[:, oc:oc + 65], lhsT=Pt[:, jj * 128:(jj + 1) * 128], rhs=vM[:, j, head, :],
                                                               start=(g == 0 and jj == 0), stop=(g == ng - 1 and jj == nj - 1)),
                                      reads=[pk, "vM"], writes=[("ps", 4 + (i % 2))])
                            for d in deferred:
                                d[0] -= 1
                            while deferred and deferred[0][0] <= 0:
                                tail_pe(deferred.pop(0)[1])
                            if g == ng - 1 and hh == 3:
                                tail_dve(i)
                                deferred.append([4, i])
                            if u % 5 == 4:
                                next(gen_next, None)
                        for d in deferred:
                            tail_pe(d[1])
                    if dbg_stage == "E" and s == 0:
                        sc.barrier()
                        dbg("mixed1", mixedT[:], [128, 8, S], "x", BF16)
                    sc.barrier()

        stop = False
        if dbg_stage == "0":
            dbg("negc", negc[:], [128, 8], "negc", F32)
        for s in range(2):
            if dbg_stage == "0":
                break
            layer0_mixer(s)
            if dbg_stage in ("A", "A1", "A2", "B", "B1", "B2"):
                break
            phase_c(0, s, xT, final=False)
            if dbg_stage == "C":
                break
            layer1_mixer(s)
            if dbg_stage in ("D", "E"):
                break
            phase_c(1, s, x1T, final=True)
        if dbg_stage == "C":
            dbg("x1T", x1T[0], [D, S], ("xsrc", 1, 0), F32)
        conv_pump(1000)
        sc.barrier(final=True)
        dbg_out["_ninst"] = (sc.ninst, dict(sc.ccnt))
    return nc, dbg_out


def _host_prep(inputs):
    f = np.float32
    g = {k: np.asarray(v, dtype=f) for k, v in inputs.items()}
    W = {}
    w_in = g["hy_w_in"][0]
    perm = np.concatenate([np.arange(0, 1600), np.arange(1664, 1984), np.arange(1600, 1664), np.arange(1984, 1988)])
    w = w_in[:, perm]
    W["w_in"] = np.ascontiguousarray(w.reshape(8, 128, WIN_COLS).transpose(1, 0, 2).reshape(1024, WIN_COLS))
    gates = np.zeros((128, 8, 128), f)
    for gi, name in enumerate(["hy_ga_w", "hy_gx_w"]):
        gw = g[name][0]
        for blk in range(4):
            for half in range(2):
                n = 2 * blk + half
                gates[half * 64:(half + 1) * 64, gi * 4 + blk, half * 64:(half + 1) * 64] = gw[n]
    W["gates"] = gates.reshape(128, 1024)

    def pk(wm, kch):
        r, c = wm.shape
        return np.ascontiguousarray(wm.reshape(kch, 128, c).transpose(1, 0, 2).reshape(128 * kch, c))

    W["w_out0"] = pk(g["hy_w_out"][0], 8)
    W["w_out1"] = pk(g["mla_w_out"][0], 8)
    w1 = g["mlp_w1"]
    W["w1"] = np.ascontiguousarray(w1.reshape(2, 8, 128, 32, 128).transpose(0, 3, 2, 1, 4).reshape(2 * 32 * 128, 1024))
    w2 = g["mlp_w2"]
    W["w2"] = np.ascontiguousarray(w2.reshape(2, 2, 16, 128, 8, 128).transpose(0, 4, 3, 1, 2, 5).reshape(2 * 8 * 128 * 2, 2048))
    W["wg"] = np.concatenate([pk(g["ple_w_gate"][l], 8) for l in range(2)], axis=0)
    W["wp"] = np.concatenate([pk(g["ple_w_proj"][l], 2) for l in range(2)], axis=0)
    wd = g["mla_w_down"][0]
    sw = (np.arange(32) + 16) % 32
    kr = wd[:, 768:800]
    wdn = np.concatenate([wd[:, 0:768], kr, kr[:, sw], kr, kr[:, sw]], axis=1)
    W["w_down"] = pk(wdn, 8)
    wuq = g["mla_w_uq"][0].reshape(512, 16, 96)
    wuq2 = np.concatenate([wuq[:, :, 0:64], wuq[:, :, 64:96], wuq[:, :, 64:96][:, :, sw]], axis=2).reshape(512, 2048)
    W["w_uq"] = pk(wuq2, 4)
    wukv = g["mla_w_ukv"][0].reshape(256, 16, 128)
    wukv2 = np.concatenate([wukv[:, :, 0:64].reshape(256, 1024), wukv[:, :, 64:128].reshape(256, 1024)], axis=1)
    W["w_ukv"] = pk(wukv2, 2)

    small = np.zeros((128, SP_N), f)

    def col(v, n):
        return v.reshape(n, 128).T

    for l in range(2):
        small[:, SP_LN + l * 32 + 0:SP_LN + l * 32 + 8] = col(g["ln1_g"][l], 8)
        small[:, SP_LN + l * 32 + 8:SP_LN + l * 32 + 16] = col(g["ln1_b"][l], 8)
        small[:, SP_LN + l * 32 + 16:SP_LN + l * 32 + 24] = col(g["ln2_g"][l], 8)
        small[:, SP_LN + l * 32 + 24:SP_LN + l * 32 + 32] = col(g["ln2_b"][l], 8)
    cw = g["hy_conv_w"][0]
    for blk in range(4):
        for tap in range(4):
            small[:, SP_CONVW + blk * 4 + tap] = cw[tap, blk * 128:(blk + 1) * 128]
    small[:, SP_CONVB:SP_CONVB + 4] = col(g["hy_conv_b"][0], 4)
    small[:, SP_GAB:SP_GAB + 4] = col(g["hy_ga_b"][0], 4)
    small[:, SP_GXB:SP_GXB + 4] = col(g["hy_gx_b"][0], 4)
    small[:, SP_LAM:SP_LAM + 4] = col(g["hy_lambda"][0], 4)
    small[:, SP_QN:SP_QN + 4] = col(g["mla_q_norm"][0], 4)
    small[:, SP_KVN:SP_KVN + 2] = col(g["mla_kv_norm"][0], 2)

    C = {}
    C["c_ident"] = np.eye(128, dtype=f)
    tl = np.arange(128)[:, None]
    sl = np.arange(128)[None, :]
    C["c_causal"] = np.where(sl <= tl, 0.0, NEG_FILL).astype(f)
    C["c_tri"] = (sl > tl).astype(f)
    pos = np.arange(S)
    C["c_kaug"] = np.stack([np.ones(S), np.ones(S), pos % 128, (pos // 128) * 128]).astype(f)
    qa = np.zeros((4, 8, S), f)
    for h in range(8):
        sl8 = 8.0 * 2.0 ** (-(h + 1))
        qa[0, h] = -sl8 * 128.0 * (pos // 128)
        qa[1, h] = -sl8 * (pos % 128)
        qa[2, h] = sl8
        qa[3, h] = sl8
    C["c_qaug"] = qa.reshape(4, 8 * S)
    freq = (np.float32(10000.0) ** (-np.arange(0, 32, 2, dtype=f) / np.float32(32))).astype(f)
    ang = pos.astype(f)[:, None] * freq[None, :]
    cos = np.cos(ang).astype(f).T
    sin = np.sin(ang).astype(f).T
    C["c_cos"] = np.concatenate([cos, cos], axis=0)
    C["c_sin"] = np.concatenate([-sin, sin], axis=0)
    return g, W, small, C


_NC_CACHE = {}


def _run(inputs, dbg_stage=None, n_cores=8):
    g, W, small, C = _host_prep(inputs)
    key = dbg_stage
    if key not in _NC_CACHE:
        _NC_CACHE[key] = build(dbg_stage)
    nc, dbg_out = _NC_CACHE[key]
    x = g["x"]
    p = g["p"]
    in_maps = []
    for c in range(n_cores):
        m = {}
        m["xT"] = np.ascontiguousarray(x[2 * c:2 * c + 2].transpose(0, 2, 1))
        m["pT"] = np.ascontiguousarray(p[:, 2 * c:2 * c + 2].transpose(0, 1, 3, 2))
        m["small"] = small
        m.update(C)
        m.update(W)
        in_maps.append(m)
    res = run_bass_kernel_spmd(nc, in_maps, core_ids=list(range(n_cores)))
    return res, dbg_out


def kernel(**inputs):
    res, _ = _run(inputs)
    out = np.empty((16, S, D), np.float32)
    for c in range(8):
        yT = np.asarray(res.results[c]["yT"])
        out[2 * c:2 * c + 2] = yT.transpose(0, 2, 1)
    return out
```

```python
import contextlib
import numpy as np
import concourse.bass as bass
import concourse.mybir as mybir
from concourse.bass_utils import run_bass_kernel_spmd

F32 = mybir.dt.float32
BF16 = mybir.dt.bfloat16
F32R = mybir.dt.float32r
AF = mybir.ActivationFunctionType
ALU = mybir.AluOpType

S = 2048
D = 1024
TT = 512
NCH = S // TT
NT = S // 128
DN_ALPHA = 4 ** 0.25
LN_EPS = 1e-5
RMS_EPS = 1e-6
import os
DBG_BARRIER = bool(int(os.environ.get('DBG_BARRIER', '0')))
NEG_FILL = -1.0e30
NEG_REPL = -2.0e30
MASK_NEG = -30000.0

SP_LN = 0
SP_CONVW = 64
SP_CONVB = 80
SP_GAB = 84
SP_GXB = 88
SP_LAM = 92
SP_QN = 96
SP_KVN = 100
SP_N = 104

WIN_COLS = 1988
WIN_TM = 1920


class Sched:
    def __init__(self, nc, es):
        self.nc = nc
        self.engs = {"pe": nc.tensor, "act": nc.scalar, "dve": nc.vector, "pool": nc.gpsimd, "sp": nc.sync}
        self.comp = ["pe", "act", "dve", "pool"]
        self.csem, self.ccnt, self.last = {}, {}, {}
        for e in self.comp:
            self.csem[e] = es.enter_context(nc.semaphore("c_" + e))
            self.ccnt[e] = 0
            self.last[e] = None
        self.rings = {}
        for rname, n in (("pool", 5), ("sp", 24), ("bg", 3)):
            self.rings[rname] = {"sem": [es.enter_context(nc.semaphore("d%s%d" % (rname, i))) for i in range(n)],
                                 "cnt": [0] * n, "next": 0}
        self.seen = {}
        self.track = {}
        self.ninst = 0

    def _flush(self, e):
        if e in self.last and self.last[e] is not None:
            self.ccnt[e] += 1
            self.last[e].then_inc(self.csem[e], 1)
            self.last[e] = None

    def _wait(self, e, key, val):
        kind, who = key
        if kind == "c":
            if who == e and e == "pe":
                return
            if val > self.ccnt[who]:
                self._flush(who)
            sem = self.csem[who]
        else:
            sem = self.rings[who[0]]["sem"][who[1]]
        if self.seen.get((e, key), 0) >= val:
            return
        self._flush(e)
        self.engs[e].wait_ge(sem, val)
        self.seen[(e, key)] = val
        self.ninst += 1

    def _deps(self, e, reads, writes):
        deps = {}
        for k in reads:
            t = self.track.get(k)
            if t and t[0] is not None:
                kk, v = t[0]
                deps[kk] = max(deps.get(kk, 0), v)
            if t and isinstance(k, tuple) and k[0] == "ps":
                for kk, v in t[1].items():
                    if kk != ("c", e):
                        deps[kk] = max(deps.get(kk, 0), v)
        for k in writes:
            t = self.track.get(k)
            if t:
                if t[0] is not None:
                    kk, v = t[0]
                    deps[kk] = max(deps.get(kk, 0), v)
                for kk, v in t[1].items():
                    deps[kk] = max(deps.get(kk, 0), v)
        for kk, v in deps.items():
            self._wait(e, kk, v)

    def _record(self, ev, reads, writes):
        kk, v = ev
        for k in reads:
            t = self.track.setdefault(k, [None, {}])
            t[1][kk] = max(t[1].get(kk, 0), v)
        for k in writes:
            self.track[k] = [ev, {}]

    def op(self, e, fn, reads=(), writes=()):
        self._deps(e, reads, writes)
        h = fn(self.engs[e])
        self.last[e] = h
        self.ninst += 1
        self._record((("c", e), self.ccnt[e] + 1), reads, writes)
        return h

    def dma(self, q, out, in_, reads=(), writes=(), bg=False):
        self._deps(q, reads, writes)
        rname = "bg" if bg else q
        ring = self.rings[rname]
        idx = ring["next"]
        ring["next"] = (idx + 1) % len(ring["sem"])
        if ring["cnt"][idx] > 0:
            self._wait(q, ("d", (rname, idx)), ring["cnt"][idx])
        h = self.engs[q].dma_start(out=out, in_=in_)
        ring["cnt"][idx] += 16
        h.then_inc(ring["sem"][idx], 16)
        self.ninst += 1
        self._record((("d", (rname, idx)), ring["cnt"][idx]), reads, writes)

    def barrier(self, final=False):
        for e in self.comp:
            self._flush(e)
        for e in ["pe", "act", "dve", "pool", "sp"]:
            for who in self.comp:
                if self.ccnt[who] > 0 and not (who == e and e == "pe"):
                    self._wait(e, ("c", who), self.ccnt[who])
            for rname, ring in self.rings.items():
                if rname == "bg" and not final:
                    continue
                for idx in range(len(ring["sem"])):
                    if ring["cnt"][idx] > 0:
                        self._wait(e, ("d", (rname, idx)), ring["cnt"][idx])
        keep = {k: v for k, v in self.track.items() if isinstance(k, tuple) and k[0] == "wb"}
        self.track.clear()
        if not final:
            self.track.update(keep)


def _w_specs():
    return {
        "w_in": (1024, WIN_COLS),
        "gates": (128, 1024),
        "w_out0": (1024, 1024),
        "w_out1": (1024, 1024),
        "w1": (2 * 32 * 128, 1024),
        "w2": (2 * 8 * 128 * 2, 2048),
        "wg": (2 * 128 * 8, 1024),
        "wp": (2 * 128 * 2, 1024),
        "w_down": (1024, 896),
        "w_uq": (512, 2048),
        "w_ukv": (256, 2048),
    }


def build(dbg_stage=None):
    nc = bass.Bass("TRN2", target_bir_lowering=False)
    dbg_out = {}
    with contextlib.ExitStack() as es:
        sc = Sched(nc, es)
        ent = es.enter_context

        def dram_in(name, shape, dt=F32):
            return nc.dram_tensor(name, list(shape), dt, kind="ExternalInput").ap()

        xT = dram_in("xT", [2, D, S])
        pT = dram_in("pT", [2, 2, 256, S])
        small_d = dram_in("small", [128, SP_N])
        c_ident = dram_in("c_ident", [128, 128])
        c_causal = dram_in("c_causal", [128, 128])
        c_tri = dram_in("c_tri", [128, 128])
        c_kaug = dram_in("c_kaug", [4, S])
        c_qaug = dram_in("c_qaug", [4, 8 * S])
        c_cos = dram_in("c_cos", [32, S])
        c_sin = dram_in("c_sin", [32, S])
        wf, wb = {}, {}
        for name, (r, c) in _w_specs().items():
            wf[name] = dram_in(name, [r, c])
            wb[name] = nc.dram_tensor(name + "_b", [r, c], BF16, kind="Internal").ap()
        x1T = nc.dram_tensor("x1T", [2, D, S], F32, kind="Internal").ap()
        x1b = nc.dram_tensor("x1b", [2, D, S], BF16, kind="Internal").ap()
        yT = nc.dram_tensor("yT", [2, D, S], F32, kind="ExternalOutput").ap()

        def dbg(name, src_ap, shape, key, dt=F32):
            t = nc.dram_tensor("dbg_" + name, list(shape), dt, kind="ExternalOutput").ap()
            dbg_out[name] = (list(shape), dt)
            sc.dma("pool", t, src_ap, reads=[key])

        def sb(name, shape, dt):
            return ent(nc.sbuf_tensor("s_" + name, list(shape), dt))

        psf = [ent(nc.psum_tensor("psf%d" % i, [128, 512], F32)) for i in range(7)]
        psb = ent(nc.psum_tensor("psb", [128, 1024], BF16))
        ident_b = sb("ident_b", [128, 128], BF16)
        identneg_b = sb("identneg_b", [128, 128], BF16)
        causal_f = sb("causal_f", [128, 128], F32)
        tri_b = sb("tri_b", [128, 128], BF16)
        ones_f = sb("ones_f", [128, 128], F32)
        small = sb("small", [128, SP_N], F32)
        negc = sb("negc", [128, 8], F32)
        mixedT = sb("mixedT", [128, 8, S], BF16)

        conv_tasks = []
        wchunks = {}
        for name in ["w_in", "gates", "w_out0", "wg", "wp", "w1", "w2", "w_down", "w_uq", "w_ukv", "w_out1"]:
            r, c = _w_specs()[name]
            step = max(16, (1 << 19) // c // 16 * 16)
            wchunks[name] = []
            for r0 in range(0, r, step):
                r1 = min(r, r0 + step)
                wchunks[name].append((r0, r1))
                conv_tasks.append((name, r0, r1))
        def _prio(t):
            name, r0, r1 = t
            half = _w_specs()[name][0] // 2
            if name in ("w1", "w2", "wg", "wp") and r0 >= half:
                return 2
            if name in ("w_down", "w_uq", "w_ukv", "w_out1"):
                return 1
            return 0
        conv_tasks.sort(key=_prio)
        conv_pos = [0]

        def conv_pump(n):
            while n > 0 and conv_pos[0] < len(conv_tasks):
                name, r0, r1 = conv_tasks[conv_pos[0]]
                conv_pos[0] += 1
                n -= 1
                sc.dma("pool", wb[name][r0:r1, :], wf[name][r0:r1, :], writes=[("wb", name, r0)], bg=True)

        def wbk(name, a=None, b=None):
            r = _w_specs()[name][0]
            a = 0 if a is None else a
            b = r if b is None else b
            need = [(name, r0, r1) for (r0, r1) in wchunks[name] if r0 < b and r1 > a]
            while any(t in conv_tasks[conv_pos[0]:] for t in need):
                conv_pump(1)
            return [("wb", n_, r0) for (n_, r0, r1) in need]

        conv_pump(6)
        sc.dma("pool", ident_b[:], c_ident[:, :], writes=["ident_b"])
        sc.dma("pool", tri_b[:], c_tri[:, :], writes=["tri_b"])
        sc.dma("sp", causal_f[:], c_causal[:, :], writes=["causal_f"])
        sc.dma("sp", small[:], small_d[:, :], writes=["small"])
        sc.op("dve", lambda e: e.memset(ones_f[:], 1.0), writes=["ones_f"])
        sc.op("dve", lambda e: e.tensor_scalar(out=identneg_b[:], in0=ident_b[:], scalar1=MASK_NEG, scalar2=None,
                                               op0=ALU.mult), reads=["ident_b"], writes=["identneg_b"])
        sc.op("act", lambda e: e.activation(out=negc[:, 4:8], in_=small[:, SP_LAM:SP_LAM + 4], func=AF.Exp, scale=-1.0),
              reads=["small"], writes=["negc_t"])
        sc.op("act", lambda e: e.activation(out=negc[:, 4:8], in_=negc[:, 4:8], func=AF.Ln, bias=1.0),
              reads=["negc_t"], writes=["negc_t"])
        sc.op("dve", lambda e: e.tensor_scalar(out=negc[:, 0:4], in0=negc[:, 4:8], scalar1=-8.0, scalar2=None,
                                               op0=ALU.mult), reads=["negc_t"], writes=["negc"])
        sc.barrier()

        def ln_feature_major(z, zkey, layer, which, out_f, out_f_key, out_b, out_b_key, hid, st, stkey):
            gof = SP_LN + layer * 32 + (0 if which == 1 else 16)
            ps_m, ps_q = psf[5], psf[6]
            tmp = hid.bitcast(F32)[:, 16:32, :].rearrange("p (m a) b -> p m (a b)", a=2)
            tk = lambda m: [("hid", 16 + 2 * m), ("hid", 17 + 2 * m)]
            for m in range(8):
                sc.op("act", lambda e: e.activation(out=hid[:, m, :], in_=z[:, m, :], func=AF.Square),
                      reads=[(zkey, m)], writes=[("hid", m)])
                sc.op("dve", lambda e: e.tensor_copy(out=hid[:, 8 + m, :], in_=z[:, m, :]),
                      reads=[(zkey, m)], writes=[("hid", 8 + m)])
            for m in range(8):
                sc.op("pe", lambda e: e.matmul(ps_m[:], lhsT=ones_b[:], rhs=hid[:, 8 + m, :], start=(m == 0), stop=(m == 7)),
                      reads=[("hid", 8 + m), "ones_b"], writes=[("ps", 5)])
            for m in range(8):
                sc.op("pe", lambda e: e.matmul(ps_q[:], lhsT=ones_b[:], rhs=hid[:, m, :], start=(m == 0), stop=(m == 7)),
                      reads=[("hid", m), "ones_b"], writes=[("ps", 6)])
            mean, msq, var, rstd = st[:, 0, :], st[:, 1, :], st[:, 2, :], st[:, 3, :]
            sc.op("act", lambda e: e.activation(out=mean, in_=ps_m[:], func=AF.Copy, scale=1.0 / D),
                  reads=[("ps", 5)], writes=[(stkey, 0)])
            sc.op("act", lambda e: e.activation(out=msq, in_=ps_m[:], func=AF.Square, scale=1.0 / D),
                  reads=[("ps", 5)], writes=[(stkey, 1)])
            sc.op("dve", lambda e: e.scalar_tensor_tensor(out=var, in0=ps_q[:], scalar=1.0 / D, in1=msq,
                                                          op0=ALU.mult, op1=ALU.subtract),
                  reads=[("ps", 6), (stkey, 1)], writes=[(stkey, 2)])
            sc.op("act", lambda e: e.activation(out=var, in_=var, func=AF.Ln, bias=eps_t[:, 0:1]),
                  reads=[(stkey, 2), "eps_t"], writes=[(stkey, 2)])
            sc.op("act", lambda e: e.activation(out=rstd, in_=var, func=AF.Exp, scale=-0.5),
                  reads=[(stkey, 2)], writes=[(stkey, 3)])
            for m in range(8):
                eng = "dve"
                sc.op(eng, lambda e: e.tensor_tensor(out=tmp[:, m, :], in0=z[:, m, :], in1=mean, op=ALU.subtract),
                      reads=[(zkey, m), (stkey, 0)], writes=tk(m))
                sc.op(eng, lambda e: e.tensor_tensor(out=tmp[:, m, :], in0=tmp[:, m, :], in1=rstd, op=ALU.mult),
                      reads=tk(m) + [(stkey, 3)], writes=tk(m))
                sc.op("act", lambda e: e.activation(out=out_b[:, m, :], in_=tmp[:, m, :], func=AF.Identity,
                                                    scale=small[:, gof + m:gof + m + 1], bias=small[:, gof + 8 + m:gof + 9 + m]),
                      reads=tk(m) + ["small"], writes=[(out_b_key, m)])
                sc.op("dve", lambda e: e.tensor_scalar(out=out_f[:, m, :], in0=tmp[:, m, :],
                                                       scalar1=small[:, gof + m:gof + m + 1],
                                                       scalar2=small[:, gof + 8 + m:gof + 9 + m],
                                                       op0=ALU.mult, op1=ALU.add),
                      reads=tk(m) + ["small"], writes=[(out_f_key, m)])

        ones_b = sb("ones_b", [128, 128], BF16)
        sc.op("dve", lambda e: e.memset(ones_b[:], 1.0), writes=["ones_b"])
        eps_t = sb("eps_t", [128, 2], F32)
        sc.op("dve", lambda e: e.memset(eps_t[:, 0:1], LN_EPS), writes=["eps_t0"])
        sc.op("dve", lambda e: e.memset(eps_t[:, 1:2], RMS_EPS), writes=["eps_t1"])
        sc.barrier()

        def phase_c(layer, s, x_src, final):
            with contextlib.ExitStack() as es2:
                e2 = es2.enter_context

                def sb2(name, shape, dt):
                    return e2(nc.sbuf_tensor("c%d%d_" % (layer, s) + name, list(shape), dt))

                wout_b = sb2("wout", [128, 8, D], BF16)
                wg_b = sb2("wg", [128, 8, D], BF16)
                wp_b = sb2("wp", [128, 2, D], BF16)
                xch = sb2("xch", [128, 8, TT], F32)
                z = sb2("z", [128, 8, TT], F32)
                hf = sb2("hf", [128, 8, TT], F32)
                hb = sb2("hb", [128, 8, TT], BF16)
                st = sb2("st", [128, 4, TT], F32)
                pb = sb2("pb", [128, 2, TT], BF16)
                hid = sb2("hid", [128, 32, TT], BF16)
                rl = [sb2("rl%d" % i, [128, TT], F32) for i in range(2)]
                NW1, NW2 = 6, 2
                w1buf = [sb2("w1b%d" % i, [128, 8, 128], BF16) for i in range(NW1)]
                w2buf = [sb2("w2b%d" % i, [128, 32, 128], BF16) for i in range(NW2)]
                wname = "w_out0" if layer == 0 else "w_out1"
                sc.dma("sp", wout_b[:], wb[wname].rearrange("(p k) c -> p k c", k=8), reads=wbk(wname), writes=["wout"])
                sc.dma("sp", wg_b[:], wb["wg"][layer * 1024:(layer + 1) * 1024, :].rearrange("(p k) c -> p k c", k=8),
                       reads=wbk("wg", layer * 1024, (layer + 1) * 1024), writes=["wg"])
                sc.dma("sp", wp_b[:], wb["wp"][layer * 256:(layer + 1) * 256, :].rearrange("(p k) c -> p k c", k=2),
                       reads=wbk("wp", layer * 256, (layer + 1) * 256), writes=["wp"])
                bank = [0]

                def nb():
                    b = bank[0]
                    bank[0] = (b + 1) % 5
                    return b

                def load_x(cc):
                    sc.dma("pool", xch[:], x_src[s, :, cc * TT:(cc + 1) * TT].rearrange("(k p) t -> p k t", p=128),
                           reads=[("xsrc", layer, s)], writes=["xch"])

                def load_p(cc):
                    sc.dma("pool", pb[:], pT[layer, s, :, cc * TT:(cc + 1) * TT].rearrange("(k p) t -> p k t", p=128), writes=["pb"])

                load_x(0)
                load_p(0)
                for c in range(NCH):
                    t0 = c * TT
                    tasks = [("w1", j) for j in range(32)] + [("w2", m) for m in range(8)]
                    issued = [0]
                    slot_of = {}

                    def issue_upto(n):
                        while issued[0] < min(n, len(tasks)):
                            kind, idx = tasks[issued[0]]
                            if kind == "w1":
                                slot = idx % NW1
                                r0 = (layer * 32 + idx) * 128
                                sc.dma("sp", w1buf[slot][:], wb["w1"][r0:r0 + 128, :].rearrange("p (k c) -> p k c", k=8),
                                       reads=wbk("w1", r0, r0 + 128), writes=[("w1buf", slot)])
                            else:
                                slot = idx % NW2
                                r0 = (layer * 8 + idx) * 256
                                sc.dma("sp", w2buf[slot][:],
                                       wb["w2"][r0:r0 + 256, :].rearrange("(p h) (j c) -> p (h j) c", h=2, c=128),
                                       reads=wbk("w2", r0, r0 + 256), writes=[("w2buf", slot)])
                            issued[0] += 1

                    issue_upto(NW1)
                    for m in range(8):
                        b = nb()
                        for k in range(8):
                            sc.op("pe", lambda e, m=m, k=k, b=b: e.matmul(psf[b][:], lhsT=wout_b[:, k, m * 128:(m + 1) * 128],
                                                                           rhs=mixedT[:, k, t0:t0 + TT], start=(k == 0), stop=(k == 7)),
                                  reads=["wout", ("mixed", k, c)], writes=[("ps", b)])
                        sc.op("dve", lambda e, m=m, b=b: e.scalar_tensor_tensor(out=z[:, m, :], in0=xch[:, m, :], scalar=DN_ALPHA,
                                                                               in1=psf[b][:], op0=ALU.mult, op1=ALU.add),
                              reads=["xch", ("ps", b)], writes=[("acc", m)])
                    if c + 1 < NCH:
                        load_x(c + 1)
                    ln_feature_major(z, "acc", layer, 1, hf, "hf", hb, "hb", hid, st, "st")
                    for m in range(8):
                        b = nb()
                        for k in range(8):
                            sc.op("pe", lambda e, m=m, k=k, b=b: e.matmul(psf[b][:], lhsT=wg_b[:, k, m * 128:(m + 1) * 128],
                                                                           rhs=hb[:, k, :], start=(k == 0), stop=(k == 7)),
                                  reads=["wg", ("hb", k)], writes=[("ps", b)])
                        r = rl[m % 2]
                        sc.op("act", lambda e, b=b, r=r: e.activation(out=r[:], in_=psf[b][:], func=AF.Sigmoid),
                              reads=[("ps", b)], writes=[("rl", m % 2)])
                        b2 = nb()
                        for k in range(2):
                            sc.op("pe", lambda e, m=m, k=k, b2=b2: e.matmul(psf[b2][:], lhsT=wp_b[:, k, m * 128:(m + 1) * 128],
                                                                             rhs=pb[:, k, :], start=(k == 0), stop=(k == 1)),
                                  reads=["wp", "pb"], writes=[("ps", b2)])
                        sc.op("dve", lambda e, m=m, b2=b2, r=r: e.tensor_tensor(out=z[:, m, :], in0=r[:], in1=psf[b2][:], op=ALU.mult),
                              reads=[("rl", m % 2), ("ps", b2)], writes=[("acc", m)])
                        sc.op("dve", lambda e, m=m: e.scalar_tensor_tensor(out=z[:, m, :], in0=hf[:, m, :], scalar=DN_ALPHA,
                                                                          in1=z[:, m, :], op0=ALU.mult, op1=ALU.add),
                              reads=[("hf", m), ("acc", m)], writes=[("acc", m)])
                    if c + 1 < NCH:
                        load_p(c + 1)
                    for j in range(32):
                        issue_upto(min(j + NW1, 32 + NW2))
                        slot = j % NW1
                        b = nb()
                        for k in range(8):
                            sc.op("pe", lambda e, k=k, b=b, slot=slot: e.matmul(psf[b][:], lhsT=w1buf[slot][:, k, :], rhs=hb[:, k, :],
                                                                                 start=(k == 0), stop=(k == 7)),
                                  reads=[("w1buf", slot), ("hb", k)], writes=[("ps", b)])
                        r = rl[j % 2]
                        sc.op("act", lambda e, b=b, r=r: e.activation(out=r[:], in_=psf[b][:], func=AF.Relu),
                              reads=[("ps", b)], writes=[("rl", j % 2)])
                        eng = "dve"
                        sc.op(eng, lambda e, j=j, r=r: e.tensor_tensor(out=hid[:, j, :], in0=r[:], in1=r[:], op=ALU.mult),
                              reads=[("rl", j % 2)], writes=[("hid", j)])
                    for m in range(8):
                        issue_upto(32 + m + NW2)
                        slot = m % NW2
                        b = nb()
                        for j in range(32):
                            sc.op("pe", lambda e, j=j, b=b, slot=slot: e.matmul(psf[b][:], lhsT=w2buf[slot][:, j, :], rhs=hid[:, j, :],
                                                                                 start=(j == 0), stop=(j == 31)),
                                  reads=[("w2buf", slot), ("hid", j)], writes=[("ps", b)])
                        sc.op("dve", lambda e, m=m, b=b: e.tensor_tensor(out=z[:, m, :], in0=z[:, m, :], in1=psf[b][:], op=ALU.add),
                              reads=[("acc", m), ("ps", b)], writes=[("acc", m)])
                    ln_feature_major(z, "acc", layer, 2, hf, "hf", hb, "hb", hid, st, "st")
                    hfr = [("hf", m) for m in range(8)]
                    hbr = [("hb", m) for m in range(8)]
                    if final:
                        sc.dma("pool", yT[s, :, t0:t0 + TT].rearrange("(k p) t -> p k t", p=128), hf[:], reads=hfr,
                               writes=[("y", s, c)])
                    else:
                        sc.dma("pool", x1T[s, :, t0:t0 + TT].rearrange("(k p) t -> p k t", p=128), hf[:], reads=hfr,
                               writes=[("xsrc", 1, s)])
                        sc.dma("pool", x1b[s, :, t0:t0 + TT].rearrange("(k p) t -> p k t", p=128), hb[:], reads=hbr,
                               writes=[("x1b", s)])
                sc.barrier()

        def layer0_mixer(s):
            with contextlib.ExitStack() as esA:
                eA = esA.enter_context

                def sbA(name, shape, dt):
                    return eA(nc.sbuf_tensor("a%d_" % s + name, list(shape), dt))

                qT = sbA("qT", [128, 8, S], BF16)
                kT = sbA("kT", [128, S], BF16)
                vA = sbA("vA", [128, NT, 65], BF16)
                iqT = sbA("iqT", [64, 4, S], BF16)
                ikT = sbA("ikT", [64, S], BF16)
                iw = sbA("iw", [128, NT, 4], F32)
                sc.dma("pool", kT[64:68, :], c_kaug[:, :], writes=["kT_aug"])
                for h in range(8):
                    sc.dma("pool", qT[64:68, h, :], c_qaug[:, h * S:(h + 1) * S], writes=[("qT_aug", h)])
                sc.op("dve", lambda e: e.memset(vA[:, :, 64:65], 1.0), writes=["vA_ones"])
                with contextlib.ExitStack() as esB:
                    eB = esB.enter_context

                    def sbB(name, shape, dt):
                        return eB(nc.sbuf_tensor("b%d_" % s + name, list(shape), dt))

                    win_b = sbB("win", [128, 8, WIN_COLS], BF16)
                    gates_b = sbB("gates", [128, 8, 128], BF16)
                    xbuf = [sbB("xb%d" % i, [128, 8, TT], BF16) for i in range(2)]
                    xrh = sbB("xrh", [128, 4, TT + 3], F32)
                    hlast = sbB("hlast", [128, 4], F32)
                    Tsets = [{n: sbB("t%d_" % i + n, [128, TT], F32) for n in
                              ["xc", "r", "ig", "a", "a2", "u", "h", "y", "g"]} for i in range(2)]
                    xcbs = [sbB("xcb%d" % i, [128, TT], BF16) for i in range(2)]
                    sc.dma("sp", win_b[:], wb["w_in"].rearrange("(p k) c -> p k c", k=8), reads=wbk("w_in"), writes=["win"])
                    sc.dma("sp", gates_b[:], wb["gates"].rearrange("p (k c) -> p k c", k=8), reads=wbk("gates"), writes=["gates"])
                    sc.op("dve", lambda e: e.memset(xrh[:, :, 0:3], 0.0), writes=[("xrh_halo", b) for b in range(4)])
                    sc.op("dve", lambda e: e.memset(hlast[:], 0.0), writes=[("hlast", b) for b in range(4)])
                    bank = [0]

                    def nb():
                        b = bank[0]
                        bank[0] = (b + 1) % 6
                        return b

                    def proj(xb, c0, M, b):
                        for k in range(8):
                            sc.op("pe", lambda e, k=k: e.matmul(psf[b][0:M, :], lhsT=win_b[:, k, c0:c0 + M], rhs=xb[:, k, :],
                                                                 start=(k == 0), stop=(k == 7)),
                                  reads=["win", ("xb", c % 2)], writes=[("ps", b)])

                    def load_xb(cc):
                        sc.dma("pool", xbuf[cc % 2][:], xT[s, :, cc * TT:(cc + 1) * TT].rearrange("(k p) t -> p k t", p=128),
                               writes=[("xb", cc % 2)])

                    load_xb(0)
                    for c in range(NCH):
                        t0 = c * TT
                        xb = xbuf[c % 2]
                        if c + 1 < NCH:
                            load_xb(c + 1)
                        conv_pump(5)
                        for h in range(8):
                            b = nb()
                            proj(xb, 1024 + 64 * h, 64, b)
                            sc.op("act", lambda e, h=h, b=b: e.activation(out=qT[0:64, h, t0:t0 + TT], in_=psf[b][0:64, :], func=AF.Copy),
                                  reads=[("ps", b)], writes=[("qT", h, c)])
                        b = nb()
                        proj(xb, 1536, 64, b)
                        sc.op("dve", lambda e, b=b: e.tensor_copy(out=kT[0:64, t0:t0 + TT], in_=psf[b][0:64, :]),
                              reads=[("ps", b)], writes=[("kT", c)])
                        for h in range(4):
                            b = nb()
                            proj(xb, 1600 + 64 * h, 64, b)
                            sc.op("act", lambda e, h=h, b=b: e.activation(out=iqT[:, h, t0:t0 + TT], in_=psf[b][0:64, :], func=AF.Copy),
                                  reads=[("ps", b)], writes=[("iqT", h, c)])
                        b = nb()
                        proj(xb, 1856, 64, b)
                        sc.op("dve", lambda e, b=b: e.tensor_copy(out=ikT[:, t0:t0 + TT], in_=psf[b][0:64, :]),
                              reads=[("ps", b)], writes=[("ikT", c)])
                        for tt in range(4 if dbg_stage != "A1" else 0):
                            tile_i = c * 4 + tt
                            b = nb()
                            for k in range(8):
                                sc.op("pe", lambda e, k=k, tt=tt, b=b: e.matmul(psf[b][:, 0:68], lhsT=xb[:, k, tt * 128:(tt + 1) * 128],
                                                                                 rhs=win_b[:, k, WIN_TM:WIN_TM + 68],
                                                                                 start=(k == 0), stop=(k == 7)),
                                      reads=["win", ("xb", c % 2)], writes=[("ps", b)])
                            sc.op("act", lambda e, b=b, tile_i=tile_i: e.activation(out=vA[:, tile_i, 0:64], in_=psf[b][:, 0:64], func=AF.Copy),
                                  reads=[("ps", b), "vA_ones"], writes=[("vA", tile_i)])
                            sc.op("dve", lambda e, b=b, tile_i=tile_i: e.tensor_scalar(out=iw[:, tile_i, :], in0=psf[b][:, 64:68],
                                                                                     scalar1=1.0 / 16.0, scalar2=None, op0=ALU.mult),
                                  reads=[("ps", b)], writes=[("iw", tile_i)])
                            if DBG_BARRIER:
                                sc.barrier()
                        def rg_block(blk):
                            T = Tsets[blk % 2]
                            xcb = xcbs[blk % 2]
                            tb = blk % 2
                            b = nb()
                            yield
                            proj(xb, 128 * blk, 128, b)
                            if c > 0:
                                yield
                                sc.op("dve", lambda e, blk=blk: e.tensor_copy(out=xrh[:, blk, 0:3], in_=xrh[:, blk, TT:TT + 3]),
                                      reads=[("xrh", blk)], writes=[("xrh_halo", blk)])
                            yield
                            sc.op("act", lambda e, blk=blk, b=b: e.activation(out=xrh[:, blk, 3:TT + 3], in_=psf[b][:], func=AF.Copy),
                                  reads=[("ps", b), ("xrh_halo", blk)], writes=[("xrh", blk)])
                            by = nb()
                            yield
                            proj(xb, 512 + 128 * blk, 128, by)
                            yield
                            sc.op("act", lambda e, by=by: e.activation(out=T["y"][:], in_=psf[by][:], func=AF.Copy),
                                  reads=[("ps", by)], writes=[("t_y", tb)])
                            cw = SP_CONVW + blk * 4
                            yield
                            sc.op("dve", lambda e, blk=blk, cw=cw: e.tensor_scalar(out=T["xc"][:], in0=xrh[:, blk, 3:TT + 3],
                                                                                 scalar1=small[:, cw + 3:cw + 4],
                                                                                 scalar2=small[:, SP_CONVB + blk:SP_CONVB + blk + 1],
                                                                                 op0=ALU.mult, op1=ALU.add),
                                  reads=[("xrh", blk), ("xrh_halo", blk), "small"], writes=[("t_xc", tb)])
                            for tap in (2, 1, 0):
                                yield
                                sc.op("dve", lambda e, blk=blk, cw=cw, tap=tap: e.scalar_tensor_tensor(
                                    out=T["xc"][:], in0=xrh[:, blk, tap:tap + TT], scalar=small[:, cw + tap:cw + tap + 1],
                                    in1=T["xc"][:], op0=ALU.mult, op1=ALU.add),
                                    reads=[("xrh", blk), ("xrh_halo", blk), ("t_xc", tb), "small"], writes=[("t_xc", tb)])
                            yield
                            sc.op("act", lambda e: e.activation(out=xcb[:], in_=T["xc"][:], func=AF.Copy), reads=[("t_xc", tb)], writes=[("xcb", tb)])
                            ba, bx = nb(), nb()
                            yield
                            sc.op("pe", lambda e, blk=blk, ba=ba: e.matmul(psf[ba][:], lhsT=gates_b[:, blk, :], rhs=xcb[:], start=True, stop=True),
                                  reads=["gates", ("xcb", tb)], writes=[("ps", ba)])
                            yield
                            sc.op("pe", lambda e, blk=blk, bx=bx: e.matmul(psf[bx][:], lhsT=gates_b[:, 4 + blk, :], rhs=xcb[:], start=True, stop=True),
                                  reads=["gates", ("xcb", tb)], writes=[("ps", bx)])
                            yield
                            sc.op("act", lambda e, blk=blk, ba=ba: e.activation(out=T["r"][:], in_=psf[ba][:], func=AF.Sigmoid,
                                                                                bias=small[:, SP_GAB + blk:SP_GAB + blk + 1]),
                                  reads=[("ps", ba), "small"], writes=[("t_r", tb)])
                            yield
                            sc.op("act", lambda e, blk=blk, bx=bx: e.activation(out=T["ig"][:], in_=psf[bx][:], func=AF.Sigmoid,
                                                                                bias=small[:, SP_GXB + blk:SP_GXB + blk + 1]),
                                  reads=[("ps", bx), "small"], writes=[("t_ig", tb)])
                            yield
                            sc.op("pool", lambda e: e.tensor_tensor(out=T["g"][:], in0=T["y"][:], in1=T["y"][:], op=ALU.mult),
                                  reads=[("t_y", tb)], writes=[("t_g", tb)])
                            yield
                            sc.op("dve", lambda e: e.tensor_scalar(out=T["g"][:], in0=T["g"][:], scalar1=0.044715, scalar2=1.0,
                                                                   op0=ALU.mult, op1=ALU.add), reads=[("t_g", tb)], writes=[("t_g", tb)])
                            yield
                            sc.op("pool", lambda e: e.tensor_tensor(out=T["g"][:], in0=T["g"][:], in1=T["y"][:], op=ALU.mult),
                                  reads=[("t_g", tb), ("t_y", tb)], writes=[("t_g", tb)])
                            yield
                            sc.op("act", lambda e: e.activation(out=T["g"][:], in_=T["g"][:], func=AF.Sigmoid, scale=1.5957691216057308),
                                  reads=[("t_g", tb)], writes=[("t_g", tb)])
                            yield
                            sc.op("pool", lambda e: e.tensor_tensor(out=T["g"][:], in0=T["g"][:], in1=T["y"][:], op=ALU.mult),
                                  reads=[("t_g", tb), ("t_y", tb)], writes=[("t_g", tb)])
                            yield
                            sc.op("act", lambda e, blk=blk: e.activation(out=T["a"][:], in_=T["r"][:], func=AF.Exp, scale=negc[:, blk:blk + 1]),
                                  reads=[("t_r", tb), "negc"], writes=[("t_a", tb)])
                            yield
                            sc.op("pool", lambda e: e.tensor_tensor(out=T["a2"][:], in0=T["a"][:], in1=T["a"][:], op=ALU.mult),
                                  reads=[("t_a", tb)], writes=[("t_a2", tb)])
                            yield
                            sc.op("act", lambda e: e.activation(out=T["a2"][:], in_=T["a2"][:], func=AF.Sqrt, scale=-1.0, bias=1.0),
                                  reads=[("t_a2", tb)], writes=[("t_a2", tb)])
                            yield
                            sc.op("dve", lambda e: e.tensor_tensor(out=T["u"][:], in0=T["ig"][:], in1=T["xc"][:], op=ALU.mult),
                                  reads=[("t_ig", tb), ("t_xc", tb)], writes=[("t_u", tb)])
                            yield
                            sc.op("dve", lambda e: e.tensor_tensor(out=T["u"][:], in0=T["u"][:], in1=T["a2"][:], op=ALU.mult),
                                  reads=[("t_u", tb), ("t_a2", tb)], writes=[("t_u", tb)])
                            yield
                            sc.op("dve", lambda e, blk=blk: e.tensor_tensor_scan(out=T["h"][:], data0=T["a"][:], data1=T["u"][:],
                                                                               initial=hlast[:, blk:blk + 1], op0=ALU.mult, op1=ALU.add),
                                  reads=[("t_a", tb), ("t_u", tb), ("hlast", blk)], writes=[("t_h", tb)])
                            yield
                            sc.op("dve", lambda e, blk=blk: e.tensor_copy(out=hlast[:, blk:blk + 1], in_=T["h"][:, TT - 1:TT]),
                                  reads=[("t_h", tb)], writes=[("hlast", blk)])
                            yield
                            sc.op("dve", lambda e, blk=blk: e.tensor_tensor(out=mixedT[:, blk, t0:t0 + TT], in0=T["h"][:], in1=T["g"][:], op=ALU.mult),
                                  reads=[("t_h", tb), ("t_g", tb)], writes=[("mixed", blk, c)])
                        if dbg_stage not in ("A1", "A2"):
                            for pair in ((0, 1), (2, 3)):
                                gens = [rg_block(bk) for bk in pair]
                                while gens:
                                    for g_ in list(gens):
                                        try:
                                            next(g_)
                                        except StopIteration:
                                            gens.remove(g_)
                    if dbg_stage in ("A", "A1", "A2") and s == 0:
                        sc.barrier()
                        dbg("mixed_rec", mixedT[:, 0:4, :], [128, 4, S], "x", BF16)
                        dbg("qT", qT[0:68, :, :], [68, 8, S], "x", BF16)
                        dbg("kT", kT[0:68, :], [68, S], "x", BF16)
                        dbg("vA", vA[:], [128, NT, 65], "x", BF16)
                        dbg("iqT", iqT[:], [64, 4, S], "x", BF16)
                        dbg("ikT", ikT[:], [64, S], "x", BF16)
                        dbg("iw", iw[:], [128, NT, 4], "x", F32)
                    sc.barrier()
                with contextlib.ExitStack() as esB:
                    eB = esB.enter_context

                    def sbB(name, shape, dt):
                        return eB(nc.sbuf_tensor("d%d_" % s + name, list(shape), dt))

                    isc = [sbB("isc%d" % i, [128, S], F32) for i in range(2)]
                    work = [sbB("work%d" % i, [128, S], F32) for i in range(2)]
                    eqm = [sbB("eqm%d" % i, [128, S], BF16) for i in range(4)]
                    rl = [sbB("rl%d" % i, [128, TT], F32) for i in range(4)]
                    m8 = [sbB("m8_%d" % i, [128, 8], F32) for i in range(2)]
                    P = [sbB("P%d" % i, [128, 512], BF16) for i in range(5)]
                    rc = sbB("rc", [128, 8], F32)
                    o_b = sbB("o_b", [128, 512], BF16)
                    def idx_topk(i):
                        L = 128 * (i + 1)
                        q0 = i * 128
                        I = isc[i % 2]
                        E = eqm[i % 4]
                        ik_ = ("isc", i % 2)
                        ek_ = ("eqm", i % 4)
                        nkb = (L + 511) // 512
                        for kb in range(nkb):
                            w = min(512, L - kb * 512)
                            k0 = kb * 512
                            for hI in range(4):
                                sc.op("pe", lambda e: e.matmul(psf[hI][:, 0:w], lhsT=iqT[:, hI, q0:q0 + 128],
                                                               rhs=ikT[:, k0:k0 + w], start=True, stop=True),
                                      reads=["iqT", "ikT"], writes=[("ps", hI)])
                                sc.op("act", lambda e: e.activation(out=rl[hI][:, 0:w], in_=psf[hI][:, 0:w], func=AF.Relu),
                                      reads=[("ps", hI)], writes=[("rl", hI)])
                            sc.op("dve", lambda e: e.tensor_scalar(out=I[:, k0:k0 + w], in0=rl[0][:, 0:w], scalar1=iw[:, i, 0:1],
                                                                   scalar2=None, op0=ALU.mult),
                                  reads=[("rl", 0), "iw"], writes=[ik_])
                            for hI in range(1, 4):
                                sc.op("dve", lambda e: e.scalar_tensor_tensor(
                                    out=I[:, k0:k0 + w], in0=rl[hI][:, 0:w], scalar=iw[:, i, hI:hI + 1], in1=I[:, k0:k0 + w],
                                    op0=ALU.mult, op1=ALU.add), reads=[("rl", hI), "iw", ik_], writes=[ik_])
                        sc.op("dve", lambda e: e.tensor_tensor(out=I[:, q0:q0 + 128], in0=I[:, q0:q0 + 128], in1=causal_f[:], op=ALU.add),
                              reads=[ik_, "causal_f"], writes=[ik_])
                        if dbg_stage == "B1":
                            if i in (1, 5):
                                dbg("isc%d" % i, I[:, 0:L], [128, L], ik_, F32)
                            return
                        if i < 2:
                            sc.op("dve", lambda e: e.tensor_scalar(out=E[:, 0:L], in0=I[:, 0:L], scalar1=-1.0e29, scalar2=None, op0=ALU.is_lt),
                                  reads=[ik_], writes=[ek_])
                        else:
                            for r in range(32):
                                src_ = I if r == 0 else work[i % 2]
                                sk = ik_ if r == 0 else ("work", i % 2)
                                yield
                                sc.op("dve", lambda e: e.max(out=m8[i % 2][:], in_=src_[:, 0:L]), reads=[sk], writes=[("m8", i % 2)])
                                yield
                                sc.op("dve", lambda e: e.match_replace(out=work[i % 2][:, 0:L], in_to_replace=m8[i % 2][:], in_values=src_[:, 0:L],
                                                                       imm_value=NEG_REPL), reads=[sk, ("m8", i % 2)], writes=[("work", i % 2)])
                            yield
                            sc.op("dve", lambda e: e.tensor_tensor(out=E[:, 0:L], in0=work[i % 2][:, 0:L], in1=I[:, 0:L], op=ALU.is_equal),
                                  reads=[("work", i % 2), ik_], writes=[ek_])
                        if dbg_stage in ("B2",) and s == 0 and i in (1, 5):
                            dbg("eqm%d" % i, E[:, 0:L], [128, L], ek_, BF16)
                            dbg("isc%d" % i, I[:, 0:L], [128, L], ik_, F32)

                    def attention(i):
                        q0 = i * 128
                        E = eqm[i % 4]
                        ek_ = ("eqm", i % 4)
                        ng = (i + 4) // 4
                        units = [(h, g) for h in range(8) for g in range(ng)]

                        def qk(u):
                            h, g = units[u]
                            nj = min(4, i + 1 - 4 * g)
                            sbank = u % 4
                            for jj in range(nj):
                                j = 4 * g + jj
                                sc.op("pe", lambda e: e.matmul(psf[sbank][:, jj * 128:(jj + 1) * 128], lhsT=kT[0:68, j * 128:(j + 1) * 128],
                                                               rhs=qT[0:68, h, q0:q0 + 128], start=True, stop=False),
                                      reads=["qT", "kT"], writes=[("ps", sbank)])
                                sc.op("pe", lambda e: e.matmul(psf[sbank][:, jj * 128:(jj + 1) * 128], lhsT=E[:, j * 128:(j + 1) * 128],
                                                               rhs=identneg_b[:], start=False, stop=True),
                                      reads=[ek_, "identneg_b"], writes=[("ps", sbank)])

                        def tail_dve(hh):
                            ob = psf[4 + hh]
                            sc.op("dve", lambda e: e.reciprocal(out=rc[:, hh * 4:(hh + 1) * 4],
                                                                in_=ob[:, 0:260].rearrange("p (h c) -> p h c", c=65)[:, :, 64]),
                                  reads=[("ps", 4 + hh)], writes=[("rc", hh)])
                            for h4 in range(4):
                                h = hh * 4 + h4
                                sc.op("dve", lambda e: e.tensor_scalar(out=o_b[:, h * 64:(h + 1) * 64], in0=ob[:, h4 * 65:h4 * 65 + 64],
                                                                       scalar1=rc[:, h:h + 1], scalar2=None, op0=ALU.mult),
                                      reads=[("ps", 4 + hh), ("rc", hh)], writes=[("o_b", hh)])

                        def tail_pe(hh):
                            for kk in (2 * hh, 2 * hh + 1):
                                sc.op("pe", lambda e: e.transpose(out=psb[:, kk * 128:(kk + 1) * 128], in_=o_b[:, kk * 128:(kk + 1) * 128],
                                                                  identity=ident_b[:]),
                                      reads=[("o_b", hh), "ident_b"], writes=[("ps", "b")])
                            sc.op("act", lambda e: e.activation(out=mixedT[:, 4 + 2 * hh:6 + 2 * hh, q0:q0 + 128],
                                                                in_=psb[:, hh * 256:(hh + 1) * 256].rearrange("p (k t) -> p k t", k=2), func=AF.Copy),
                                  reads=[("ps", "b")], writes=[("mixed_att", i, hh)])

                        deferred = []
                        LOOK = 3
                        for u0 in range(min(LOOK, len(units))):
                            qk(u0)
                        for u in range(len(units)):
                            h, g = units[u]
                            nj = min(4, i + 1 - 4 * g)
                            sbank = u % 4
                            Pt = P[u % 5]
                            pk = ("P", u % 5)
                            if u + LOOK < len(units):
                                qk(u + LOOK)
                            sc.op("act", lambda e: e.activation(out=Pt[:, 0:nj * 128], in_=psf[sbank][:, 0:nj * 128], func=AF.Exp, scale=0.125),
                                  reads=[("ps", sbank)], writes=[pk])
                            ob = psf[4 + h // 4]
                            oc = (h % 4) * 65
                            for jj in range(nj):
                                j = 4 * g + jj
                                sc.op("pe", lambda e: e.matmul(ob[:, oc:oc + 65], lhsT=Pt[:, jj * 128:(jj + 1) * 128], rhs=vA[:, j, :],
                                                               start=(g == 0 and jj == 0), stop=(g == ng - 1 and jj == nj - 1)),
                                      reads=[pk, "vA"], writes=[("ps", 4 + h // 4)])
                            for d in deferred:
                                d[0] -= 1
                            while deferred and deferred[0][0] <= 0:
                                tail_pe(deferred.pop(0)[1])
                            if g == ng - 1 and h % 4 == 3:
                                tail_dve(h // 4)
                                deferred.append([4, h // 4])
                        for d in deferred:
                            tail_pe(d[1])

                    def run_pair(p):
                        gens = [idx_topk(t) for t in (2 * p, 2 * p + 1)]
                        while gens:
                            for g_ in list(gens):
                                try:
                                    next(g_)
                                except StopIteration:
                                    gens.remove(g_)

                    run_pair(0)
                    for p in range(NT // 2):
                        if p + 1 < NT // 2:
                            run_pair(p + 1)
                        conv_pump(4)
                        if dbg_stage in ("B1", "B2"):
                            continue
                        attention(2 * p)
                        attention(2 * p + 1)
                    if dbg_stage in ("A", "B") and s == 0:
                        sc.barrier()
                        dbg("mixed_att", mixedT[:, 4:8, :], [128, 4, S], "x", BF16)
                    sc.barrier()

        def layer1_mixer(s):
            with contextlib.ExitStack() as esA:
                eA = esA.enter_context

                def sbA(name, shape, dt):
                    return eA(nc.sbuf_tensor("m%d_" % s + name, list(shape), dt))

                cqn = sbA("cqn", [128, 4, S], BF16)
                ckvn = sbA("ckvn", [128, 2, S], BF16)
                krope = sbA("krope", [128, S], BF16)
                vM = sbA("vM", [128, NT, 16, 65], BF16)
                cosF = sbA("cossin", [128, S], F32)
                sinS = cosF
                wuq_b = sbA("wuq", [128, 4, 2048], BF16)
                wukv_b = sbA("wukv", [128, 2, 2048], BF16)
                sc.dma("sp", cosF[64:96, :], c_cos[:, :], writes=["cosF"])
                sc.dma("sp", sinS[96:128, :], c_sin[:, :], writes=["sinS"])
                sc.dma("sp", wuq_b[:], wb["w_uq"].rearrange("(p k) c -> p k c", k=4), reads=wbk("w_uq"), writes=["wuq"])
                sc.dma("sp", wukv_b[:], wb["w_ukv"].rearrange("(p k) c -> p k c", k=2), reads=wbk("w_ukv"), writes=["wukv"])
                sc.op("dve", lambda e: e.memset(vM[:, :, :, 64:65], 1.0), writes=["vM_ones"])
                bank = [0]

                def nb():
                    b = bank[0]
                    bank[0] = (b + 1) % 4
                    return b

                with contextlib.ExitStack() as esB:
                    eB = esB.enter_context

                    def sbB(name, shape, dt):
                        return eB(nc.sbuf_tensor("n%d_" % s + name, list(shape), dt))

                    wdn_b = sbB("wdn", [128, 8, 896], BF16)
                    xbuf = [sbB("xb%d" % i, [128, 8, TT], BF16) for i in range(2)]
                    cf = sbB("cf", [128, 6, TT], F32)
                    sq = sbB("sq", [128, 6, TT], F32)
                    rs = sbB("rs", [128, 2, TT], F32)
                    rt = sbB("rt", [128, 2, TT], F32)
                    sc.dma("sp", wdn_b[:], wb["w_down"].rearrange("(p k) c -> p k c", k=8), reads=wbk("w_down"), writes=["wdn"])
                    def load_xb(cc):
                        sc.dma("pool", xbuf[cc % 2][:], x1b[s, :, cc * TT:(cc + 1) * TT].rearrange("(k p) t -> p k t", p=128),
                               reads=[("x1b", s)], writes=[("xb", cc % 2)])

                    load_xb(0)
                    for c in range(NCH):
                        t0 = c * TT
                        xb = xbuf[c % 2]
                        if c + 1 < NCH:
                            load_xb(c + 1)
                        for blk in range(7):
                            b = nb()
                            for k in range(8):
                                sc.op("pe", lambda e, k=k, blk=blk, b=b: e.matmul(psf[b][:], lhsT=wdn_b[:, k, blk * 128:(blk + 1) * 128],
                                                                                   rhs=xb[:, k, :], start=(k == 0), stop=(k == 7)),
                                      reads=["wdn", ("xb", c % 2)], writes=[("ps", b)])
                            if blk < 6:
                                sc.op("act", lambda e, blk=blk, b=b: e.activation(out=cf[:, blk, :], in_=psf[b][:], func=AF.Copy),
                                      reads=[("ps", b)], writes=[("cf", blk)])
                                sc.op("act", lambda e, blk=blk, b=b: e.activation(out=sq[:, blk, :], in_=psf[b][:], func=AF.Square),
                                      reads=[("ps", b)], writes=[("sq", blk)])
                            else:
                                sc.op("dve", lambda e, b=b: e.tensor_tensor(out=rt[64:96, 0, :], in0=psf[b][96:128, :], in1=sinS[96:128, t0:t0 + TT], op=ALU.mult),
                                      reads=[("ps", b), "sinS"], writes=["rt0"])
                                sc.op("dve", lambda e, b=b: e.tensor_tensor(out=rt[64:96, 1, :], in0=psf[b][64:96, :], in1=cosF[64:96, t0:t0 + TT], op=ALU.mult),
                                      reads=[("ps", b), "cosF"], writes=["rt1"])
                                sc.op("dve", lambda e: e.tensor_tensor(out=krope[64:96, t0:t0 + TT], in0=rt[64:96, 0, :], in1=rt[64:96, 1, :], op=ALU.add),
                                      reads=["rt0", "rt1"], writes=[("krope", c)])
                        for grp, (k0, nk, dim, nof) in enumerate([(0, 4, 512, SP_QN), (4, 2, 256, SP_KVN)]):
                            pq = psf[4 + grp]
                            for kk in range(nk):
                                sc.op("pe", lambda e, kk=kk, k0=k0, nk=nk, pq=pq: e.matmul(pq[:], lhsT=ones_f[:], rhs=sq[:, k0 + kk, :],
                                                                                          start=(kk == 0), stop=(kk == nk - 1)),
                                      reads=[("sq", k0 + kk), "ones_f"], writes=[("ps", 4 + grp)])
                            sc.op("act", lambda e, grp=grp, dim=dim, pq=pq: e.activation(out=rs[:, grp, :], in_=pq[:], func=AF.Ln, scale=1.0 / dim,
                                                                                         bias=eps_t[:, 1:2]),
                                  reads=[("ps", 4 + grp), "eps_t"], writes=[("rs", grp)])
                            sc.op("act", lambda e, grp=grp: e.activation(out=rs[:, grp, :], in_=rs[:, grp, :], func=AF.Exp, scale=-0.5),
                                  reads=[("rs", grp)], writes=[("rs", grp)])
                            for kk in range(nk):
                                dst = cqn[:, kk, t0:t0 + TT] if grp == 0 else ckvn[:, kk, t0:t0 + TT]
                                sc.op("dve", lambda e, kk=kk, k0=k0, grp=grp, nof=nof, dst=dst: e.scalar_tensor_tensor(
                                    out=dst, in0=cf[:, k0 + kk, :], scalar=small[:, nof + kk:nof + kk + 1], in1=rs[:, grp, :],
                                    op0=ALU.mult, op1=ALU.mult),
                                    reads=[("cf", k0 + kk), ("rs", grp), "small"], writes=[("cn", grp, kk, c)])
                        for tt in range(4):
                            ti = c * 4 + tt
                            for half in range(2):
                                b = nb()
                                for kk in range(2):
                                    sc.op("pe", lambda e, kk=kk, tt=tt, half=half, b=b: e.matmul(
                                        psf[b][:], lhsT=ckvn[:, kk, t0 + tt * 128:t0 + (tt + 1) * 128],
                                        rhs=wukv_b[:, kk, 1024 + half * 512:1024 + (half + 1) * 512], start=(kk == 0), stop=(kk == 1)),
                                        reads=[("cn", 1, kk, c), "wukv"], writes=[("ps", b)])
                                sc.op("act", lambda e, ti=ti, half=half, b=b: e.activation(
                                    out=vM[:, ti, half * 8:(half + 1) * 8, 0:64], in_=psf[b][:].rearrange("p (h d) -> p h d", d=64), func=AF.Copy),
                                    reads=[("ps", b), "vM_ones"], writes=[("vM", ti, half)])
                    if dbg_stage == "D" and s == 0:
                        sc.barrier()
                        dbg("cqn", cqn[:], [128, 4, S], "x", BF16)
                        dbg("ckvn", ckvn[:], [128, 2, S], "x", BF16)
                        dbg("krope", krope[64:96, :], [32, S], "x", BF16)
                        dbg("vM", vM[:], [128, NT, 16, 65], "x", BF16)
                    sc.barrier()
                with contextlib.ExitStack() as esB:
                    eB = esB.enter_context

                    def sbB(name, shape, dt):
                        return eB(nc.sbuf_tensor("e%d_" % s + name, list(shape), dt))

                    qTb = [sbB("qT%d" % i, [128, 4, S], BF16) for i in range(2)]
                    kTb = [sbB("kT%d" % i, [128, 4, S], BF16) for i in range(2)]
                    rt = sbB("rt", [128, 2, TT], F32)
                    P = [sbB("P%d" % i, [128, 512], BF16) for i in range(5)]
                    rc = sbB("rc", [128, 8], F32)
                    o_b = sbB("o_b", [128, 512], BF16)
                    pcnt = [0]
                    def proj_gen(hg):
                        qT = qTb[hg % 2]
                        kT = kTb[hg % 2]
                        pb_ = hg % 2
                        b = 6
                        for hh in range(4):
                            head = hg * 4 + hh
                            for c in range(NCH):
                                t0 = c * TT
                                for kk in range(4):
                                    sc.op("pe", lambda e: e.matmul(psf[b][:], lhsT=wuq_b[:, kk, head * 128:(head + 1) * 128], rhs=cqn[:, kk, t0:t0 + TT],
                                                                   start=(kk == 0), stop=(kk == 3)), reads=["wuq", "cqn"], writes=[("ps", b)])
                                sc.op("dve", lambda e: e.tensor_copy(out=qT[0:64, hh, t0:t0 + TT], in_=psf[b][0:64, :]),
                                      reads=[("ps", b)], writes=[("qT", pb_, hh)])
                                sc.op("dve", lambda e: e.tensor_tensor(out=rt[64:96, 0, :], in0=psf[b][96:128, :], in1=sinS[96:128, t0:t0 + TT], op=ALU.mult),
                                      reads=[("ps", b), "sinS"], writes=["rt0"])
                                sc.op("dve", lambda e: e.tensor_tensor(out=rt[64:96, 1, :], in0=psf[b][64:96, :], in1=cosF[64:96, t0:t0 + TT], op=ALU.mult),
                                      reads=[("ps", b), "cosF"], writes=["rt1"])
                                sc.op("dve", lambda e: e.tensor_tensor(out=qT[64:96, hh, t0:t0 + TT], in0=rt[64:96, 0, :], in1=rt[64:96, 1, :], op=ALU.add),
                                      reads=["rt0", "rt1"], writes=[("qT", pb_, hh)])
                                yield
                            sc.op("pool", lambda e: e.tensor_copy(out=kT[64:96, hh, :], in_=krope[64:96, :]),
                                  reads=["krope"], writes=[("kT", pb_, hh)])
                        for pr in range(2):
                            h0 = hg * 4 + pr * 2
                            for c in range(NCH):
                                t0 = c * TT
                                for kk in range(2):
                                    sc.op("pe", lambda e: e.matmul(psf[b][:], lhsT=wukv_b[:, kk, h0 * 64:h0 * 64 + 128], rhs=ckvn[:, kk, t0:t0 + TT],
                                                                   start=(kk == 0), stop=(kk == 1)), reads=["wukv", "ckvn"], writes=[("ps", b)])
                                sc.op("dve", lambda e: e.tensor_copy(out=kT[0:64, pr * 2, t0:t0 + TT], in_=psf[b][0:64, :]),
                                      reads=[("ps", b)], writes=[("kT", pb_, pr * 2)])
                                sc.op("dve", lambda e: e.tensor_copy(out=kT[0:64, pr * 2 + 1, t0:t0 + TT], in_=psf[b][64:128, :]),
                                      reads=[("ps", b)], writes=[("kT", pb_, pr * 2 + 1)])
                                yield

                    gen_next = proj_gen(0)
                    for hg in range(4):
                        for _ in gen_next:
                            pass
                        gen_next = proj_gen(hg + 1) if hg + 1 < 4 else iter(())
                        qT = qTb[hg % 2]
                        kT = kTb[hg % 2]
                        pb_ = hg % 2
                        if dbg_stage == "E" and s == 0 and hg == 0:
                            sc.barrier()
                            dbg("qT", qT[0:96, :, :], [96, 4, S], "x", BF16)
                            dbg("kT", kT[0:96, :, :], [96, 4, S], "x", BF16)
                        units = [(i, hh, g) for i in range(NT) for hh in range(4) for g in range((i + 4) // 4)]

                        def qk(u):
                            i, hh, g = units[u]
                            q0 = i * 128
                            nj = min(4, i + 1 - 4 * g)
                            sbank = u % 4
                            for jj in range(nj):
                                j = 4 * g + jj
                                diag = (j == i)
                                sc.op("pe", lambda e: e.matmul(psf[sbank][:, jj * 128:(jj + 1) * 128], lhsT=kT[0:96, hh, j * 128:(j + 1) * 128],
                                                               rhs=qT[0:96, hh, q0:q0 + 128], start=True, stop=not diag),
                                      reads=[("qT", pb_, hh), ("kT", pb_, hh)], writes=[("ps", sbank)])
                                if diag:
                                    sc.op("pe", lambda e: e.matmul(psf[sbank][:, jj * 128:(jj + 1) * 128], lhsT=tri_b[:], rhs=identneg_b[:],
                                                                   start=False, stop=True),
                                          reads=["tri_b", "identneg_b"], writes=[("ps", sbank)])

                        def tail_dve(i):
                            ob = psf[4 + (i % 2)]
                            obk = ("ps", 4 + (i % 2))
                            sc.op("dve", lambda e: e.reciprocal(out=rc[:, (i % 2) * 4:(i % 2) * 4 + 4],
                                                                in_=ob[:, 0:260].rearrange("p (h c) -> p h c", c=65)[:, :, 64]),
                                  reads=[obk], writes=[("rc", i % 2)])
                            for hh in range(4):
                                sc.op("dve", lambda e: e.tensor_scalar(out=o_b[:, (i % 2) * 256 + hh * 64:(i % 2) * 256 + (hh + 1) * 64],
                                                                       in0=ob[:, hh * 65:hh * 65 + 64],
                                                                       scalar1=rc[:, (i % 2) * 4 + hh:(i % 2) * 4 + hh + 1], scalar2=None, op0=ALU.mult),
                                      reads=[obk, ("rc", i % 2)], writes=[("o_b", i % 2)])

                        def tail_pe(i):
                            q0 = i * 128
                            o0 = (i % 2) * 256
                            for kk in range(2):
                                sc.op("pe", lambda e: e.transpose(out=psb[:, o0 + kk * 128:o0 + (kk + 1) * 128],
                                                                  in_=o_b[:, o0 + kk * 128:o0 + (kk + 1) * 128], identity=ident_b[:]),
                                      reads=[("o_b", i % 2), "ident_b"], writes=[("ps", "b")])
                            sc.op("act", lambda e: e.activation(out=mixedT[:, 2 * hg:2 * hg + 2, q0:q0 + 128],
                                                                in_=psb[:, o0:o0 + 256].rearrange("p (k t) -> p k t", k=2), func=AF.Copy),
                                  reads=[("ps", "b")], writes=[("mixed_att", i)])

                        deferred = []
                        LOOK = 3
                        for u0 in range(min(LOOK, len(units))):
                            qk(u0)
                        for u in range(len(units)):
                            i, hh, g = units[u]
                            head = hg * 4 + hh
                            ng = (i + 4) // 4
                            nj = min(4, i + 1 - 4 * g)
                            sbank = u % 4
                            Pt = P[u % 5]
                            pk = ("P", u % 5)
                            if u + LOOK < len(units):
                                qk(u + LOOK)
                            sc.op("act", lambda e: e.activation(out=Pt[:, 0:nj * 128], in_=psf[sbank][:, 0:nj * 128], func=AF.Exp,
                                                                scale=96.0 ** -0.5),
                                  reads=[("ps", sbank)], writes=[pk])
                            ob = psf[4 + (i % 2)]
                            oc = hh * 65
                            for jj in range(nj):
                                j = 4 * g + jj
                                sc.op("pe", lambda e: e.matmul(ob[:, oc:oc + 65], lhsT=Pt[:, jj * 128:(jj + 1) * 128], rhs=vM[:, j, head, :],
                                                               start=(g == 0 and jj == 0), stop=(g == ng - 1 and jj == nj - 1)),
                                      reads=[pk, "vM"], writes=[("ps", 4 + (i % 2))])
                            for d in deferred:
                                d[0] -= 1
                            while deferred and deferred[0][0] <= 0:
                                tail_pe(deferred.pop(0)[1])
                            if g == ng - 1 and hh == 3:
                                tail_dve(i)
                                deferred.append([4, i])
                            if u % 5 == 4:
                                next(gen_next, None)
                        for d in deferred:
                            tail_pe(d[1])
                    if dbg_stage == "E" and s == 0:
                        sc.barrier()
                        dbg("mixed1", mixedT[:], [128, 8, S], "x", BF16)
                    sc.barrier()

        stop = False
        if dbg_stage == "0":
            dbg("negc", negc[:], [128, 8], "negc", F32)
        for s in range(2):
            if dbg_stage == "0":
                break
            layer0_mixer(s)
            if dbg_stage in ("A", "A1", "A2", "B", "B1", "B2"):
                break
            phase_c(0, s, xT, final=False)
            if dbg_stage == "C":
                break
            layer1_mixer(s)
            if dbg_stage in ("D", "E"):
                break
            phase_c(1, s, x1T, final=True)
        if dbg_stage == "C":
            dbg("x1T", x1T[0], [D, S], ("xsrc", 1, 0), F32)
        conv_pump(1000)
        sc.barrier(final=True)
        dbg_out["_ninst"] = (sc.ninst, dict(sc.ccnt))
    return nc, dbg_out


def _host_prep(inputs):
    f = np.float32
    g = {k: np.asarray(v, dtype=f) for k, v in inputs.items()}
    W = {}
    w_in = g["hy_w_in"][0]
    perm = np.concatenate([np.arange(0, 1600), np.arange(1664, 1984), np.arange(1600, 1664), np.arange(1984, 1988)])
    w = w_in[:, perm]
    W["w_in"] = np.ascontiguousarray(w.reshape(8, 128, WIN_COLS).transpose(1, 0, 2).reshape(1024, WIN_COLS))
    gates = np.zeros((128, 8, 128), f)
    for gi, name in enumerate(["hy_ga_w", "hy_gx_w"]):
        gw = g[name][0]
        for blk in range(4):
            for half in range(2):
                n = 2 * blk + half
                gates[half * 64:(half + 1) * 64, gi * 4 + blk, half * 64:(half + 1) * 64] = gw[n]
    W["gates"] = gates.reshape(128, 1024)

    def pk(wm, kch):
        r, c = wm.shape
        return np.ascontiguousarray(wm.reshape(kch, 128, c).transpose(1, 0, 2).reshape(128 * kch, c))

    W["w_out0"] = pk(g["hy_w_out"][0], 8)
    W["w_out1"] = pk(g["mla_w_out"][0], 8)
    w1 = g["mlp_w1"]
    W["w1"] = np.ascontiguousarray(w1.reshape(2, 8, 128, 32, 128).transpose(0, 3, 2, 1, 4).reshape(2 * 32 * 128, 1024))
    w2 = g["mlp_w2"]
    W["w2"] = np.ascontiguousarray(w2.reshape(2, 2, 16, 128, 8, 128).transpose(0, 4, 3, 1, 2, 5).reshape(2 * 8 * 128 * 2, 2048))
    W["wg"] = np.concatenate([pk(g["ple_w_gate"][l], 8) for l in range(2)], axis=0)
    W["wp"] = np.concatenate([pk(g["ple_w_proj"][l], 2) for l in range(2)], axis=0)
    wd = g["mla_w_down"][0]
    sw = (np.arange(32) + 16) % 32
    kr = wd[:, 768:800]
    wdn = np.concatenate([wd[:, 0:768], kr, kr[:, sw], kr, kr[:, sw]], axis=1)
    W["w_down"] = pk(wdn, 8)
    wuq = g["mla_w_uq"][0].reshape(512, 16, 96)
    wuq2 = np.concatenate([wuq[:, :, 0:64], wuq[:, :, 64:96], wuq[:, :, 64:96][:, :, sw]], axis=2).reshape(512, 2048)
    W["w_uq"] = pk(wuq2, 4)
    wukv = g["mla_w_ukv"][0].reshape(256, 16, 128)
    wukv2 = np.concatenate([wukv[:, :, 0:64].reshape(256, 1024), wukv[:, :, 64:128].reshape(256, 1024)], axis=1)
    W["w_ukv"] = pk(wukv2, 2)

    small = np.zeros((128, SP_N), f)

    def col(v, n):
        return v.reshape(n, 128).T

    for l in range(2):
        small[:, SP_LN + l * 32 + 0:SP_LN + l * 32 + 8] = col(g["ln1_g"][l], 8)
        small[:, SP_LN + l * 32 + 8:SP_LN + l * 32 + 16] = col(g["ln1_b"][l], 8)
        small[:, SP_LN + l * 32 + 16:SP_LN + l * 32 + 24] = col(g["ln2_g"][l], 8)
        small[:, SP_LN + l * 32 + 24:SP_LN + l * 32 + 32] = col(g["ln2_b"][l], 8)
    cw = g["hy_conv_w"][0]
    for blk in range(4):
        for tap in range(4):
            small[:, SP_CONVW + blk * 4 + tap] = cw[tap, blk * 128:(blk + 1) * 128]
    small[:, SP_CONVB:SP_CONVB + 4] = col(g["hy_conv_b"][0], 4)
    small[:, SP_GAB:SP_GAB + 4] = col(g["hy_ga_b"][0], 4)
    small[:, SP_GXB:SP_GXB + 4] = col(g["hy_gx_b"][0], 4)
    small[:, SP_LAM:SP_LAM + 4] = col(g["hy_lambda"][0], 4)
    small[:, SP_QN:SP_QN + 4] = col(g["mla_q_norm"][0], 4)
    small[:, SP_KVN:SP_KVN + 2] = col(g["mla_kv_norm"][0], 2)

    C = {}
    C["c_ident"] = np.eye(128, dtype=f)
    tl = np.arange(128)[:, None]
    sl = np.arange(128)[None, :]
    C["c_causal"] = np.where(sl <= tl, 0.0, NEG_FILL).astype(f)
    C["c_tri"] = (sl > tl).astype(f)
    pos = np.arange(S)
    C["c_kaug"] = np.stack([np.ones(S), np.ones(S), pos % 128, (pos // 128) * 128]).astype(f)
    qa = np.zeros((4, 8, S), f)
    for h in range(8):
        sl8 = 8.0 * 2.0 ** (-(h + 1))
        qa[0, h] = -sl8 * 128.0 * (pos // 128)
        qa[1, h] = -sl8 * (pos % 128)
        qa[2, h] = sl8
        qa[3, h] = sl8
    C["c_qaug"] = qa.reshape(4, 8 * S)
    freq = (np.float32(10000.0) ** (-np.arange(0, 32, 2, dtype=f) / np.float32(32))).astype(f)
    ang = pos.astype(f)[:, None] * freq[None, :]
    cos = np.cos(ang).astype(f).T
    sin = np.sin(ang).astype(f).T
    C["c_cos"] = np.concatenate([cos, cos], axis=0)
    C["c_sin"] = np.concatenate([-sin, sin], axis=0)
    return g, W, small, C


_NC_CACHE = {}


def _run(inputs, dbg_stage=None, n_cores=8):
    g, W, small, C = _host_prep(inputs)
    key = dbg_stage
    if key not in _NC_CACHE:
        _NC_CACHE[key] = build(dbg_stage)
    nc, dbg_out = _NC_CACHE[key]
    x = g["x"]
    p = g["p"]
    in_maps = []
    for c in range(n_cores):
        m = {}
        m["xT"] = np.ascontiguousarray(x[2 * c:2 * c + 2].transpose(0, 2, 1))
        m["pT"] = np.ascontiguousarray(p[:, 2 * c:2 * c + 2].transpose(0, 1, 3, 2))
        m["small"] = small
        m.update(C)
        m.update(W)
        in_maps.append(m)
    res = run_bass_kernel_spmd(nc, in_maps, core_ids=list(range(n_cores)))
    return res, dbg_out


def kernel(**inputs):
    res, _ = _run(inputs)
    out = np.empty((16, S, D), np.float32)
    for c in range(8):
        yT = np.asarray(res.results[c]["yT"])
        out[2 * c:2 * c + 2] = yT.transpose(0, 2, 1)
    return out
```
